# Optimizing a Trainium2 kernel written in Bass

```python
import math
import jax
import jax.numpy as jnp
from jax import lax
import numpy as np

D_MODEL = 1024
BATCH = 8
SEQ = 2048
DEPTH = 1

S5_WIDTH = D_MODEL // 2
S5_GROUP = 16
S5_GROUPS = S5_WIDTH // S5_GROUP
S5_STATE = 64
DT_MIN = 1e-3
DT_MAX = 1e-1

GLA_HEADS = 4
GLA_DK = D_MODEL // 16
GLA_DV = D_MODEL // 8
GLA_KEY = GLA_HEADS * GLA_DK
GLA_VAL = GLA_HEADS * GLA_DV
GLA_GATE_RANK = 16
GLA_GATE_TAU = 16.0
GLA_CHUNK = 64

N_EXPERTS = 256
TOP_K = 8
N_GROUPS = 8
TOPK_GROUPS = 4
EXPERT_FF = D_MODEL // 4
SHARED_FF = D_MODEL // 4
ROUTE_SCALE = 2.5
MOE_BLOCK = 128

EPS = 1e-6
IN_SPLITS = (S5_WIDTH, GLA_KEY, GLA_KEY, GLA_VAL, GLA_GATE_RANK, GLA_VAL, D_MODEL, D_MODEL)
IN_WIDTH = sum(IN_SPLITS)
IN_OFFSETS = [int(o) for o in np.cumsum(IN_SPLITS)[:-1]]

kernel_name = 'hybrid_s5_gla_moe_block'


def rmsnorm(x, g):
    xf = x.astype(jnp.float32)
    r = lax.rsqrt(jnp.mean(xf * xf, axis=-1, keepdims=True) + EPS)
    return (xf * r).astype(x.dtype) * g


def _cmul(ar, ai, br, bi):
    return ar * br - ai * bi, ar * bi + ai * br


def _affine_combine(left, right):
    a1r, a1i, b1r, b1i = left
    a2r, a2i, b2r, b2i = right
    ar, ai = _cmul(a2r, a2i, a1r, a1i)
    br, bi = _cmul(a2r, a2i, b1r, b1i)
    return ar, ai, br + b2r, bi + b2i


def s5_branch(u, lam_re, lam_im, log_dt, b_re, b_im, c_re, c_im, d_skip, w_glu, b_glu):
    bsz, seq, _ = u.shape
    f32 = jnp.float32
    ug = u.reshape(bsz, seq, S5_GROUPS, S5_GROUP).astype(f32)
    lr = lam_re.astype(f32)
    li = lam_im.astype(f32)
    dt = jnp.exp(log_dt.astype(f32))[:, None]
    mag = jnp.exp(lr * dt)
    abar_re = mag * jnp.cos(li * dt)
    abar_im = mag * jnp.sin(li * dt)
    den = lr * lr + li * li
    num_re = abar_re - 1.0
    coef_re = (num_re * lr + abar_im * li) / den
    coef_im = (abar_im * lr - num_re * li) / den
    bbar_re, bbar_im = _cmul(coef_re[..., None], coef_im[..., None], b_re.astype(f32), b_im.astype(f32))
    bu_re = jnp.einsum('blgh,gph->blgp', ug, bbar_re)
    bu_im = jnp.einsum('blgh,gph->blgp', ug, bbar_im)
    a_re = jnp.broadcast_to(abar_re, (1, seq) + abar_re.shape)
    a_im = jnp.broadcast_to(abar_im, (1, seq) + abar_im.shape)
    _, _, s_re, s_im = lax.associative_scan(_affine_combine, (a_re, a_im, bu_re, bu_im), axis=1)
    y = (jnp.einsum('blgp,ghp->blgh', s_re, c_re.astype(f32))
         - jnp.einsum('blgp,ghp->blgh', s_im, c_im.astype(f32))
         + d_skip.astype(f32) * ug)
    y = jax.nn.gelu(y.reshape(bsz, seq, S5_WIDTH)).astype(u.dtype)
    return y * jax.nn.sigmoid(y @ w_glu + b_glu)


def gla_chunk_scan(q, k, v, log_a):
    f32 = jnp.float32
    q, k, v, log_a = (t.astype(f32) for t in (q, k, v, log_a))
    bsz, nh, seq, dk = q.shape
    dv = v.shape[-1]
    nck = seq // GLA_CHUNK

    def to_chunks(t):
        return jnp.moveaxis(t.reshape(bsz, nh, nck, GLA_CHUNK, t.shape[-1]), 2, 0)

    qc, kc, vc, gc = (to_chunks(t) for t in (q, k, v, log_a))
    bc = jnp.cumsum(gc, axis=3)
    causal = jnp.tril(jnp.ones((GLA_CHUNK, GLA_CHUNK), dtype=bool))[:, :, None]

    def step(state, inp):
        qi, ki, vi, bi = inp
        b_last = bi[:, :, -1:, :]
        o_inter = jnp.einsum('bhcd,bhde->bhce', qi * jnp.exp(bi), state)
        rel = bi[:, :, :, None, :] - bi[:, :, None, :, :]
        decay = jnp.exp(jnp.where(causal, rel, -jnp.inf))
        scores = jnp.einsum('bhid,bhjd,bhijd->bhij', qi, ki, decay)
        out = o_inter + jnp.einsum('bhij,bhje->bhie', scores, vi)
        state = (jnp.exp(b_last[:, :, 0, :])[..., None] * state
                 + jnp.einsum('bhjd,bhje->bhde', ki * jnp.exp(b_last - bi), vi))
        return state, out

    s0 = jnp.zeros((bsz, nh, dk, dv), f32)
    _, oc = lax.scan(step, s0, (qc, kc, vc, bc))
    return jnp.moveaxis(oc, 0, 2).reshape(bsz, nh, seq, dv)


def gla_branch(q, k, v, gk_low, r, w_gk2, b_gk2, norm_g):
    bsz, seq, _ = q.shape

    def heads(t, dh):
        return t.reshape(bsz, seq, GLA_HEADS, dh).transpose(0, 2, 1, 3)

    log_a = jax.nn.log_sigmoid((gk_low @ w_gk2 + b_gk2).astype(jnp.float32)) / GLA_GATE_TAU
    o = gla_chunk_scan(heads(q, GLA_DK) * (GLA_DK ** -0.5), heads(k, GLA_DK),
                       heads(v, GLA_DV), heads(log_a, GLA_DK))
    o = rmsnorm(o.astype(q.dtype), norm_g)
    o = o.transpose(0, 2, 1, 3).reshape(bsz, seq, GLA_VAL)
    return o * jax.nn.silu(r)


def moe_ffn(h, w_router, router_bias, w_gate, w_up, w_down, ws_gate, ws_up, ws_down):
    bsz, seq, d = h.shape
    t = bsz * seq
    hf = h.reshape(t, d)
    scores = jax.nn.sigmoid((hf @ w_router).astype(jnp.float32))
    biased = scores + router_bias.astype(jnp.float32)
    grp = biased.reshape(t, N_GROUPS, N_EXPERTS // N_GROUPS)
    grp_score = lax.top_k(grp, 2)[0].sum(-1)
    _, grp_idx = lax.top_k(grp_score, TOPK_GROUPS)
    grp_mask = jnp.any(grp_idx[:, :, None] == jnp.arange(N_GROUPS)[None, None, :], axis=1)
    exp_mask = jnp.repeat(grp_mask, N_EXPERTS // N_GROUPS, axis=1)
    _, idx = lax.top_k(jnp.where(exp_mask, biased, -jnp.inf), TOP_K)
    wts = jnp.take_along_axis(scores, idx, axis=1)
    wts = wts / jnp.sum(wts, axis=-1, keepdims=True) * ROUTE_SCALE

    n_assign = t * TOP_K
    e_flat = idx.reshape(-1)
    tok_flat = jnp.repeat(jnp.arange(t, dtype=jnp.int32), TOP_K)
    w_flat = wts.reshape(-1)
    order = jnp.argsort(e_flat)
    e_s, tok_s, w_s = e_flat[order], tok_flat[order], w_flat[order]
    counts = jnp.bincount(e_flat, length=N_EXPERTS)
    padded = (counts + MOE_BLOCK - 1) // MOE_BLOCK * MOE_BLOCK
    pad_end = jnp.cumsum(padded)
    pad_start = pad_end - padded
    start = jnp.cumsum(counts) - counts
    dest = pad_start[e_s] + (jnp.arange(n_assign, dtype=jnp.int32) - start[e_s])
    n_slots = -(-(n_assign + N_EXPERTS * (MOE_BLOCK - 1)) // MOE_BLOCK) * MOE_BLOCK
    n_blocks = n_slots // MOE_BLOCK
    slot_tok = jnp.full((n_slots,), t, jnp.int32).at[dest].set(tok_s)
    slot_w = jnp.zeros((n_slots,), jnp.float32).at[dest].set(w_s)
    block_exp = jnp.minimum(
        jnp.searchsorted(pad_end, jnp.arange(n_blocks, dtype=jnp.int32) * MOE_BLOCK, side='right'),
        N_EXPERTS - 1)
    h_pad = jnp.concatenate([hf, jnp.zeros((1, d), hf.dtype)], axis=0)

    def block_step(acc, blk):
        toks, bw, e = blk
        xb = h_pad[toks]
        yb = (jax.nn.silu(xb @ w_gate[e]) * (xb @ w_up[e])) @ w_down[e]
        return acc.at[toks].add(yb * bw[:, None].astype(yb.dtype)), None

    acc0 = jnp.zeros((t + 1, d), hf.dtype)
    routed, _ = lax.scan(block_step, acc0, (slot_tok.reshape(n_blocks, MOE_BLOCK),
                                            slot_w.reshape(n_blocks, MOE_BLOCK), block_exp))
    shared = (jax.nn.silu(hf @ ws_gate) * (hf @ ws_up)) @ ws_down
    return (routed[:t] + shared).reshape(bsz, seq, d)


def setup_inputs(seed: int = 0) -> dict:
    key = jax.random.key(seed)
    ks = iter(jax.random.split(key, 40))
    f32 = jnp.float32

    def nrm(shape, scale):
        return scale * jax.random.normal(next(ks), shape, f32)

    L = DEPTH
    n_idx = jnp.arange(S5_STATE, dtype=f32)
    inp = {}
    inp['x'] = nrm((BATCH, SEQ, D_MODEL), 1.0)
    inp['c'] = nrm((BATCH, D_MODEL), 1.0)
    inp['ada_w'] = nrm((L, D_MODEL, 6 * D_MODEL), 0.5 * D_MODEL ** -0.5)
    inp['ada_b'] = nrm((L, 6 * D_MODEL), 0.02)
    inp['norm1_g'] = 1.0 + nrm((L, D_MODEL), 0.05)
    inp['w_in'] = nrm((L, D_MODEL, IN_WIDTH), D_MODEL ** -0.5)
    inp['s5_lam_re'] = -0.5 + nrm((L, S5_GROUPS, S5_STATE), 0.01)
    inp['s5_lam_im'] = jnp.pi * n_idx + nrm((L, S5_GROUPS, S5_STATE), 0.01)
    inp['s5_log_dt'] = jax.random.uniform(next(ks), (L, S5_GROUPS), f32,
                                          math.log(DT_MIN), math.log(DT_MAX))
    inp['s5_b_re'] = nrm((L, S5_GROUPS, S5_STATE, S5_GROUP), (2 * S5_GROUP) ** -0.5)
    inp['s5_b_im'] = nrm((L, S5_GROUPS, S5_STATE, S5_GROUP), (2 * S5_GROUP) ** -0.5)
    inp['s5_c_re'] = nrm((L, S5_GROUPS, S5_GROUP, S5_STATE), (2 * S5_STATE) ** -0.5)
    inp['s5_c_im'] = nrm((L, S5_GROUPS, S5_GROUP, S5_STATE), (2 * S5_STATE) ** -0.5)
    inp['s5_d'] = nrm((L, S5_GROUPS, S5_GROUP), 1.0)
    inp['s5_w_glu'] = nrm((L, S5_WIDTH, S5_WIDTH), S5_WIDTH ** -0.5)
    inp['s5_b_glu'] = nrm((L, S5_WIDTH), 0.02)
    inp['w_proj_a'] = nrm((L, S5_WIDTH, D_MODEL), S5_WIDTH ** -0.5)
    inp['gla_w_gk2'] = nrm((L, GLA_GATE_RANK, GLA_KEY), GLA_GATE_RANK ** -0.5)
    inp['gla_b_gk2'] = nrm((L, GLA_KEY), 0.1)
    inp['gla_norm_g'] = 1.0 + nrm((L, GLA_DV), 0.05)
    inp['w_proj_b'] = nrm((L, GLA_VAL, D_MODEL), GLA_VAL ** -0.5)
    inp['w_out'] = nrm((L, D_MODEL, D_MODEL), D_MODEL ** -0.5)
    inp['norm2_g'] = 1.0 + nrm((L, D_MODEL), 0.05)
    inp['router_w'] = nrm((L, D_MODEL, N_EXPERTS), D_MODEL ** -0.5)
    inp['router_bias'] = nrm((L, N_EXPERTS), 0.01)
    inp['exp_w_gate'] = nrm((L, N_EXPERTS, D_MODEL, EXPERT_FF), D_MODEL ** -0.5)
    inp['exp_w_up'] = nrm((L, N_EXPERTS, D_MODEL, EXPERT_FF), D_MODEL ** -0.5)
    inp['exp_w_down'] = nrm((L, N_EXPERTS, EXPERT_FF, D_MODEL), EXPERT_FF ** -0.5)
    inp['sh_w_gate'] = nrm((L, D_MODEL, SHARED_FF), D_MODEL ** -0.5)
    inp['sh_w_up'] = nrm((L, D_MODEL, SHARED_FF), D_MODEL ** -0.5)
    inp['sh_w_down'] = nrm((L, SHARED_FF, D_MODEL), SHARED_FF ** -0.5)
    inp['final_g'] = 1.0 + nrm((D_MODEL,), 0.05)
    return inp


def reference(x, c, ada_w, ada_b, norm1_g, w_in, s5_lam_re, s5_lam_im, s5_log_dt, s5_b_re, s5_b_im,
              s5_c_re, s5_c_im, s5_d, s5_w_glu, s5_b_glu, w_proj_a, gla_w_gk2, gla_b_gk2, gla_norm_g,
              w_proj_b, w_out, norm2_g, router_w, router_bias, exp_w_gate, exp_w_up, exp_w_down,
              sh_w_gate, sh_w_up, sh_w_down, final_g):
    for l in range(DEPTH):
        mod = jax.nn.silu(c) @ ada_w[l] + ada_b[l]
        sh1, sc1, g1, sh2, sc2, g2 = jnp.split(mod[:, None, :], 6, axis=-1)
        h = rmsnorm(x, norm1_g[l]) * (1.0 + sc1) + sh1
        u_s5, q, k, v, gk_low, r, gate_a, gate_b = jnp.split(h @ w_in[l], IN_OFFSETS, axis=-1)
        y_a = s5_branch(u_s5, s5_lam_re[l], s5_lam_im[l], s5_log_dt[l], s5_b_re[l], s5_b_im[l],
                        s5_c_re[l], s5_c_im[l], s5_d[l], s5_w_glu[l], s5_b_glu[l])
        y_b = gla_branch(q, k, v, gk_low, r, gla_w_gk2[l], gla_b_gk2[l], gla_norm_g[l])
        mixed = (jax.nn.sigmoid(gate_a) * (y_a @ w_proj_a[l])
                 + jax.nn.sigmoid(gate_b) * (y_b @ w_proj_b[l]))
        x = x + g1 * (mixed @ w_out[l])
        h = rmsnorm(x, norm2_g[l]) * (1.0 + sc2) + sh2
        x = x + g2 * moe_ffn(h, router_w[l], router_bias[l], exp_w_gate[l], exp_w_up[l],
                             exp_w_down[l], sh_w_gate[l], sh_w_up[l], sh_w_down[l])
    return rmsnorm(x, final_g)
```

```python
import numpy as np
import concourse.bass as bass
import concourse.mybir as mybir
from concourse.bass_utils import run_bass_kernel_spmd

F32 = mybir.dt.float32
BF16 = mybir.dt.bfloat16
I32 = mybir.dt.int32
AF = mybir.ActivationFunctionType
ALU = mybir.AluOpType
AX = mybir.AxisListType
DTSIZE = {F32: 4, BF16: 2, I32: 4}

COMPUTE = ("pe", "act", "dve", "pool")
NDMASEM = 24

D = 1024
L = 2048
NT = 16
EPS = 1e-6
S5W = 512
NG = 32
IN_W = 4112
OFF_U, OFF_Q, OFF_K, OFF_V, OFF_GK, OFF_R, OFF_GA, OFF_GB = 0, 512, 768, 1024, 1536, 1552, 2064, 3088
NEXP = 256
TWO_PI = 6.283185307179586


class Reg:
    __slots__ = ("name", "lw", "rd")

    def __init__(self, name, inherit=None):
        self.name = name
        self.lw = {}
        self.rd = dict(inherit) if inherit else {}


class Prog:
    def __init__(self, nc):
        self.nc = nc
        self.q = {e: [] for e in COMPUTE + ("sp",)}
        self.cnt = {e: 0 for e in COMPUTE}
        self.known = {e: {} for e in COMPUTE + ("sp",)}
        self.targets = {e: set() for e in COMPUTE}
        self.ndma = {e: 0 for e in ("sp", "act", "pool")}
        self.sems = {}
        self.dmasems = {}

    def _deps(self, eng, reads, writes, wacc=()):
        deps = {}
        for r in reads:
            for k, v in r.lw.items():
                if deps.get(k, 0) < v:
                    deps[k] = v
        for w in writes:
            for k, v in w.lw.items():
                if deps.get(k, 0) < v:
                    deps[k] = v
            for k, v in w.rd.items():
                if deps.get(k, 0) < v:
                    deps[k] = v
        for w in wacc:
            for k, v in w.rd.items():
                if deps.get(k, 0) < v:
                    deps[k] = v
        waits = []
        kn = self.known[eng]
        for k, v in deps.items():
            if k == "pe" and eng == "pe" and not getattr(self, "pe_selfsync", False):
                continue
            if kn.get(k, 0) >= v:
                continue
            kn[k] = v
            waits.append((k, v))
            if k in COMPUTE:
                self.targets[k].add(v)
        return waits

    def _mark(self, tok, reads, writes, wacc=()):
        k, v = tok
        for r in reads:
            if r.rd.get(k, 0) < v:
                r.rd[k] = v
        for w in writes:
            w.lw = {k: v}
            w.rd = {}
        for w in wacc:
            if w.lw.get(k, 0) < v:
                w.lw[k] = v

    def op(self, eng, fn, reads=(), writes=(), wacc=(), pmode="full"):
        waits = self._deps(eng, reads, writes, wacc)
        if eng == "pe":
            if pmode != getattr(self, "last_pmode", "full") and self.cnt["pe"] > 0:
                prev = self.cnt["pe"]
                if self.known["pe"].get("pe", 0) < prev:
                    self.known["pe"]["pe"] = prev
                    waits.append(("pe", prev))
                    self.targets["pe"].add(prev)
            self.last_pmode = pmode
        self.cnt[eng] += 1
        idx = self.cnt[eng]
        self.q[eng].append(("op", waits, fn, idx))
        self._mark((eng, idx), reads, writes, wacc)
        return idx

    def dma(self, eng, fn, reads=(), writes=(), wacc=()):
        waits = self._deps(eng, reads, writes, wacc)
        d = self.ndma[eng]
        self.ndma[eng] += 1
        si = d % NDMASEM
        key = ("dma", eng, si)
        val = 16 * (d // NDMASEM + 1)
        if d >= NDMASEM:
            pv = val - 16
            if self.known[eng].get(key, 0) < pv:
                self.known[eng][key] = pv
                waits.append((key, pv))
        self.q[eng].append(("dma", waits, fn, (key, val)))
        self._mark((key, val), reads, writes, wacc)
        return (key, val)

    def wait_all_dma(self, eng):
        waits = []
        for qe, n in self.ndma.items():
            for si in range(min(n, NDMASEM)):
                cntsi = (n - 1 - si) // NDMASEM + 1
                waits.append((("dma", qe, si), 16 * cntsi))
        self.q[eng].append(("wait", waits, None, None))

    def build(self):
        nc = self.nc
        for e in COMPUTE:
            self.sems[e] = nc.alloc_semaphore(name=f"sem_{e}")
        for qe, n in self.ndma.items():
            for si in range(min(n, NDMASEM)):
                self.dmasems[("dma", qe, si)] = nc.alloc_semaphore(name=f"dsem_{qe}_{si}")
        rank = {}
        for e in COMPUTE:
            rank[e] = {v: i + 1 for i, v in enumerate(sorted(self.targets[e]))}
        handles = {"pe": "tensor", "act": "scalar", "dve": "vector", "pool": "gpsimd", "sp": "sync"}

        def replay(e, h):
            for kind, waits, fn, tok in self.q[e]:
                for k, v in waits:
                    if k in COMPUTE:
                        h.wait_ge(self.sems[k], rank[k][v])
                    else:
                        h.wait_ge(self.dmasems[k], v)
                if kind == "op":
                    ins = fn(h)
                    if tok in rank[e]:
                        ins.then_inc(self.sems[e], 1)
                elif kind == "dma":
                    ins = fn(h)
                    ins.then_inc(self.dmasems[tok[0]], 16)

        with nc.Block() as block:
            for e in COMPUTE + ("sp",):
                if not self.q[e]:
                    continue

                def mk(e):
                    def f(h):
                        replay(e, h)
                    return f
                getattr(block, handles[e])(mk(e))


class Tile:
    def __init__(self, t, name, inherit):
        self.t = t
        self.name = name
        self.inherit = inherit
        self.regs = {}

    def r(self, key=None):
        if key not in self.regs:
            self.regs[key] = Reg(f"{self.name}:{key}", self.inherit)
        return self.regs[key]

    def __getitem__(self, idx):
        return self.t[idx]


class Arena:
    def __init__(self, nc, base=16512, top=229344):
        self.nc = nc
        self.ptr = base
        self.top = top
        self.hist = []
        self.n = 0

    def alloc(self, name, shape, dt, at=None):
        size = int(np.prod(shape[1:])) * DTSIZE[dt]
        size = (size + 31) // 32 * 32
        if at is None:
            start, end = self.ptr, self.ptr + size
            self.ptr = end
        else:
            start, end = at, at + size
        assert end <= self.top, f"SBUF overflow allocating {name}: {end} > {self.top}"
        self.n += 1
        t = self.nc.alloc_sbuf_tensor_at(f"{name}_{self.n}", list(shape), dt, offset=start)
        inherit = {}
        keep = []
        for (s, e, old) in self.hist:
            if s < end and start < e:
                for rg in old.regs.values():
                    for k, v in rg.lw.items():
                        inherit[k] = max(inherit.get(k, 0), v)
                    for k, v in rg.rd.items():
                        inherit[k] = max(inherit.get(k, 0), v)
                for k, v in old.inherit.items():
                    inherit[k] = max(inherit.get(k, 0), v)
            keep.append((s, e, old))
        T = Tile(t, name, inherit)
        self.hist = keep + [(start, end, T)]
        return T

    def mark(self):
        return self.ptr

    def release(self, m):
        self.ptr = m


def bcast_free(ap, n):
    return bass.AP(ap.tensor, ap.offset, [list(ap.ap[0]), [0, n]])


def build_program(stage=99, n_exp=NEXP + 1):
    nc = bass.Bass("TRN2", target_bir_lowering=False)
    P = Prog(nc)
    A = Arena(nc)

    def din(name, shape, dt=F32):
        return nc.dram_tensor(name, list(shape), dt, kind="ExternalInput").ap()

    x_d = din("x", [L, D])
    c_d = din("c", [D])
    adaw_d = din("ada_w", [D, 6 * D])
    adab_d = din("ada_b", [6 * D])
    n1g_d = din("norm1_g", [D])
    win_d = din("w_in", [D, IN_W])
    consts_d = din("consts", [128, 1024])
    lamre_d = din("s5_lam_re", [NG, 64])
    lamim_d = din("s5_lam_im", [NG, 64])
    logdt_d = din("s5_log_dt", [NG])
    bre_d = din("s5_b_re", [NG, 64, 16])
    bim_d = din("s5_b_im", [NG, 64, 16])
    cre_d = din("s5_c_re", [NG * 16, 64])
    cim_d = din("s5_c_im", [NG * 16, 64])
    s5d_d = din("s5_d", [S5W])
    wglu_d = din("s5_w_glu", [S5W, S5W])
    bglu_d = din("s5_b_glu", [S5W])
    wpa_d = din("w_proj_a", [S5W, D])
    wgk2_d = din("gla_w_gk2", [16, 256])
    bgk2_d = din("gla_b_gk2", [256])
    gng_d = din("gla_norm_g", [128])
    wpb_d = din("w_proj_b", [512, D])
    wout_d = din("w_out", [D, D])
    n2g_d = din("norm2_g", [D])
    rw_d = din("router_w", [D, NEXP])
    rb_d = din("router_bias", [NEXP])
    if stage >= 7:
        ewg_d = din("exp_w_gate", [n_exp * 128, 2048])
        ewu_d = din("exp_w_up", [n_exp * 128, 2048])
        ewd_d = din("exp_w_down", [n_exp * 128, 2048])
    fg_d = din("final_g", [D])
    out_d = nc.dram_tensor("out", [L, D], F32, kind="ExternalOutput").ap()
    dbg = {}

    def dbg_out(name, shape, dt=F32):
        dbg[name] = nc.dram_tensor(name, list(shape), dt, kind="ExternalOutput").ap()
        return dbg[name]

    banks = []
    for i in range(8):
        pt = nc.alloc_psum_tensor(f"bank{i}", [128, 512], F32)
        banks.append(Tile(pt, f"bank{i}", {}))
    bank_i = [0]

    def bank():
        b = banks[bank_i[0] % 8]
        bank_i[0] += 1
        return b

    cst = A.alloc("cst", [128, 1024], F32)
    P.dma("sp", lambda e: e.dma_start(out=cst[:], in_=consts_d), writes=[cst.r()])
    identf = cst[:, 0:128]
    identb_t = A.alloc("identb", [128, 128], BF16)
    P.op("dve", lambda e: e.tensor_copy(out=identb_t[:], in_=identf), reads=[cst.r()], writes=[identb_t.r()])

    cT = A.alloc("cT", [128, 8], F32)
    P.dma("sp", lambda e: e.dma_start(out=cT[:], in_=c_d.rearrange("(p k) -> p k", k=8)), writes=[cT.r()])
    scT = A.alloc("scT", [128, 8], F32)
    P.op("act", lambda e: e.activation(out=scT[:], in_=cT[:], func=AF.Silu), reads=[cT.r()], writes=[scT.r()])
    colp = A.alloc("colp", [128, 8, 8], F32)
    P.dma("sp", lambda e: e.dma_start(out=colp[:, 0, :], in_=n1g_d.rearrange("(k p) -> p k", p=128), allow_slow_non_contiguous=True), writes=[colp.r()])
    P.dma("sp", lambda e: e.dma_start(out=colp[:, 3, :], in_=n2g_d.rearrange("(k p) -> p k", p=128), allow_slow_non_contiguous=True), writes=[colp.r()])
    for j, off in ((1, 0), (2, D), (4, 3 * D), (5, 4 * D)):
        P.dma("sp", (lambda j, off: lambda e: e.dma_start(
            out=colp[:, j, :], in_=adab_d[off:off + D].rearrange("(k p) -> p k", p=128), allow_slow_non_contiguous=True))(j, off), writes=[colp.r()])
    modcol = A.alloc("modcol", [128, 4, 8], F32)
    g12 = A.alloc("g12", [128, 2, 1024], F32)
    m_ada = A.mark()
    rowb = A.alloc("rowb", [128, 2, 1024], F32)
    for j, off in ((0, 2 * D), (1, 5 * D)):
        src = adab_d[off:off + D]
        srcb = bass.AP(src.tensor, src.offset, [[0, 128], [1, D]])
        P.dma("sp", (lambda j, srcb: lambda e: e.dma_start(out=rowb[:, j, :], in_=srcb))(j, srcb), writes=[rowb.r(j)])
    adaw_v = adaw_d.rearrange("(p k) n -> p k n", k=8)
    colidx = {0: 0, 1: 1, 3: 2, 4: 3}
    rowidx = {2: 0, 5: 1}
    wblk = [A.alloc(f"adaw{i}", [128, 8, 512], F32) for i in range(2)]
    nb = 0
    for blk in range(6):
        for half in range(2):
            wb = wblk[nb % 2]
            nb += 1
            n0 = blk * 1024 + half * 512
            P.dma("sp", (lambda wb, n0: lambda e: e.dma_start(out=wb[:], in_=adaw_v[:, :, n0:n0 + 512]))(wb, n0),
                  writes=[wb.r()])
            if blk in rowidx:
                ps = bank()
                for k in range(8):
                    P.op("pe", (lambda ps, wb, k: lambda e: e.matmul(
                        ps[:], lhsT=bcast_free(scT[:, k:k + 1], 128), rhs=wb[:, k, :], start=(k == 0), stop=(k == 7)))(ps, wb, k),
                        reads=[scT.r(), wb.r()], writes=[ps.r()], pmode="f32")
                j = rowidx[blk]
                P.op("dve", (lambda ps, j, half: lambda e: e.tensor_tensor(
                    out=g12[:, j, half * 512:(half + 1) * 512], in0=ps[:], in1=rowb[:, j, half * 512:(half + 1) * 512], op=ALU.add))(ps, j, half),
                    reads=[ps.r(), rowb.r(j)], writes=[g12.r(j)])
            else:
                ps = bank()
                j = colidx[blk]
                for cc in range(4):
                    for k in range(8):
                        P.op("pe", (lambda ps, wb, cc, k: lambda e: e.matmul(
                            ps[:, cc:cc + 1], lhsT=wb[:, k, cc * 128:(cc + 1) * 128], rhs=scT[:, k:k + 1],
                            start=(k == 0), stop=(k == 7)))(ps, wb, cc, k),
                            reads=[scT.r(), wb.r()], writes=[ps.r()], pmode="f32")
                jb = {0: 1, 1: 2, 2: 4, 3: 5}[j]
                P.op("dve", (lambda ps, j, jb, half: lambda e: e.tensor_tensor(
                    out=modcol[:, j, half * 4:(half + 1) * 4], in0=ps[:, 0:4], in1=colp[:, jb, half * 4:(half + 1) * 4], op=ALU.add))(ps, j, jb, half),
                    reads=[ps.r(), colp.r()], writes=[modcol.r()])
    A.release(m_ada)
    AB = A.alloc("AB", [128, 2, 8], F32)
    for j, (jg, jsc) in enumerate(((0, 1), (3, 3))):
        P.op("dve", (lambda j, jg, jsc: lambda e: e.scalar_tensor_tensor(
            out=AB[:, j, :], in0=modcol[:, jsc, :], scalar=1.0, in1=colp[:, jg, :], op0=ALU.add, op1=ALU.mult))(j, jg, jsc),
            reads=[modcol.r(), colp.r()], writes=[AB.r()])

    if stage == 0:
        o1 = dbg_out("d_modcol", [128, 32]); o2 = dbg_out("d_g12", [128, 2048]); o3 = dbg_out("d_AB", [128, 16])
        P.dma("sp", lambda e: e.dma_start(out=o1, in_=modcol[:].rearrange("p a b -> p (a b)")), reads=[modcol.r()])
        P.dma("sp", lambda e: e.dma_start(out=o2, in_=g12[:].rearrange("p a b -> p (a b)")), reads=[g12.r(0), g12.r(1)])
        P.dma("sp", lambda e: e.dma_start(out=o3, in_=AB[:].rearrange("p a b -> p (a b)")), reads=[AB.r()])
        P.wait_all_dma("sp")
        P.build()
        return nc


    import math

    def fv(tile, col0, dims, p0=0, np_=128):
        a = tile[p0:p0 + np_, col0:col0 + 1]
        return bass.AP(a.tensor, a.offset, [list(a.ap[0])] + [[st, ct] for st, ct in dims])

    ev_i = [0]

    def evac_copy(out_ap, in_ap, reads, writes):
        ev_i[0] += 1
        if ev_i[0] % 2:
            P.op("dve", lambda e: e.tensor_copy(out=out_ap, in_=in_ap), reads=reads, writes=writes)
        else:
            P.op("act", lambda e: e.copy(out=out_ap, in_=in_ap), reads=reads, writes=writes)

    sq = A.alloc("sq", [128, D], F32)
    ssq = A.alloc("ssq", [128, 16], F32)
    rstd = A.alloc("rstd", [128, 16], F32)
    hT_off = A.mark()
    hT = A.alloc("hT", [128, 8, L], BF16)
    hT_regs = lambda n: [hT.r((k, n)) for k in range(8)]
    m_B = A.mark()
    xin = [A.alloc(f"xin{i}", [128, 4, D], F32) for i in range(2)]
    xnb = [A.alloc(f"xnb{i}", [128, 4, D], BF16) for i in range(2)]
    x_v = x_d.rearrange("(n p) d -> p n d", p=128)
    for blk in range(4):
        xi = xin[blk % 2]
        xb = xnb[blk % 2]
        P.dma("sp", lambda e, xi=xi, blk=blk: e.dma_start(out=xi[:], in_=x_v[:, blk * 4:(blk + 1) * 4, :]), writes=[xi.r()])
        for j in range(4):
            P.op("act", lambda e, xi=xi, j=j, blk=blk: e.activation(out=sq[:], in_=xi[:, j, :], func=AF.Square,
                                                                     accum_out=ssq[:, blk * 4 + j:blk * 4 + j + 1]),
                 reads=[xi.r()], writes=[sq.r(), ssq.r(blk)])
        rs = rstd[:, blk * 4:(blk + 1) * 4]
        P.op("dve", lambda e, rs=rs, blk=blk: e.tensor_scalar(out=rs, in0=ssq[:, blk * 4:(blk + 1) * 4], scalar1=1.0 / D, scalar2=EPS,
                                                               op0=ALU.mult, op1=ALU.add), reads=[ssq.r(blk)], writes=[rstd.r(blk)])
        P.op("act", lambda e, rs=rs: e.activation(out=rs, in_=rs, func=AF.Sqrt), reads=[rstd.r(blk)], writes=[rstd.r(blk)])
        P.op("dve", lambda e, rs=rs: e.reciprocal(out=rs, in_=rs), reads=[rstd.r(blk)], writes=[rstd.r(blk)])
        for j in range(4):
            P.op("act", lambda e, xi=xi, xb=xb, j=j, blk=blk: e.activation(out=xb[:, j, :], in_=xi[:, j, :], func=AF.Copy,
                                                                           scale=rstd[:, blk * 4 + j:blk * 4 + j + 1]),
                 reads=[xi.r(), rstd.r(blk)], writes=[xb.r(j)])
        for k in range(8):
            ps = bank()
            psb = ps.t[:].bitcast(BF16)
            for j in range(4):
                P.op("pe", lambda e, psb=psb, xb=xb, j=j, k=k: e.transpose(psb[:, j * 128:(j + 1) * 128], xb[:, j, k * 128:(k + 1) * 128], identb_t[:]),
                     reads=[xb.r(j), identb_t.r()], writes=[ps.r()])
            P.op("dve", lambda e, psb=psb, k=k, blk=blk: e.tensor_scalar(
                out=hT[:, k, blk * 512:(blk + 1) * 512], in0=psb[:, 0:512], scalar1=AB[:, 0, k:k + 1], scalar2=modcol[:, 0, k:k + 1],
                op0=ALU.mult, op1=ALU.add), reads=[ps.r(), AB.r(), modcol.r()], writes=[hT.r((k, blk))])
    A.release(m_B)

    if stage == 1:
        o1 = dbg_out("d_hT", [128, 8 * L], BF16)
        P.dma("sp", lambda e: e.dma_start(out=o1, in_=hT[:].rearrange("p a b -> p (a b)")), reads=[hT.r((k, n)) for k in range(8) for n in range(4)])
        P.wait_all_dma("sp")
        P.build()
        return nc

    m_C = A.mark()
    mixedT_off = A.mark()
    mixedT = A.alloc("mixedT", [128, 8, L], BF16)
    m_C2 = A.mark()
    win_v = win_d.rearrange("(k p) n -> p k n", p=128)
    wu = A.alloc("wu", [128, 8, S5W], BF16)
    P.dma("pool", lambda e: e.dma_start(out=wu[:], in_=win_v[:, :, OFF_U:OFF_U + S5W]), writes=[wu.r()])
    dcol = A.alloc("dcol", [128, 8], F32)
    P.dma("sp", lambda e: e.dma_start(out=dcol[:, 0:4], in_=s5d_d.rearrange("(k p) -> p k", p=128), allow_slow_non_contiguous=True), writes=[dcol.r()])
    P.dma("sp", lambda e: e.dma_start(out=dcol[:, 4:8], in_=bglu_d.rearrange("(k p) -> p k", p=128), allow_slow_non_contiguous=True), writes=[dcol.r()])

    uT = A.alloc("uT", [128, 4, L], BF16)
    for m in range(4):
        for n in range(4):
            ps = bank()
            for k in range(8):
                P.op("pe", lambda e, ps=ps, m=m, n=n, k=k: e.matmul(ps[:], lhsT=wu[:, k, m * 128:(m + 1) * 128], rhs=hT[:, k, n * 512:(n + 1) * 512],
                                                                   start=(k == 0), stop=(k == 7)),
                     reads=[wu.r(), hT.r((k, n))], writes=[ps.r()])
            evac_copy(uT[:, m, n * 512:(n + 1) * 512], ps[:], [ps.r()], [uT.r((m, n))])

    NE = 11
    NW = 32 * NE
    PR = A.alloc("PR", [128, NW], F32)
    PIs = A.alloc("PIs", [128, NW], F32)
    BT = A.alloc("BT", [128, 32, 128], BF16)
    Cpad = A.alloc("Cpad", [128, 32 * 128], F32)
    yT = A.alloc("yT", [128, 4, L], BF16)
    m_tmp = A.mark()
    lam_in = A.alloc("lam_in", [32, 2, 128], F32)
    for j, src in enumerate((lamre_d, lamim_d)):
        for hh in range(2):
            P.dma("sp", lambda e, j=j, hh=hh, src=src: e.dma_start(out=lam_in[:, j, hh * 64:(hh + 1) * 64], in_=src), writes=[lam_in.r()])
    LRLI = A.alloc("LRLI", [128, 64], F32)
    for j in range(2):
        ps = bank()
        P.op("pe", lambda e, ps=ps, j=j: e.transpose(ps[:, 0:32], lam_in[:, j, :], cst[0:32, 0:32]), reads=[lam_in.r(), cst.r()], writes=[ps.r()], pmode="k32")
        P.op("dve", lambda e, ps=ps, j=j: e.tensor_copy(out=LRLI[:, j * 32:(j + 1) * 32], in_=ps[:, 0:32]), reads=[ps.r()], writes=[LRLI.r()])
    DT = A.alloc("DT", [128, 32], F32)
    P.dma("sp", lambda e: e.dma_start(out=DT[:], in_=bass.AP(logdt_d.tensor, logdt_d.offset, [[0, 128], [1, 32]])), writes=[DT.r()])
    P.op("act", lambda e: e.activation(out=DT[:], in_=DT[:], func=AF.Exp), reads=[DT.r()], writes=[DT.r()])
    LDT = A.alloc("LDT", [128, 64], F32)
    for j in range(2):
        P.op("dve", lambda e, j=j: e.tensor_tensor(out=LDT[:, j * 32:(j + 1) * 32], in0=LRLI[:, j * 32:(j + 1) * 32], in1=DT[:], op=ALU.mult),
             reads=[LRLI.r(), DT.r()], writes=[LDT.r()])
    E_b = fv(cst, 272, [(0, 32), (1, NE)])
    ARG = A.alloc("ARG", [128, NW], F32)
    ANG = A.alloc("ANG", [128, NW], F32)
    P.op("dve", lambda e: e.tensor_tensor(out=fv(ARG, 0, [(NE, 32), (1, NE)]), in0=fv(LDT, 0, [(1, 32), (0, NE)]), in1=E_b, op=ALU.mult),
         reads=[LDT.r(), cst.r()], writes=[ARG.r()])
    P.op("dve", lambda e: e.tensor_tensor(out=fv(ANG, 0, [(NE, 32), (1, NE)]), in0=fv(LDT, 32, [(1, 32), (0, NE)]), in1=E_b, op=ALU.mult),
         reads=[LDT.r(), cst.r()], writes=[ANG.r()])
    MAG = A.alloc("MAG", [128, NW], F32)
    P.op("act", lambda e: e.activation(out=MAG[:], in_=ARG[:], func=AF.Exp), reads=[ARG.r()], writes=[MAG.r()])
    tmpf = A.alloc("tmpf", [128, NW], F32)
    tmpm = A.alloc("tmpm", [128, NW], F32)
    tmpi = A.alloc("tmpi", [128, NW], I32)
    SIN = A.alloc("SIN", [128, NW], F32)
    COS = A.alloc("COS", [128, NW], F32)

    def sin_of(shift, out_t):
        rt = [tmpf.r()]
        P.op("dve", lambda e: e.tensor_scalar(out=tmpf[:], in0=ANG[:], scalar1=shift, scalar2=1.0 / TWO_PI, op0=ALU.add, op1=ALU.mult),
             reads=[ANG.r()], writes=rt)
        P.op("dve", lambda e: e.tensor_copy(out=tmpi[:], in_=tmpf[:]), reads=rt, writes=[tmpi.r()])
        P.op("dve", lambda e: e.tensor_copy(out=tmpf[:], in_=tmpi[:]), reads=[tmpi.r()], writes=rt)
        P.op("dve", lambda e: e.scalar_tensor_tensor(out=tmpf[:], in0=tmpf[:], scalar=-TWO_PI, in1=ANG[:], op0=ALU.mult, op1=ALU.add),
             reads=rt + [ANG.r()], writes=rt)
        if shift != 0.0:
            P.op("dve", lambda e: e.tensor_scalar(out=tmpf[:], in0=tmpf[:], scalar1=shift, scalar2=None, op0=ALU.add), reads=rt, writes=rt)
        P.op("dve", lambda e: e.tensor_scalar(out=tmpm[:], in0=tmpf[:], scalar1=math.pi, scalar2=-TWO_PI, op0=ALU.is_gt, op1=ALU.mult),
             reads=rt, writes=[tmpm.r()])
        P.op("dve", lambda e: e.tensor_tensor(out=tmpf[:], in0=tmpf[:], in1=tmpm[:], op=ALU.add), reads=rt + [tmpm.r()], writes=rt)
        P.op("dve", lambda e: e.tensor_scalar(out=tmpm[:], in0=tmpf[:], scalar1=-math.pi, scalar2=TWO_PI, op0=ALU.is_lt, op1=ALU.mult),
             reads=rt, writes=[tmpm.r()])
        P.op("dve", lambda e: e.tensor_tensor(out=tmpf[:], in0=tmpf[:], in1=tmpm[:], op=ALU.add), reads=rt + [tmpm.r()], writes=rt)
        P.op("dve", lambda e: e.tensor_scalar(out=tmpf[:], in0=tmpf[:], scalar1=3.14159, scalar2=-3.14159, op0=ALU.min, op1=ALU.max), reads=rt, writes=rt)
        P.op("act", lambda e: e.activation(out=out_t[:], in_=tmpf[:], func=AF.Sin), reads=rt, writes=[out_t.r()])

    sin_of(0.0, SIN)
    sin_of(math.pi / 2, COS)
    PIu = A.alloc("PIu", [128, NW], F32)
    P.op("dve", lambda e: e.tensor_tensor(out=PR[:], in0=MAG[:], in1=COS[:], op=ALU.mult), reads=[MAG.r(), COS.r()], writes=[PR.r()])
    P.op("dve", lambda e: e.tensor_tensor(out=PIu[:], in0=MAG[:], in1=SIN[:], op=ALU.mult), reads=[MAG.r(), SIN.r()], writes=[PIu.r()])
    P.op("dve", lambda e: e.tensor_scalar(out=PIs[:], in0=PIu[:], scalar1=cst[:, 256:257], scalar2=None, op0=ALU.mult), reads=[PIu.r(), cst.r()], writes=[PIs.r()])
    cf = A.alloc("cf", [128, 256], F32)
    ar = fv(PR, 0, [(NE, 32)])
    ai = fv(PIu, 0, [(NE, 32)])
    lr_ = LRLI[:, 0:32]
    li_ = LRLI[:, 32:64]
    cfr = [cf.r()]
    P.op("dve", lambda e: e.tensor_scalar(out=cf[:, 0:32], in0=ar, scalar1=-1.0, scalar2=None, op0=ALU.add), reads=[PR.r()], writes=cfr)
    P.op("dve", lambda e: e.tensor_tensor(out=cf[:, 32:64], in0=lr_, in1=lr_, op=ALU.mult), reads=[LRLI.r()], writes=cfr)
    P.op("dve", lambda e: e.tensor_tensor(out=cf[:, 64:96], in0=li_, in1=li_, op=ALU.mult), reads=[LRLI.r()], writes=cfr)
    P.op("dve", lambda e: e.tensor_tensor(out=cf[:, 32:64], in0=cf[:, 32:64], in1=cf[:, 64:96], op=ALU.add), reads=cfr, writes=cfr)
    P.op("dve", lambda e: e.reciprocal(out=cf[:, 32:64], in_=cf[:, 32:64]), reads=cfr, writes=cfr)
    P.op("dve", lambda e: e.tensor_tensor(out=cf[:, 64:96], in0=cf[:, 0:32], in1=lr_, op=ALU.mult), reads=cfr + [LRLI.r()], writes=cfr)
    P.op("dve", lambda e: e.tensor_tensor(out=cf[:, 96:128], in0=ai, in1=li_, op=ALU.mult), reads=[PIu.r(), LRLI.r()], writes=cfr)
    P.op("dve", lambda e: e.tensor_tensor(out=cf[:, 64:96], in0=cf[:, 64:96], in1=cf[:, 96:128], op=ALU.add), reads=cfr, writes=cfr)
    P.op("dve", lambda e: e.tensor_tensor(out=cf[:, 128:160], in0=cf[:, 64:96], in1=cf[:, 32:64], op=ALU.mult), reads=cfr, writes=cfr)
    P.op("dve", lambda e: e.tensor_tensor(out=cf[:, 64:96], in0=ai, in1=lr_, op=ALU.mult), reads=[PIu.r(), LRLI.r()], writes=cfr)
    P.op("dve", lambda e: e.tensor_tensor(out=cf[:, 96:128], in0=cf[:, 0:32], in1=li_, op=ALU.mult), reads=cfr + [LRLI.r()], writes=cfr)
    P.op("dve", lambda e: e.tensor_tensor(out=cf[:, 64:96], in0=cf[:, 64:96], in1=cf[:, 96:128], op=ALU.subtract), reads=cfr, writes=cfr)
    P.op("dve", lambda e: e.tensor_tensor(out=cf[:, 160:192], in0=cf[:, 64:96], in1=cf[:, 32:64], op=ALU.mult), reads=cfr, writes=cfr)
    P.op("dve", lambda e: e.tensor_scalar(out=cf[:, 160:192], in0=cf[:, 160:192], scalar1=cst[:, 257:258], scalar2=None, op0=ALU.mult), reads=cfr + [cst.r()], writes=cfr)
    RB = A.alloc("RB", [128, 2, 32, 16], F32)
    bre_v = bre_d.rearrange("g p h -> p g h")
    bim_v = bim_d.rearrange("g p h -> p g h")
    for (p0, var, src) in ((0, 0, bre_v), (64, 0, bim_v), (0, 1, bim_v), (64, 1, bre_v)):
        P.dma("sp", lambda e, p0=p0, var=var, src=src: e.dma_start(out=RB[p0:p0 + 64, var, :, :], in_=src), writes=[RB.r()])
    B1 = A.alloc("B1", [128, 512], F32)
    B1v = fv(B1, 0, [(16, 32), (1, 16)])
    P.op("dve", lambda e: e.tensor_tensor(out=B1v, in0=RB[:, 0, :, :], in1=fv(cf, 4 * 32, [(1, 32), (0, 16)]), op=ALU.mult), reads=[RB.r()] + cfr, writes=[B1.r()])
    P.op("dve", lambda e: e.tensor_tensor(out=RB[:, 1, :, :], in0=RB[:, 1, :, :], in1=fv(cf, 5 * 32, [(1, 32), (0, 16)]), op=ALU.mult), reads=[RB.r()] + cfr, writes=[RB.r()])
    P.op("dve", lambda e: e.tensor_tensor(out=B1v, in0=B1v, in1=RB[:, 1, :, :], op=ALU.add), reads=[RB.r(), B1.r()], writes=[B1.r()])
    for ch in range(4):
        ps = bank()
        P.op("pe", lambda e, ps=ps, ch=ch: e.transpose(ps[:, 0:128], B1[:, ch * 128:(ch + 1) * 128], cst[:, 0:128]), reads=[B1.r(), cst.r()], writes=[ps.r()], pmode="f32")
        for gl in range(8):
            P.op("dve", lambda e, ps=ps, ch=ch, gl=gl: e.tensor_scalar(out=BT[:, ch * 8 + gl, :], in0=ps[:, 0:128], scalar1=cst[:, 264 + gl:265 + gl], scalar2=None, op0=ALU.mult),
                 reads=[ps.r(), cst.r()], writes=[BT.r()])
    Cin = A.alloc("Cin", [128, 4, 128], F32)
    P.dma("sp", lambda e: e.dma_start(out=Cin[:, :, 0:64], in_=cre_d.rearrange("(k r) p -> r k p", r=128)), writes=[Cin.r()])
    P.dma("sp", lambda e: e.dma_start(out=Cin[:, :, 64:128], in_=cim_d.rearrange("(k r) p -> r k p", r=128)), writes=[Cin.r()])
    CT1 = A.alloc("CT1", [128, 512], F32)
    for r4 in range(4):
        ps = bank()
        P.op("pe", lambda e, ps=ps, r4=r4: e.transpose(ps[:, 0:128], Cin[:, r4, :], cst[:, 0:128]), reads=[Cin.r(), cst.r()], writes=[ps.r()], pmode="f32")
        P.op("dve", lambda e, ps=ps, r4=r4: e.tensor_scalar(out=CT1[:, r4 * 128:(r4 + 1) * 128], in0=ps[:, 0:128], scalar1=cst[:, 256:257], scalar2=None, op0=ALU.mult),
             reads=[ps.r(), cst.r()], writes=[CT1.r()])
    P.op("dve", lambda e: e.tensor_tensor(out=fv(Cpad, 0, [(128, 32), (16, 8), (1, 16)]), in0=fv(CT1, 0, [(16, 32), (0, 8), (1, 16)]),
                                          in1=fv(cst, 288, [(8, 32), (1, 8), (0, 16)]), op=ALU.mult), reads=[CT1.r(), cst.r()], writes=[Cpad.r()])

    A.release(m_tmp)
    m_scan = A.mark()
    F32R = mybir.dt.float32r
    r32 = lambda ap: ap.bitcast(F32R)
    PAD = 1024
    identR = A.alloc("identR", [128, 128], F32)
    P.op("dve", lambda e: e.tensor_copy(out=r32(identR[:]), in_=cst[:, 0:128]), reads=[cst.r()], writes=[identR.r()])
    SAB = [A.alloc(f"S{i}", [128, PAD + L], F32) for i in range(2)]
    for t_ in SAB:
        P.op("dve", lambda e, t_=t_: e.tensor_scalar(out=r32(t_[:, 0:PAD]), in0=cst[:, 0:PAD], scalar1=0.0, scalar2=None, op0=ALU.mult), reads=[cst.r()], writes=[t_.r("pad")])
    Rt = [A.alloc(f"R{i}", [128, NE, 128], F32) for i in range(2)]
    ytmp = [A.alloc(f"ytmp{i}", [128, 512], F32) for i in range(2)]
    ybanks = banks[4:8]
    rot_i = [0]

    def rbank():
        b = banks[rot_i[0] % 4]
        rot_i[0] += 1
        return b

    def sregs(t_, lo):
        rr = []
        if lo < 0:
            rr.append(t_.r("pad"))
        for n in range(4):
            if lo < (n + 1) * 512 and n * 512 < lo + 512:
                rr.append(t_.r(n))
        return rr

    for g in range(NG):
        chunk, gl = divmod(g, 8)
        R = Rt[g % 2]
        for k in range(NE):
            P.op("act", lambda e, R=R, k=k, g=g: e.activation(out=r32(R[:, k, :]), in_=cst[:, 0:128], func=AF.Copy, scale=PR[:, g * NE + k:g * NE + k + 1]),
                 reads=[cst.r(), PR.r()], writes=[R.r(k)])
            P.op("dve", lambda e, R=R, k=k, g=g: e.scalar_tensor_tensor(out=r32(R[:, k, :]), in0=cst[:, 128:256], scalar=PIs[:, g * NE + k:g * NE + k + 1], in1=R[:, k, :],
                                                                       op0=ALU.mult, op1=ALU.add), reads=[cst.r(), PIs.r(), R.r(k)], writes=[R.r(k)])
        cur, nxt = SAB
        for n in range(4):
            ps = rbank()
            P.op("pe", lambda e, ps=ps, g=g, chunk=chunk, n=n: e.matmul(ps[:], lhsT=BT[:, g, :], rhs=uT[:, chunk, n * 512:(n + 1) * 512], start=True, stop=True),
                 reads=[BT.r(), uT.r((chunk, n))], writes=[ps.r()])
            evac_copy(r32(cur[:, PAD + n * 512:PAD + (n + 1) * 512]), ps[:], [ps.r()], [cur.r(n)])
        for k in range(NE):
            sh = 1 << k
            for n in range(4):
                lo = n * 512 - sh
                dst = nxt[:, PAD + n * 512:PAD + (n + 1) * 512]
                srcn = cur[:, PAD + n * 512:PAD + (n + 1) * 512]
                if lo + 512 <= 0:
                    P.op("pool", lambda e, dst=dst, srcn=srcn: e.tensor_copy(out=r32(dst), in_=srcn), reads=[cur.r(n)], writes=[nxt.r(n)])
                    continue
                ps = rbank()
                P.op("pe", lambda e, ps=ps, srcn=srcn: e.matmul(ps[:], lhsT=r32(identR[:]), rhs=r32(srcn), start=True, stop=False),
                     reads=[identR.r(), cur.r(n)], writes=[ps.r()], pmode="f32")
                P.op("pe", lambda e, ps=ps, R=R, k=k, cur=cur, lo=lo: e.matmul(ps[:], lhsT=r32(R[:, k, :]), rhs=r32(cur[:, PAD + lo:PAD + lo + 512]), start=False, stop=True),
                     reads=[R.r(k)] + sregs(cur, lo), writes=[ps.r()], pmode="f32")
                evac_copy(r32(dst), ps[:], [ps.r()], [nxt.r(n)])
            cur, nxt = nxt, cur
        for n in range(4):
            yb = ybanks[n]
            P.op("pe", lambda e, yb=yb, g=g, cur=cur, n=n, gl=gl: e.matmul(yb[:], lhsT=Cpad[:, g * 128:(g + 1) * 128], rhs=cur[:, PAD + n * 512:PAD + (n + 1) * 512],
                                                                          start=(gl == 0), stop=(gl == 7)),
                 reads=[Cpad.r(), cur.r(n)], writes=[yb.r()], pmode="f32")
        if gl == 7:
            for n in range(4):
                yb = ybanks[n]
                yt = ytmp[n % 2]
                P.op("dve", lambda e, yb=yb, yt=yt, chunk=chunk, n=n: e.scalar_tensor_tensor(out=yt[:], in0=uT[:, chunk, n * 512:(n + 1) * 512], scalar=dcol[:, chunk:chunk + 1],
                                                                                           in1=yb[:], op0=ALU.mult, op1=ALU.add),
                     reads=[uT.r((chunk, n)), dcol.r(), yb.r()], writes=[yt.r()])
                P.op("act", lambda e, yt=yt, chunk=chunk, n=n: e.activation(out=yT[:, chunk, n * 512:(n + 1) * 512], in_=yt[:], func=AF.Gelu_apprx_tanh),
                     reads=[yt.r()], writes=[yT.r((chunk, n))])

    if stage == 2:
        o1 = dbg_out("d_yT", [128, 4 * L], BF16)
        o2 = dbg_out("d_uT", [128, 4 * L], BF16)
        P.dma("sp", lambda e: e.dma_start(out=o1, in_=yT[:].rearrange("p a b -> p (a b)")), reads=[yT.r((k, n)) for k in range(4) for n in range(4)])
        P.dma("sp", lambda e: e.dma_start(out=o2, in_=uT[:].rearrange("p a b -> p (a b)")), reads=[uT.r((k, n)) for k in range(4) for n in range(4)])
        P.wait_all_dma("sp")
        P.build()
        return nc


    A.release(m_scan)
    wglu = A.alloc("wglu", [128, 4, S5W], BF16)
    P.dma("pool", lambda e: e.dma_start(out=wglu[:], in_=wglu_d.rearrange("(k p) n -> p k n", p=128)), writes=[wglu.r()])
    wpa = A.alloc("wpa", [128, 4, D], BF16)
    P.dma("pool", lambda e: e.dma_start(out=wpa[:], in_=wpa_d.rearrange("(k p) n -> p k n", p=128)), writes=[wpa.r()])
    wga = A.alloc("wga", [128, 8, D], BF16)
    P.dma("pool", lambda e: e.dma_start(out=wga[:], in_=win_v[:, :, OFF_GA:OFF_GA + D]), writes=[wga.r()])
    yaT = A.alloc("yaT", [128, 4, L], BF16)
    sgb = [A.alloc(f"sgb{i}", [128, 512], BF16) for i in range(2)]
    sgf = [A.alloc(f"sgf{i}", [128, 512], F32) for i in range(2)]
    it = 0
    for m in range(4):
        for n in range(4):
            ps = bank()
            for k in range(4):
                P.op("pe", lambda e, ps=ps, m=m, n=n, k=k: e.matmul(ps[:], lhsT=wglu[:, k, m * 128:(m + 1) * 128], rhs=yT[:, k, n * 512:(n + 1) * 512],
                                                                   start=(k == 0), stop=(k == 3)), reads=[wglu.r(), yT.r((k, n))], writes=[ps.r()])
            sg = sgb[it % 2]
            it += 1
            P.op("act", lambda e, ps=ps, sg=sg, m=m: e.activation(out=sg[:], in_=ps[:], func=AF.Sigmoid, bias=dcol[:, 4 + m:5 + m]),
                 reads=[ps.r(), dcol.r()], writes=[sg.r()])
            P.op("dve", lambda e, sg=sg, m=m, n=n: e.tensor_tensor(out=yaT[:, m, n * 512:(n + 1) * 512], in0=yT[:, m, n * 512:(n + 1) * 512], in1=sg[:], op=ALU.mult),
                 reads=[sg.r(), yT.r((m, n))], writes=[yaT.r((m, n))])

    def proj_gate(wp, wg, srcT, first):
        it = 0
        for m in range(8):
            for n in range(4):
                ps1 = bank()
                for k in range(4):
                    P.op("pe", lambda e, ps1=ps1, m=m, n=n, k=k: e.matmul(ps1[:], lhsT=wp[:, k, m * 128:(m + 1) * 128], rhs=srcT[:, k, n * 512:(n + 1) * 512],
                                                                         start=(k == 0), stop=(k == 3)), reads=[wp.r(), srcT.r((k, n))], writes=[ps1.r()])
                ps2 = bank()
                for k in range(8):
                    P.op("pe", lambda e, ps2=ps2, m=m, n=n, k=k: e.matmul(ps2[:], lhsT=wg[:, k, m * 128:(m + 1) * 128], rhs=hT[:, k, n * 512:(n + 1) * 512],
                                                                         start=(k == 0), stop=(k == 7)), reads=[wg.r(), hT.r((k, n))], writes=[ps2.r()])
                sg = sgf[it % 2]
                it += 1
                P.op("act", lambda e, ps2=ps2, sg=sg: e.activation(out=sg[:], in_=ps2[:], func=AF.Sigmoid), reads=[ps2.r()], writes=[sg.r()])
                dst = mixedT[:, m, n * 512:(n + 1) * 512]
                if first:
                    P.op("dve", lambda e, ps1=ps1, sg=sg, dst=dst: e.tensor_tensor(out=dst, in0=ps1[:], in1=sg[:], op=ALU.mult),
                         reads=[ps1.r(), sg.r()], writes=[mixedT.r((m, n))])
                else:
                    P.op("dve", lambda e, ps1=ps1, sg=sg: e.tensor_tensor(out=sg[:], in0=ps1[:], in1=sg[:], op=ALU.mult),
                         reads=[ps1.r(), sg.r()], writes=[sg.r()])
                    P.op("pool", lambda e, sg=sg, dst=dst: e.tensor_tensor(out=dst, in0=dst, in1=sg[:], op=ALU.add),
                         reads=[sg.r(), mixedT.r((m, n))], writes=[mixedT.r((m, n))])

    proj_gate(wpa, wga, yaT, True)
    A.release(m_C2)

    P.pe_selfsync = True
    qtT = A.alloc("qtT", [128, 2, L], BF16)
    ktT = A.alloc("ktT", [128, 2, L], BF16)
    vtok = A.alloc("vtok", [128, NT, 512], BF16)
    rstok = A.alloc("rstok", [128, NT, 512], BF16)
    ktok = A.alloc("ktok", [128, NT, 256], BF16)
    eblast = A.alloc("eblast", [128, 2, NT], F32)
    gsm = A.alloc("gsm", [128, 512], F32)
    m_G = A.mark()
    wgla = A.alloc("wgla", [128, 8, 1552], BF16)
    P.dma("pool", lambda e: e.dma_start(out=wgla[:], in_=win_v[:, :, OFF_Q:OFF_Q + 1552]), writes=[wgla.r()])
    WQ, WK, WV, WGK, WR = 0, 256, 512, 1024, 1040
    wgk2 = A.alloc("wgk2", [16, 256], BF16)
    P.dma("pool", lambda e: e.dma_start(out=wgk2[:], in_=wgk2_d), writes=[wgk2.r()])
    P.dma("sp", lambda e: e.dma_start(out=gsm[:, 0:2], in_=bgk2_d.rearrange("(k p) -> p k", p=128), allow_slow_non_contiguous=True), writes=[gsm.r()])
    P.dma("sp", lambda e: e.dma_start(out=gsm[:, 128:256], in_=bass.AP(gng_d.tensor, gng_d.offset, [[0, 128], [1, 128]])), writes=[gsm.r()])
    P.op("dve", lambda e: e.tensor_scalar(out=gsm[:, 0:2], in0=gsm[:, 0:2], scalar1=-1.0, scalar2=None, op0=ALU.mult), reads=[gsm.r()], writes=[gsm.r()])
    P.op("pool", lambda e: e.memset(gsm[:, 256:384], 1.0), writes=[gsm.r()])
    gkT = A.alloc("gkT", [16, L], BF16)
    for n in range(4):
        ps = bank()
        for k in range(8):
            P.op("pe", lambda e, ps=ps, n=n, k=k: e.matmul(ps[0:16, :], lhsT=wgla[:, k, WGK:WGK + 16], rhs=hT[:, k, n * 512:(n + 1) * 512], start=(k == 0), stop=(k == 7)),
                 reads=[wgla.r(), hT.r((k, n))], writes=[ps.r()], pmode="m16")
        evac_copy(gkT[:, n * 512:(n + 1) * 512], ps[0:16, :], [ps.r()], [gkT.r(n)])
    cum = [A.alloc(f"cum{i}", [128, 512], F32) for i in range(2)]
    ebt = [A.alloc(f"ebt{i}", [128, 512], F32) for i in range(2)]
    enbt = [A.alloc(f"enbt{i}", [128, 512], F32) for i in range(2)]
    it = 0
    for n in range(4):
        for t2 in range(2):
            cu, eb_, enb_ = cum[it % 2], ebt[it % 2], enbt[it % 2]
            it += 1
            ps = bank()
            P.op("pe", lambda e, ps=ps, n=n, t2=t2: e.matmul(ps[:], lhsT=wgk2[:, t2 * 128:(t2 + 1) * 128], rhs=gkT[:, n * 512:(n + 1) * 512], start=True, stop=True),
                 reads=[wgk2.r(), gkT.r(n)], writes=[ps.r()], pmode="k16")
            P.op("act", lambda e, ps=ps, cu=cu, t2=t2: e.activation(out=cu[:], in_=ps[:], func=AF.Exp, scale=-1.0, bias=gsm[:, t2:t2 + 1]), reads=[ps.r(), gsm.r()], writes=[cu.r()])
            P.op("act", lambda e, cu=cu: e.activation(out=cu[:], in_=cu[:], func=AF.Ln, bias=1.0), reads=[cu.r()], writes=[cu.r()])
            for c4 in range(4):
                P.op("dve", lambda e, cu=cu, c4=c4: e.tensor_tensor_scan(out=cu[:, c4 * 128:(c4 + 1) * 128], data0=gsm[:, 256:384], data1=cu[:, c4 * 128:(c4 + 1) * 128],
                                                                        initial=0.0, op0=ALU.mult, op1=ALU.add), reads=[cu.r(), gsm.r()], writes=[cu.r()])
            P.op("act", lambda e, cu=cu, eb_=eb_: e.activation(out=eb_[:], in_=cu[:], func=AF.Exp, scale=-1.0 / 16.0), reads=[cu.r()], writes=[eb_.r()])
            P.op("act", lambda e, cu=cu, enb_=enb_: e.activation(out=enb_[:], in_=cu[:], func=AF.Exp, scale=1.0 / 16.0), reads=[cu.r()], writes=[enb_.r()])
            P.op("pool", lambda e, eb_=eb_, n=n, t2=t2: e.tensor_copy(out=eblast[:, t2, n * 4:(n + 1) * 4], in_=fv(eb_, 127, [(128, 4)])), reads=[eb_.r()], writes=[eblast.r()])
            psq = bank()
            for k in range(8):
                P.op("pe", lambda e, psq=psq, n=n, t2=t2, k=k: e.matmul(psq[:], lhsT=wgla[:, k, WQ + t2 * 128:WQ + (t2 + 1) * 128], rhs=hT[:, k, n * 512:(n + 1) * 512],
                                                                       start=(k == 0), stop=(k == 7)), reads=[wgla.r(), hT.r((k, n))], writes=[psq.r()])
            P.op("dve", lambda e, psq=psq, eb_=eb_, n=n, t2=t2: e.scalar_tensor_tensor(out=qtT[:, t2, n * 512:(n + 1) * 512], in0=psq[:], scalar=0.125, in1=eb_[:],
                                                                                      op0=ALU.mult, op1=ALU.mult), reads=[psq.r(), eb_.r()], writes=[qtT.r((t2, n))])
            psk = bank()
            for k in range(8):
                P.op("pe", lambda e, psk=psk, n=n, t2=t2, k=k: e.matmul(psk[:], lhsT=wgla[:, k, WK + t2 * 128:WK + (t2 + 1) * 128], rhs=hT[:, k, n * 512:(n + 1) * 512],
                                                                       start=(k == 0), stop=(k == 7)), reads=[wgla.r(), hT.r((k, n))], writes=[psk.r()])
            P.op("dve", lambda e, psk=psk, enb_=enb_, n=n, t2=t2: e.tensor_tensor(out=ktT[:, t2, n * 512:(n + 1) * 512], in0=psk[:], in1=enb_[:], op=ALU.mult),
                 reads=[psk.r(), enb_.r()], writes=[ktT.r((t2, n))])
    for c in range(NT):
        n = c // 4
        psv = bank()
        for k in range(8):
            P.op("pe", lambda e, psv=psv, c=c, k=k: e.matmul(psv[:], lhsT=hT[:, k, c * 128:(c + 1) * 128], rhs=wgla[:, k, WV:WV + 512], start=(k == 0), stop=(k == 7)),
                 reads=[wgla.r(), hT.r((k, n))], writes=[psv.r()])
        evac_copy(vtok[:, c, :], psv[:], [psv.r()], [vtok.r(c)])
        psr = bank()
        for k in range(8):
            P.op("pe", lambda e, psr=psr, c=c, k=k: e.matmul(psr[:], lhsT=hT[:, k, c * 128:(c + 1) * 128], rhs=wgla[:, k, WR:WR + 512], start=(k == 0), stop=(k == 7)),
                 reads=[wgla.r(), hT.r((k, n))], writes=[psr.r()])
        P.op("act", lambda e, psr=psr, c=c: e.activation(out=rstok[:, c, :], in_=psr[:], func=AF.Silu), reads=[psr.r()], writes=[rstok.r(c)])
        pst = bank()
        pstb = pst.t[:].bitcast(BF16)
        for t2 in range(2):
            P.op("pe", lambda e, pstb=pstb, c=c, t2=t2: e.transpose(pstb[:, t2 * 128:(t2 + 1) * 128], ktT[:, t2, c * 128:(c + 1) * 128], identb_t[:]),
                 reads=[ktT.r((t2, n)), identb_t.r()], writes=[pst.r()])
        evac_copy(ktok[:, c, :], pstb[:, 0:256], [pst.r()], [ktok.r(c)])
    A.release(m_G)
    wpb = A.alloc("wpb", [128, 4, D], BF16)
    P.dma("pool", lambda e: e.dma_start(out=wpb[:], in_=wpb_d.rearrange("(k p) n -> p k n", p=128)), writes=[wpb.r()])
    wgb = A.alloc("wgb", [128, 8, D], BF16)
    P.dma("pool", lambda e: e.dma_start(out=wgb[:], in_=win_v[:, :, OFF_GB:OFF_GB + D]), writes=[wgb.r()])
    ybT = A.alloc("ybT", [128, 4, L], BF16)
    S32 = A.alloc("S32", [128, 2, 128], F32)
    Sbf = A.alloc("Sbf", [128, 2, 128], BF16)
    PT = [A.alloc(f"PT{i}", [128, 4, 128], BF16) for i in range(2)]
    otok = [A.alloc(f"otok{i}", [128, 512], F32) for i in range(2)]
    onrm = [A.alloc(f"onrm{i}", [128, 512], F32) for i in range(2)]
    ybtok = [A.alloc(f"ybtok{i}", [128, 512], BF16) for i in range(2)]
    oss = [A.alloc(f"oss{i}", [128, 8], F32) for i in range(2)]
    osq = A.alloc("osq", [128, 128], F32)
    P.op("pool", lambda e: e.memset(S32[:], 0.0), writes=[S32.r()])
    maskb = fv(cst, 544, [(0, 4), (1, 128)])
    for c in range(NT):
        n = c // 4
        cs = slice(c * 128, (c + 1) * 128)
        pss = bank()
        for hd in range(4):
            t2, po = hd // 2, (hd % 2) * 64
            P.op("pe", lambda e, pss=pss, hd=hd, t2=t2, po=po, cs=cs: e.matmul(pss[:, hd * 128:(hd + 1) * 128], lhsT=ktT[po:po + 64, t2, cs], rhs=qtT[po:po + 64, t2, cs],
                                                                              start=True, stop=True), reads=[ktT.r((t2, n)), qtT.r((t2, n))], writes=[pss.r()], pmode="k64")
        pt = PT[c % 2]
        P.op("dve", lambda e, pss=pss, pt=pt: e.tensor_tensor(out=pt[:], in0=pss[:].rearrange("p (a b) -> p a b", a=4), in1=maskb, op=ALU.mult),
             reads=[pss.r(), cst.r()], writes=[pt.r()])
        pso = bank()
        for hd in range(4):
            t2, po = hd // 2, (hd % 2) * 64
            P.op("pe", lambda e, pso=pso, pt=pt, hd=hd, c=c: e.matmul(pso[:, hd * 128:(hd + 1) * 128], lhsT=pt[:, hd, :], rhs=vtok[:, c, hd * 128:(hd + 1) * 128],
                                                                     start=True, stop=(c == 0)), reads=[pt.r(), vtok.r(c)], writes=[pso.r()])
            if c > 0:
                P.op("pe", lambda e, pso=pso, hd=hd, t2=t2, po=po, cs=cs: e.matmul(pso[:, hd * 128:(hd + 1) * 128], lhsT=qtT[po:po + 64, t2, cs], rhs=Sbf[po:po + 64, t2, :],
                                                                                  start=False, stop=True), reads=[qtT.r((t2, n)), Sbf.r()], writes=[pso.r()], pmode="k64")
        ot, on_, yb_, os_ = otok[c % 2], onrm[c % 2], ybtok[c % 2], oss[c % 2]
        P.op("act", lambda e, pso=pso, ot=ot: e.copy(out=ot[:], in_=pso[:]), reads=[pso.r()], writes=[ot.r()])
        for hd in range(4):
            P.op("act", lambda e, ot=ot, os_=os_, hd=hd: e.activation(out=osq[:], in_=ot[:, hd * 128:(hd + 1) * 128], func=AF.Square, accum_out=os_[:, hd:hd + 1]),
                 reads=[ot.r()], writes=[osq.r(), os_.r()])
        P.op("dve", lambda e, os_=os_: e.tensor_scalar(out=os_[:, 4:8], in0=os_[:, 0:4], scalar1=1.0 / 128.0, scalar2=EPS, op0=ALU.mult, op1=ALU.add), reads=[os_.r()], writes=[os_.r()])
        P.op("act", lambda e, os_=os_: e.activation(out=os_[:, 4:8], in_=os_[:, 4:8], func=AF.Sqrt), reads=[os_.r()], writes=[os_.r()])
        P.op("dve", lambda e, os_=os_: e.reciprocal(out=os_[:, 4:8], in_=os_[:, 4:8]), reads=[os_.r()], writes=[os_.r()])
        for hd in range(4):
            P.op("dve", lambda e, ot=ot, on_=on_, os_=os_, hd=hd: e.scalar_tensor_tensor(out=on_[:, hd * 128:(hd + 1) * 128], in0=ot[:, hd * 128:(hd + 1) * 128], scalar=os_[:, 4 + hd:5 + hd],
                                                                                        in1=gsm[:, 128:256], op0=ALU.mult, op1=ALU.mult), reads=[ot.r(), os_.r(), gsm.r()], writes=[on_.r()])
        P.op("pool", lambda e, on_=on_, yb_=yb_, c=c: e.tensor_tensor(out=yb_[:], in0=on_[:], in1=rstok[:, c, :], op=ALU.mult), reads=[on_.r(), rstok.r(c)], writes=[yb_.r()])
        pst = bank()
        pstb = pst.t[:].bitcast(BF16)
        for m in range(4):
            P.op("pe", lambda e, pstb=pstb, yb_=yb_, m=m: e.transpose(pstb[:, m * 128:(m + 1) * 128], yb_[:, m * 128:(m + 1) * 128], identb_t[:]),
                 reads=[yb_.r(), identb_t.r()], writes=[pst.r()])
        evac_copy(ybT[:, :, cs], pstb[:, 0:512].rearrange("p (a b) -> p a b", a=4), [pst.r()], [ybT.r((m, n)) for m in range(4)])
        if c < NT - 1:
            for t2 in range(2):
                psu = bank()
                P.op("pe", lambda e, psu=psu, c=c, t2=t2: e.matmul(psu[:, 0:256], lhsT=ktok[:, c, t2 * 128:(t2 + 1) * 128], rhs=vtok[:, c, t2 * 256:(t2 + 1) * 256], start=True, stop=True),
                     reads=[ktok.r(c), vtok.r(c)], writes=[psu.r()])
                for hl in range(2):
                    rows = slice(hl * 64, (hl + 1) * 64)
                    P.op("dve", lambda e, psu=psu, t2=t2, hl=hl, rows=rows: e.tensor_tensor(out=S32[rows, t2, :], in0=psu[rows, hl * 128:(hl + 1) * 128], in1=S32[rows, t2, :], op=ALU.add),
                         reads=[psu.r(), S32.r()], writes=[S32.r()])
                    P.op("act", lambda e, t2=t2, rows=rows, c=c: e.activation(out=S32[rows, t2, :], in_=S32[rows, t2, :], func=AF.Copy, scale=eblast[rows, t2, c:c + 1]),
                         reads=[S32.r(), eblast.r()], writes=[S32.r()])
                    P.op("dve", lambda e, t2=t2, rows=rows: e.tensor_copy(out=Sbf[rows, t2, :], in_=S32[rows, t2, :]), reads=[S32.r()], writes=[Sbf.r()])

    if stage == 3:
        o1 = dbg_out("d_ybT", [128, 4 * L], BF16)
        P.dma("sp", lambda e: e.dma_start(out=o1, in_=ybT[:].rearrange("p a b -> p (a b)")), reads=[ybT.r((k, n)) for k in range(4) for n in range(4)])
        o2 = dbg_out("d_yaT", [128, 4 * L], BF16)
        P.dma("sp", lambda e: e.dma_start(out=o2, in_=yaT[:].rearrange("p a b -> p (a b)")), reads=[yaT.r((k, n)) for k in range(4) for n in range(4)])
        P.wait_all_dma("sp")
        P.build()
        return nc

    P.pe_selfsync = False
    proj_gate(wpb, wgb, ybT, False)

    if stage == 4:
        o1 = dbg_out("d_mixedT", [128, 8 * L], BF16)
        P.dma("sp", lambda e: e.dma_start(out=o1, in_=mixedT[:].rearrange("p a b -> p (a b)")), reads=[mixedT.r((k, n)) for k in range(8) for n in range(4)])
        P.wait_all_dma("sp")
        P.build()
        return nc


    A.release(m_C2)
    acc = A.alloc("acc", [128, NT, D], F32)
    m_E = A.mark()
    wout = A.alloc("wout", [128, 8, D], BF16)
    P.dma("pool", lambda e: e.dma_start(out=wout[:], in_=wout_d.rearrange("(k p) n -> p k n", p=128)), writes=[wout.r()])
    for k in range(8):
        eng = "dve" if k % 2 == 0 else "pool"
        P.op(eng, lambda e, k=k: e.tensor_tensor(out=wout[:, k, :], in0=wout[:, k, :], in1=g12[:, 0, :], op=ALU.mult), reads=[wout.r(), g12.r(0)], writes=[wout.r()])
    xin2 = [A.alloc(f"xin2_{i}", [128, 4, D], F32) for i in range(2)]
    for blk in range(4):
        xi = xin2[blk % 2]
        P.dma("sp", lambda e, xi=xi, blk=blk: e.dma_start(out=xi[:], in_=x_v[:, blk * 4:(blk + 1) * 4, :]), writes=[xi.r()])
        for j in range(4):
            c = blk * 4 + j
            for half in range(2):
                ps = bank()
                for k in range(8):
                    P.op("pe", lambda e, ps=ps, c=c, k=k, half=half: e.matmul(ps[:], lhsT=mixedT[:, k, c * 128:(c + 1) * 128], rhs=wout[:, k, half * 512:(half + 1) * 512],
                                                                             start=(k == 0), stop=(k == 7)), reads=[mixedT.r((k, blk)), wout.r()], writes=[ps.r()])
                P.op("dve", lambda e, ps=ps, xi=xi, j=j, c=c, half=half: e.tensor_tensor(out=acc[:, c, half * 512:(half + 1) * 512], in0=ps[:], in1=xi[:, j, half * 512:(half + 1) * 512], op=ALU.add),
                     reads=[ps.r(), xi.r()], writes=[acc.r((c, half))])
    A.release(m_E)

    if stage == 5:
        o1 = dbg_out("d_x1", [L, D], F32)
        P.dma("sp", lambda e: e.dma_start(out=o1.rearrange("(n p) d -> p n d", p=128), in_=acc[:]), reads=[acc.r((c, h_)) for c in range(NT) for h_ in range(2)])
        P.wait_all_dma("sp")
        P.build()
        return nc

    NB = 384
    NSLOT = NB * 128
    xn2_d = nc.dram_tensor("xn2_scr", [L, D], BF16).ap()
    slottab_d = nc.dram_tensor("slottab_scr", [NSLOT, 2], I32).ap()
    yslots_d = nc.dram_tensor("yslots_scr", [NSLOT, D], F32).ap()
    bexp_d = nc.dram_tensor("bexp_scr", [NB], I32).ap()
    r_xn2, r_slottab, r_yslots, r_bexp = Reg("xn2"), Reg("slottab"), Reg("yslots"), Reg("bexp")
    h2T = A.alloc("h2T", [128, 8, L], BF16, at=hT_off)
    Msk = A.alloc("Msk", [128, NT, NEXP], BF16, at=mixedT_off + 17664)
    Wr = A.alloc("Wr", [128, NT, NEXP + 1], F32, at=mixedT_off)
    rbias = A.alloc("rbias", [128, NEXP], F32, at=mixedT_off + 16512)
    m_F = A.mark()
    wrt = A.alloc("wrt", [128, 8, NEXP], F32)
    P.dma("sp", lambda e: e.dma_start(out=wrt[:], in_=rw_d.rearrange("(k p) n -> p k n", p=128)), writes=[wrt.r()])
    P.dma("sp", lambda e: e.dma_start(out=rbias[:], in_=bass.AP(rb_d.tensor, rb_d.offset, [[0, 128], [1, NEXP]])), writes=[rbias.r()])
    P.op("pool", lambda e: e.memset(Wr[:, :, NEXP:NEXP + 1], 1.0), writes=[Wr.r("sh")])
    xnf = A.alloc("xnf", [128, 4, D], F32)
    h2f = A.alloc("h2f", [128, 8, 512], F32)
    rt = A.alloc("rt", [128, 2048], F32)
    SC, BI, MB, SEL = 0, 256, 512, 768
    M8 = A.alloc("M8", [128, 128], F32)
    for blk in range(4):
        for j in range(4):
            c = blk * 4 + j
            P.op("act", lambda e, c=c, blk=blk: e.activation(out=sq[:], in_=acc[:, c, :], func=AF.Square, accum_out=ssq[:, c:c + 1]),
                 reads=[acc.r((c, 0)), acc.r((c, 1))], writes=[sq.r(), ssq.r(blk)])
        rs = rstd[:, blk * 4:(blk + 1) * 4]
        P.op("dve", lambda e, rs=rs, blk=blk: e.tensor_scalar(out=rs, in0=ssq[:, blk * 4:(blk + 1) * 4], scalar1=1.0 / D, scalar2=EPS, op0=ALU.mult, op1=ALU.add),
             reads=[ssq.r(blk)], writes=[rstd.r(blk)])
        P.op("act", lambda e, rs=rs: e.activation(out=rs, in_=rs, func=AF.Sqrt), reads=[rstd.r(blk)], writes=[rstd.r(blk)])
        P.op("dve", lambda e, rs=rs: e.reciprocal(out=rs, in_=rs), reads=[rstd.r(blk)], writes=[rstd.r(blk)])
        for j in range(4):
            c = blk * 4 + j
            P.op("act", lambda e, j=j, c=c: e.activation(out=xnf[:, j, :], in_=acc[:, c, :], func=AF.Copy, scale=rstd[:, c:c + 1]),
                 reads=[acc.r((c, 0)), acc.r((c, 1)), rstd.r(blk)], writes=[xnf.r(j)])
            P.dma("pool", lambda e, j=j, c=c: e.dma_start(out=xn2_d[c * 128:(c + 1) * 128, :], in_=xnf[:, j, :]), reads=[xnf.r(j)], wacc=[r_xn2])
        for k in range(8):
            ps = bank()
            for j in range(4):
                P.op("pe", lambda e, ps=ps, j=j, k=k: e.transpose(ps[:, j * 128:(j + 1) * 128], xnf[:, j, k * 128:(k + 1) * 128], cst[:, 0:128]),
                     reads=[xnf.r(j), cst.r()], writes=[ps.r()], pmode="f32")
            P.op("dve", lambda e, ps=ps, k=k, blk=blk: e.tensor_scalar(out=h2T[:, k, blk * 512:(blk + 1) * 512], in0=ps[:], scalar1=AB[:, 1, k:k + 1], scalar2=modcol[:, 2, k:k + 1],
                                                                      op0=ALU.mult, op1=ALU.add), reads=[ps.r(), AB.r(), modcol.r()], writes=[h2T.r((k, blk))])
            P.op("dve", lambda e, ps=ps, k=k: e.tensor_scalar(out=h2f[:, k, :], in0=ps[:], scalar1=AB[:, 1, k:k + 1], scalar2=modcol[:, 2, k:k + 1], op0=ALU.mult, op1=ALU.add),
                 reads=[ps.r(), AB.r(), modcol.r()], writes=[h2f.r(k)])
        for j in range(4):
            c = blk * 4 + j
            ps = bank()
            for k in range(8):
                P.op("pe", lambda e, ps=ps, j=j, k=k: e.matmul(ps[:, 0:NEXP], lhsT=h2f[:, k, j * 128:(j + 1) * 128], rhs=wrt[:, k, :], start=(k == 0), stop=(k == 7)),
                     reads=[h2f.r(k), wrt.r()], writes=[ps.r()], pmode="f32")
            rr = [rt.r()]
            mr = [M8.r()]
            P.op("act", lambda e, ps=ps: e.activation(out=rt[:, SC:SC + 256], in_=ps[:, 0:NEXP], func=AF.Sigmoid), reads=[ps.r()], writes=rr)
            P.op("dve", lambda e: e.tensor_tensor(out=rt[:, BI:BI + 256], in0=rt[:, SC:SC + 256], in1=rbias[:], op=ALU.add), reads=rr + [rbias.r()], writes=rr)
            for gq in range(8):
                P.op("dve", lambda e, gq=gq: e.max(out=M8[:, gq * 8:(gq + 1) * 8], in_=rt[:, BI + gq * 32:BI + (gq + 1) * 32]), reads=rr, writes=mr)
            P.op("dve", lambda e: e.tensor_tensor(out=M8[:, 64:72], in0=fv(M8, 0, [(8, 8)]), in1=fv(M8, 1, [(8, 8)]), op=ALU.add), reads=mr, writes=mr)
            P.op("dve", lambda e: e.max(out=M8[:, 72:80], in_=M8[:, 64:72]), reads=mr, writes=mr)
            P.op("dve", lambda e: e.tensor_scalar(out=M8[:, 80:88], in0=M8[:, 64:72], scalar1=M8[:, 75:76], scalar2=None, op0=ALU.is_ge), reads=mr, writes=mr)
            P.op("dve", lambda e: e.scalar_tensor_tensor(out=fv(rt, MB, [(32, 8), (1, 32)]), in0=fv(rt, BI, [(32, 8), (1, 32)]), scalar=2.0, in1=fv(M8, 80, [(1, 8), (0, 32)]),
                                                         op0=ALU.add, op1=ALU.mult), reads=rr + mr, writes=rr)
            P.op("dve", lambda e: e.max(out=M8[:, 88:96], in_=rt[:, MB:MB + 256]), reads=rr, writes=mr)
            P.op("dve", lambda e: e.tensor_scalar(out=rt[:, SEL:SEL + 256], in0=rt[:, MB:MB + 256], scalar1=M8[:, 95:96], scalar2=None, op0=ALU.is_ge), reads=rr + mr, writes=rr)
            P.op("pool", lambda e, c=c: e.tensor_copy(out=Msk[:, c, :], in_=rt[:, SEL:SEL + 256]), reads=rr, writes=[Msk.r(c)])
            P.op("dve", lambda e: e.tensor_tensor(out=rt[:, SEL:SEL + 256], in0=rt[:, SEL:SEL + 256], in1=rt[:, SC:SC + 256], op=ALU.mult), reads=rr, writes=rr)
            P.op("dve", lambda e: e.tensor_reduce(out=M8[:, 96:97], in_=rt[:, SEL:SEL + 256], axis=AX.X, op=ALU.add), reads=rr, writes=mr)
            P.op("dve", lambda e: e.reciprocal(out=M8[:, 97:98], in_=M8[:, 96:97]), reads=mr, writes=mr)
            P.op("dve", lambda e, c=c: e.tensor_scalar(out=Wr[:, c, 0:NEXP], in0=rt[:, SEL:SEL + 256], scalar1=M8[:, 97:98], scalar2=2.5, op0=ALU.mult, op1=ALU.mult),
                 reads=rr + mr, writes=[Wr.r(c)])
    A.release(m_F)

    if stage == 6:
        o1 = dbg_out("d_W", [L, NEXP + 1], F32)
        P.dma("sp", lambda e: e.dma_start(out=o1.rearrange("(n p) d -> p n d", p=128), in_=Wr[:]), reads=[Wr.r(c) for c in range(NT)] + [Wr.r("sh")])
        o2 = dbg_out("d_h2T", [128, 8 * L], BF16)
        P.dma("sp", lambda e: e.dma_start(out=o2, in_=h2T[:].rearrange("p a b -> p (a b)")), reads=[h2T.r((k, n)) for k in range(8) for n in range(4)])
        P.wait_all_dma("sp")
        P.build()
        return nc

    IOA = bass.IndirectOffsetOnAxis
    regcache = {}

    def breg(e, val):
        if ("b", val) not in regcache:
            regcache[("b", val)] = e.to_reg(val)
        return regcache[("b", val)]
    m_G0 = A.mark()
    wsh = A.alloc("wsh", [128, 2, 2048], BF16)
    wdsh = A.alloc("wdsh", [128, 2048], BF16)
    P.dma("pool", lambda e: e.dma_start(out=wsh[:, 0, :], in_=ewg_d[(n_exp - 1) * 128:n_exp * 128, :]), writes=[wsh.r("g")])
    P.dma("pool", lambda e: e.dma_start(out=wsh[:, 1, :], in_=ewu_d[(n_exp - 1) * 128:n_exp * 128, :]), writes=[wsh.r("u")])
    P.dma("pool", lambda e: e.dma_start(out=wdsh[:], in_=ewd_d[(n_exp - 1) * 128:n_exp * 128, :]), writes=[wdsh.r()])
    aT = [A.alloc(f"aT{i}", [128, 2, 512], BF16) for i in range(2)]
    sgm = [A.alloc(f"sgm{i}", [128, 512], BF16) for i in range(2)]
    for kk in range(2):
        P.op("pool", lambda e, kk=kk: e.tensor_tensor(out=wdsh[:, kk * 1024:(kk + 1) * 1024], in0=wdsh[:, kk * 1024:(kk + 1) * 1024], in1=g12[:, 1, :], op=ALU.mult),
             reads=[wdsh.r(), g12.r(1)], writes=[wdsh.r()])
    for n in range(4):
        at_ = aT[n % 2]
        for f in range(2):
            psg = bank()
            for k in range(8):
                P.op("pe", lambda e, psg=psg, f=f, k=k, n=n: e.matmul(psg[:], lhsT=wsh[:, 0, k * 256 + f * 128:k * 256 + (f + 1) * 128], rhs=h2T[:, k, n * 512:(n + 1) * 512], start=(k == 0), stop=(k == 7)),
                     reads=[wsh.r("g"), h2T.r((k, n))], writes=[psg.r()])
            psu = bank()
            for k in range(8):
                P.op("pe", lambda e, psu=psu, f=f, k=k, n=n: e.matmul(psu[:], lhsT=wsh[:, 1, k * 256 + f * 128:k * 256 + (f + 1) * 128], rhs=h2T[:, k, n * 512:(n + 1) * 512], start=(k == 0), stop=(k == 7)),
                     reads=[wsh.r("u"), h2T.r((k, n))], writes=[psu.r()])
            sg = sgm[f]
            P.op("act", lambda e, psg=psg, sg=sg: e.activation(out=sg[:], in_=psg[:], func=AF.Silu), reads=[psg.r()], writes=[sg.r()])
            P.op("dve", lambda e, psu=psu, sg=sg, at_=at_, f=f: e.tensor_tensor(out=at_[:, f, :], in0=psu[:], in1=sg[:], op=ALU.mult), reads=[psu.r(), sg.r()], writes=[at_.r(f)])
        for j in range(4):
            c = n * 4 + j
            for half in range(2):
                psy = bank()
                for f in range(2):
                    P.op("pe", lambda e, psy=psy, at_=at_, f=f, j=j, half=half: e.matmul(psy[:], lhsT=at_[:, f, j * 128:(j + 1) * 128], rhs=wdsh[:, f * 1024 + half * 512:f * 1024 + (half + 1) * 512],
                                                                                   start=(f == 0), stop=(f == 1)), reads=[at_.r(f), wdsh.r()], writes=[psy.r()])
                dst = acc[:, c, half * 512:(half + 1) * 512]
                P.op("dve", lambda e, psy=psy, dst=dst: e.tensor_tensor(out=dst, in0=psy[:], in1=dst, op=ALU.add), reads=[psy.r(), acc.r((c, half))], writes=[acc.r((c, half))])
    A.release(m_G0)

    onesb = A.alloc("onesb", [128, 128], BF16)
    sutb = A.alloc("sutb", [128, 128], BF16)
    onesf = A.alloc("onesf", [128, 256], F32)
    P.op("pool", lambda e: e.memset(onesb[:], 1.0), writes=[onesb.r()])
    P.op("pool", lambda e: e.memset(onesf[:], 1.0), writes=[onesf.r()])
    P.op("dve", lambda e: e.tensor_copy(out=sutb[:], in_=cst[:, 704:832]), reads=[cst.r()], writes=[sutb.r()])
    zt = A.alloc("zt", [128, 768], I32)
    P.op("pool", lambda e: e.memset(zt[:], 0), writes=[zt.r()])
    P.dma("sp", lambda e: e.dma_start(out=slottab_d.rearrange("(p n) two -> p (n two)", p=128), in_=zt[:]), reads=[zt.r()], writes=[r_slottab])
    cnt = A.alloc("cntt", [128, 1024], F32)
    cr_ = [cnt.r()]
    ps = bank()
    for c in range(NT):
        P.op("pe", lambda e, ps=ps, c=c: e.matmul(ps[:, 0:256], lhsT=onesb[:], rhs=Msk[:, c, :], start=(c == 0), stop=(c == NT - 1)), reads=[onesb.r(), Msk.r(c)], writes=[ps.r()])
    P.op("dve", lambda e, ps=ps: e.tensor_copy(out=cnt[:, 0:256], in_=ps[:, 0:256]), reads=[ps.r()], writes=cr_)
    widx = A.alloc("widx", [128, NB], I32)
    m_cmp = A.mark()
    cmp = A.alloc("cmpt", [128, 4096], F32)
    P.op("dve", lambda e: e.tensor_tensor(out=fv(cmp, 0, [(16, 256), (1, 16)]), in0=fv(cnt, 0, [(1, 256), (0, 16)]), in1=fv(cst, 680, [(0, 256), (1, 16)]), op=ALU.is_gt),
         reads=cr_ + [cst.r()], writes=[cmp.r()])
    P.op("dve", lambda e: e.tensor_reduce(out=cnt[:, 256:512], in_=fv(cmp, 0, [(16, 256), (1, 16)]), axis=AX.X, op=ALU.add), reads=[cmp.r()], writes=cr_)
    P.op("dve", lambda e: e.tensor_tensor_scan(out=cnt[:, 512:768], data0=onesf[:], data1=cnt[:, 256:512], initial=0.0, op0=ALU.mult, op1=ALU.add), reads=cr_ + [onesf.r()], writes=cr_)
    P.op("dve", lambda e: e.tensor_tensor(out=cnt[:, 768:1024], in0=cnt[:, 512:768], in1=cnt[:, 256:512], op=ALU.subtract), reads=cr_, writes=cr_)
    P.op("dve", lambda e: e.tensor_scalar(out=cnt[:, 768:1024], in0=cnt[:, 768:1024], scalar1=128.0, scalar2=None, op0=ALU.mult), reads=cr_, writes=cr_)
    bx = A.alloc("bx", [128, 8], F32)
    bxi = A.alloc("bxi", [128, 4], I32)
    for i in range(3):
        P.op("dve", lambda e, i=i: e.tensor_scalar(out=bx[:, 4 + i:5 + i], in0=cst[:, 840:841], scalar1=float(128 * i), scalar2=None, op0=ALU.add), reads=[cst.r()], writes=[bx.r()])
        P.op("dve", lambda e, i=i: e.tensor_scalar(out=cmp[:, 0:256], in0=cnt[:, 512:768], scalar1=bx[:, 4 + i:5 + i], scalar2=None, op0=ALU.is_le), reads=cr_ + [bx.r()], writes=[cmp.r()])
        P.op("dve", lambda e, i=i: e.tensor_reduce(out=bx[:, i:i + 1], in_=cmp[:, 0:256], axis=AX.X, op=ALU.add), reads=[cmp.r()], writes=[bx.r()])
    onesf128 = A.alloc("onesf128", [128, 128], F32)
    P.op("pool", lambda e: e.memset(onesf128[:], 1.0), writes=[onesf128.r()])
    dg = A.alloc("dg", [128, 3, 128], F32)
    widf = A.alloc("widf", [128, NB], F32)
    wtl = A.alloc("wtl", [128, NB], F32)
    psb_ = bank()
    for i in range(3):
        P.op("dve", lambda e, i=i: e.tensor_scalar(out=dg[:, i, :], in0=cst[:, 0:128], scalar1=bx[:, i:i + 1], scalar2=None, op0=ALU.mult), reads=[cst.r(), bx.r()], writes=[dg.r(i)])
        P.op("pe", lambda e, i=i, psb_=psb_: e.matmul(psb_[:, i * 128:(i + 1) * 128], lhsT=onesf128[:], rhs=dg[:, i, :], start=True, stop=True), reads=[onesf128.r(), dg.r(i)], writes=[psb_.r()], pmode="f32")
    P.op("dve", lambda e, psb_=psb_: e.tensor_scalar(out=widf[:], in0=psb_[:, 0:NB], scalar1=128.0, scalar2=cst[:, 840:841], op0=ALU.mult, op1=ALU.add), reads=[psb_.r(), cst.r()], writes=[widf.r()])
    P.op("dve", lambda e, psb_=psb_: e.tensor_scalar(out=wtl[:], in0=psb_[:, 0:NB], scalar1=float(NEXP), scalar2=1.0e6, op0=ALU.is_ge, op1=ALU.mult), reads=[psb_.r()], writes=[wtl.r()])
    P.op("dve", lambda e: e.tensor_tensor(out=widf[:], in0=widf[:], in1=wtl[:], op=ALU.add), reads=[widf.r(), wtl.r()], writes=[widf.r()])
    P.op("dve", lambda e: e.tensor_copy(out=widx[:], in_=widf[:]), reads=[widf.r()], writes=[widx.r()])
    A.release(m_cmp)
    tokid = A.alloc("tokid", [128, NT], I32)
    P.op("pool", lambda e: e.iota(tokid[:], [[128, NT]], base=0, channel_multiplier=1), writes=[tokid.r()])
    d8 = A.alloc("d8", [128, NT, 8], F32)
    d8i = A.alloc("d8i", [128, NT, 8], I32)
    m_bk = A.mark()
    keyt = [A.alloc(f"key{i}", [128, 256], F32) for i in range(2)]
    eqt = A.alloc("eqt", [128, 256], F32)
    junk = A.alloc("junk", [128, 256], F32)
    w8t = [A.alloc(f"w8_{i}", [128, 8], F32) for i in range(2)]
    ixt = [[A.alloc(f"ix{i}_{j}", [128, 1], I32) for j in range(8)] for i in range(2)]
    rct = [[A.alloc(f"rc{i}_{j}", [128, 2], I32) for j in range(8)] for i in range(2)]
    for c in range(NT):
        ps = bank()
        for c2 in range(c):
            P.op("pe", lambda e, ps=ps, c2=c2: e.matmul(ps[:, 0:256], lhsT=onesb[:], rhs=Msk[:, c2, :], start=(c2 == 0), stop=False), reads=[onesb.r(), Msk.r(c2)], writes=[ps.r()])
        P.op("pe", lambda e, ps=ps, c=c: e.matmul(ps[:, 0:256], lhsT=sutb[:], rhs=Msk[:, c, :], start=(c == 0), stop=True), reads=[sutb.r(), Msk.r(c)], writes=[ps.r()])
        key, w8 = keyt[c % 2], w8t[c % 2]
        P.op("dve", lambda e, ps=ps, key=key: e.scalar_tensor_tensor(out=key[:], in0=ps[:, 0:256], scalar=1.0, in1=cnt[:, 768:1024], op0=ALU.add, op1=ALU.add), reads=[ps.r()] + cr_, writes=[key.r()])
        P.op("dve", lambda e, key=key, c=c: e.tensor_tensor(out=key[:], in0=key[:], in1=Msk[:, c, :], op=ALU.mult), reads=[key.r(), Msk.r(c)], writes=[key.r()])
        P.op("dve", lambda e, key=key, c=c: e.max(out=d8[:, c, :], in_=key[:]), reads=[key.r()], writes=[d8.r(c)])
        for j in range(8):
            P.op("dve", lambda e, key=key, w8=w8, c=c, j=j: e.scalar_tensor_tensor(out=junk[:], in0=key[:], scalar=d8[:, c, j:j + 1], in1=Wr[:, c, 0:NEXP], op0=ALU.is_equal, op1=ALU.mult,
                                                                                   accum_out=w8[:, j:j + 1]), reads=[key.r(), d8.r(c), Wr.r(c)], writes=[junk.r(), w8.r()])
        P.op("dve", lambda e, c=c: e.tensor_scalar(out=d8[:, c, :], in0=d8[:, c, :], scalar1=-1.0, scalar2=None, op0=ALU.add), reads=[d8.r(c)], writes=[d8.r(c)])
        P.op("dve", lambda e, c=c: e.tensor_copy(out=d8i[:, c, :], in_=d8[:, c, :]), reads=[d8.r(c)], writes=[d8i.r(c)])
        for j in range(8):
            ix, rc = ixt[c % 2][j], rct[c % 2][j]
            P.op("dve", lambda e, ix=ix, c=c, j=j: e.tensor_copy(out=ix[:], in_=d8i[:, c, j:j + 1]), reads=[d8i.r(c)], writes=[ix.r()])
            P.op("pool", lambda e, rc=rc, c=c: e.tensor_copy(out=rc[:, 0:1], in_=tokid[:, c:c + 1]), reads=[tokid.r()], writes=[rc.r()])
            P.op("pool", lambda e, rc=rc, w8=w8, j=j: e.tensor_copy(out=rc[:, 1:2], in_=w8[:, j:j + 1].bitcast(I32)), reads=[w8.r()], writes=[rc.r()])
            P.dma("pool", lambda e, ix=ix, rc=rc: e.indirect_dma_start(out=slottab_d, out_offset=IOA(ap=ix[:, :], axis=0), in_=rc[:], in_offset=None,
                                                                     bounds_check=breg(e, NSLOT - 1), oob_is_err=False), reads=[rc.r(), ix.r()], wacc=[r_slottab])
    A.release(m_bk)

    if stage == 7:
        o1 = dbg_out("d_slottab", [NSLOT, 2], I32)
        P.dma("sp", lambda e: e.dma_start(out=o1, in_=slottab_d), reads=[r_slottab])
        o2 = dbg_out("d_d8i", [128, NT * 8], I32)
        P.dma("sp", lambda e: e.dma_start(out=o2, in_=d8i[:].rearrange("p a b -> p (a b)")), reads=[d8i.r(c) for c in range(NT)])
        o3 = dbg_out("d_widx", [128, NB], I32)
        P.dma("sp", lambda e: e.dma_start(out=o3, in_=widx[:]), reads=[widx.r()])
        o5 = dbg_out("d_W", [L, NEXP + 1], F32)
        P.dma("sp", lambda e: e.dma_start(out=o5.rearrange("(n p) d -> p n d", p=128), in_=Wr[:]), reads=[Wr.r(c) for c in range(NT)] + [Wr.r("sh")])
        o4 = dbg_out("d_cnt", [128, 1024], F32)
        P.dma("sp", lambda e: e.dma_start(out=o4, in_=cnt[:]), reads=cr_)
        P.wait_all_dma("sp")
        P.build()
        return nc

    m_blk = A.mark()
    NBUF = 4
    NFE = 6
    wgu = [A.alloc(f"wgu{i}", [128, 2, 2048], BF16, at=hT_off + i * 8192) for i in range(NBUF)]
    ysb = [A.alloc(f"ysb{i}", [128, D], F32) for i in range(2)]
    wixt = [A.alloc(f"wix{i}", [128, 1], I32) for i in range(NBUF)]
    stt = [A.alloc(f"st{i}", [128, 2], I32) for i in range(NFE)]
    stk = [A.alloc(f"stk{i}", [128, 1], I32) for i in range(NFE)]
    Xg = [A.alloc(f"Xg{i}", [128, D], BF16) for i in range(NFE)]
    XgT = [A.alloc(f"XgT{i}", [128, 8, 128], BF16) for i in range(2)]
    sgs = [A.alloc(f"sgs{i}", [128, 256], BF16) for i in range(2)]
    aTk = [A.alloc(f"aTk{i}", [128, 256], BF16) for i in range(2)]
    aTs = [A.alloc(f"aTs{i}", [128, 256], BF16) for i in range(2)]
    a2 = AB[:, 1, 0:1]
    A2b = bass.AP(a2.tensor, a2.offset, [list(a2.ap[0]), [1, 8], [0, 128]])
    b2 = modcol[:, 2, 0:1]
    B2b = bass.AP(b2.tensor, b2.offset, [list(b2.ap[0]), [1, 8], [0, 128]])
    nblk_run = NB if stage >= 99 else 4

    def front_end(b):
        s_, sk, xg = stt[b % NFE], stk[b % NFE], Xg[b % NFE]
        P.dma("sp", lambda e, s_=s_, b=b: e.dma_start(out=s_[:], in_=slottab_d[b * 128:(b + 1) * 128, :]), reads=[r_slottab], writes=[s_.r()])
        P.op("dve", lambda e, sk=sk, s_=s_: e.tensor_copy(out=sk[:], in_=s_[:, 0:1]), reads=[s_.r()], writes=[sk.r()])
        P.dma("pool", lambda e, xg=xg, sk=sk: e.indirect_dma_start(out=xg[:], out_offset=None, in_=xn2_d, in_offset=IOA(ap=sk[:, :], axis=0), bounds_check=breg(e, L - 1), oob_is_err=False),
              reads=[sk.r(), r_xn2], writes=[xg.r()])

    NWD = 5
    wdn = [A.alloc(f"wdn4_{i}", [128, 2048], BF16) for i in range(NWD)]

    def load_blk(b):
        wb, wd_, wx = wgu[b % NBUF], wdn[b % NWD], wixt[b % NBUF]
        P.op("pool", lambda e, wx=wx, b=b: e.tensor_copy(out=wx[:], in_=widx[:, b:b + 1]), reads=[widx.r()], writes=[wx.r()])
        for dst, src_d, rg in ((wb[:, 0, :], ewg_d, wb.r("g")), (wb[:, 1, :], ewu_d, wb.r("u")), (wd_[:], ewd_d, wd_.r())):
            P.dma("pool", lambda e, dst=dst, src_d=src_d, wx=wx: e.indirect_dma_start(out=dst, out_offset=None, in_=src_d, in_offset=IOA(ap=wx[:, :], axis=0),
                                                                                 bounds_check=breg(e, n_exp * 128 - 1), oob_is_err=False), reads=[wx.r()], writes=[rg])

    def st_T(b):
        xg, xt = Xg[b % NFE], XgT[b % 2]
        pst = bank()
        pstb = pst.t[:].bitcast(BF16)
        for k in range(8):
            P.op("pe", lambda e, pstb=pstb, xg=xg, k=k: e.transpose(pstb[:, k * 128:(k + 1) * 128], xg[:, k * 128:(k + 1) * 128], identb_t[:]), reads=[xg.r(), identb_t.r()], writes=[pst.r()])
        P.op("dve", lambda e, pstb=pstb, xt=xt: e.tensor_tensor(out=xt[:], in0=pstb[:, 0:1024].rearrange("p (a b) -> p a b", a=8), in1=A2b, op=ALU.mult), reads=[pst.r(), AB.r()], writes=[xt.r()])
        P.op("dve", lambda e, xt=xt: e.tensor_tensor(out=xt[:], in0=xt[:], in1=B2b, op=ALU.add), reads=[xt.r(), modcol.r()], writes=[xt.r()])

    def st_MM(b):
        wb, s_, xt = wgu[b % NBUF], stt[b % NFE], XgT[b % 2]
        psgu = bank()
        for k in range(8):
            wv = wb[:, 0, k * 256:k * 256 + 1]
            rhs = bass.AP(wv.tensor, wv.offset, [list(wv.ap[0]), [2048, 2], [1, 256]])
            P.op("pe", lambda e, psgu=psgu, xt=xt, k=k, rhs=rhs: e.matmul(psgu[:].rearrange("p (a b) -> p a b", a=2), lhsT=xt[:, k, :], rhs=rhs, start=(k == 0), stop=(k == 7)),
                 reads=[wb.r("g"), wb.r("u"), xt.r()], writes=[psgu.r()])
        sg, ak = sgs[b % 2], aTk[b % 2]
        swc = s_[:, 1:2].bitcast(F32)
        P.op("act", lambda e, psgu=psgu, sg=sg: e.activation(out=sg[:], in_=psgu[:, 0:256], func=AF.Silu), reads=[psgu.r()], writes=[sg.r()])
        P.op("dve", lambda e, psgu=psgu, sg=sg, ak=ak, swc=swc: e.scalar_tensor_tensor(out=ak[:], in0=sg[:], scalar=swc, in1=psgu[:, 256:512], op0=ALU.mult, op1=ALU.mult),
             reads=[psgu.r(), sg.r(), s_.r()], writes=[ak.r()])

    def st_T2(b):
        ak, at_ = aTk[b % 2], aTs[b % 2]
        psa = bank()
        psab = psa.t[:].bitcast(BF16)
        for f in range(2):
            P.op("pe", lambda e, psab=psab, ak=ak, f=f: e.transpose(psab[:, f * 128:(f + 1) * 128], ak[:, f * 128:(f + 1) * 128], identb_t[:]), reads=[ak.r(), identb_t.r()], writes=[psa.r()])
        P.op("act", lambda e, psab=psab, at_=at_: e.copy(out=at_[:], in_=psab[:, 0:256]), reads=[psa.r()], writes=[at_.r()])

    def st_MM2(b):
        at_, wd_, ys = aTs[b % 2], wdn[b % NWD], ysb[b % 2]
        for half in range(2):
            psy = bank()
            for f in range(2):
                P.op("pe", lambda e, psy=psy, at_=at_, wd_=wd_, f=f, half=half: e.matmul(psy[:], lhsT=at_[:, f * 128:(f + 1) * 128], rhs=wd_[:, f * 1024 + half * 512:f * 1024 + (half + 1) * 512],
                                                                                       start=(f == 0), stop=(f == 1)), reads=[at_.r(), wd_.r()], writes=[psy.r()])
            if half == 0:
                P.op("act", lambda e, psy=psy, ys=ys: e.copy(out=ys[:, 0:512], in_=psy[:]), reads=[psy.r()], writes=[ys.r(0)])
            else:
                P.op("dve", lambda e, psy=psy, ys=ys: e.tensor_copy(out=ys[:, 512:1024], in_=psy[:]), reads=[psy.r()], writes=[ys.r(1)])
        P.dma("act", lambda e, ys=ys, b=b: e.dma_start(out=yslots_d[b * 128:(b + 1) * 128, :], in_=ys[:]), reads=[ys.r(0), ys.r(1)], wacc=[r_yslots])

    for b in range(min(4, nblk_run)):
        front_end(b)
    for b in range(min(3, nblk_run)):
        load_blk(b)
    st_T(0)
    for b in range(nblk_run):
        if b + 4 < nblk_run:
            front_end(b + 4)
        if b + 3 < nblk_run:
            load_blk(b + 3)
        st_MM(b)
        if b + 1 < nblk_run:
            st_T(b + 1)
        st_T2(b)
        if b >= 1:
            st_MM2(b - 1)
    st_MM2(nblk_run - 1)

    A.release(m_blk)
    gbs = [[A.alloc(f"gb{s_}_{i}", [128, D], F32, at=(hT_off if s_ == 0 else mixedT_off) + i * 4096) for i in range(8)] for s_ in range(2)]
    ixgs = [[A.alloc(f"ixg{s_}_{j}", [128, 1], I32) for j in range(8)] for s_ in range(2)]
    for c in range(NT if stage >= 99 else 0):
        gb, ixg = gbs[c % 2], ixgs[c % 2]
        for j in range(8):
            P.op("dve", lambda e, c=c, j=j, ixg=ixg: e.tensor_copy(out=ixg[j][:], in_=d8i[:, c, j:j + 1]), reads=[d8i.r(c)], writes=[ixg[j].r()])
            P.dma("pool", lambda e, j=j, gb=gb, ixg=ixg: e.indirect_dma_start(out=gb[j][:], out_offset=None, in_=yslots_d, in_offset=IOA(ap=ixg[j][:, :], axis=0), bounds_check=breg(e, NSLOT - 1), oob_is_err=False),
                  reads=[ixg[j].r(), r_yslots], writes=[gb[j].r()])
        for (a_, b_, eng) in ((0, 1, "dve"), (2, 3, "pool"), (4, 5, "dve"), (6, 7, "pool"), (0, 2, "dve"), (4, 6, "pool"), (0, 4, "dve")):
            P.op(eng, lambda e, a_=a_, b_=b_, gb=gb: e.tensor_tensor(out=gb[a_][:], in0=gb[a_][:], in1=gb[b_][:], op=ALU.add), reads=[gb[a_].r(), gb[b_].r()], writes=[gb[a_].r()])
        P.op("dve", lambda e, gb=gb: e.tensor_tensor(out=gb[0][:], in0=gb[0][:], in1=g12[:, 1, :], op=ALU.mult), reads=[gb[0].r(), g12.r(1)], writes=[gb[0].r()])
        P.op("pool", lambda e, c=c, gb=gb: e.tensor_tensor(out=acc[:, c, :], in0=acc[:, c, :], in1=gb[0][:], op=ALU.add), reads=[gb[0].r(), acc.r((c, 0)), acc.r((c, 1))], writes=[acc.r((c, 0)), acc.r((c, 1))])

    fgb = A.alloc("fgb", [128, D], F32)
    P.dma("sp", lambda e: e.dma_start(out=fgb[:], in_=bass.AP(fg_d.tensor, fg_d.offset, [[0, 128], [1, D]])), writes=[fgb.r()])
    otl = [A.alloc(f"otl{i}", [128, D], F32) for i in range(2)]
    out_v = out_d.rearrange("(n p) d -> p n d", p=128)
    for c in range(NT):
        accr = [acc.r((c, 0)), acc.r((c, 1))]
        P.op("act", lambda e, c=c: e.activation(out=sq[:], in_=acc[:, c, :], func=AF.Square, accum_out=ssq[:, c:c + 1]), reads=accr, writes=[sq.r(), ssq.r(c // 4)])
        rs = rstd[:, c:c + 1]
        P.op("dve", lambda e, rs=rs, c=c: e.tensor_scalar(out=rs, in0=ssq[:, c:c + 1], scalar1=1.0 / D, scalar2=EPS, op0=ALU.mult, op1=ALU.add), reads=[ssq.r(c // 4)], writes=[rstd.r(c // 4)])
        P.op("act", lambda e, rs=rs: e.activation(out=rs, in_=rs, func=AF.Sqrt), reads=[rstd.r(c // 4)], writes=[rstd.r(c // 4)])
        P.op("dve", lambda e, rs=rs: e.reciprocal(out=rs, in_=rs), reads=[rstd.r(c // 4)], writes=[rstd.r(c // 4)])
        ot = otl[c % 2]
        P.op("dve", lambda e, ot=ot, rs=rs, c=c: e.scalar_tensor_tensor(out=ot[:], in0=acc[:, c, :], scalar=rs, in1=fgb[:], op0=ALU.mult, op1=ALU.mult),
             reads=accr + [rstd.r(c // 4), fgb.r()], writes=[ot.r()])
        P.dma("sp", lambda e, ot=ot, c=c: e.dma_start(out=out_v[:, c, :], in_=ot[:]), reads=[ot.r()])

    P.wait_all_dma("sp")
    P.build()
    return nc


def make_consts():
    c = np.zeros((128, 1024), np.float32)
    c[:, 0:128] = np.eye(128, dtype=np.float32)
    k = np.arange(128)
    c[k, 128 + (k + 64) % 128] = 1.0
    c[:64, 256] = 1.0
    c[64:, 256] = -1.0
    c[:64, 257] = -1.0
    c[64:, 257] = 1.0
    for gl in range(8):
        c[gl * 16:(gl + 1) * 16, 264 + gl] = 1.0
    c[:, 272:283] = (2.0 ** np.arange(11))[None, :]
    oh = np.zeros((32, 8), np.float32)
    oh[np.arange(32), np.arange(32) % 8] = 1.0
    c[:, 288:544] = oh.reshape(1, 256)
    j = np.arange(128)[:, None]
    i = np.arange(128)[None, :]
    c[:, 544:672] = (j <= i).astype(np.float32)
    c[:, 680:696] = (128.0 * np.arange(16))[None, :]
    c[:, 704:832] = (j < i).astype(np.float32)
    c[:, 840] = np.arange(128)
    return c


PER_BATCH = ("x", "c")
SHARED = ("ada_w", "ada_b", "norm1_g", "w_in", "s5_lam_re", "s5_lam_im", "s5_log_dt", "s5_b_re", "s5_b_im", "s5_c_re", "s5_c_im", "s5_d", "s5_w_glu", "s5_b_glu", "w_proj_a", "gla_w_gk2", "gla_b_gk2", "gla_norm_g", "w_proj_b", "w_out", "norm2_g", "router_w", "router_bias")
RESHAPE = {"s5_c_re": (512, 64), "s5_c_im": (512, 64), "s5_d": (512,)}


def make_inmaps(inputs, cores=range(8), n_exp=NEXP + 1):
    consts = make_consts()
    shared = {k: np.ascontiguousarray(np.asarray(inputs[k])[0]) for k in SHARED}
    for k, shp in RESHAPE.items():
        shared[k] = shared[k].reshape(shp)
    shared["final_g"] = np.ascontiguousarray(np.asarray(inputs["final_g"]))
    if "exp_w_gate" in inputs:
        for k, ks in (("exp_w_gate", "sh_w_gate"), ("exp_w_up", "sh_w_up"), ("exp_w_down", "sh_w_down")):
            full = np.concatenate([np.asarray(inputs[k])[0], np.asarray(inputs[ks])], axis=0) if n_exp == NEXP + 1 else np.asarray(inputs[k])[0]
            full = full[:n_exp]
            ne, kd, nn = full.shape
            shared[k] = np.ascontiguousarray(full.reshape(ne, kd // 128, 128, nn).transpose(0, 2, 1, 3)).reshape(ne * 128, (kd // 128) * nn)
    maps = []
    for b in cores:
        m = {k: np.ascontiguousarray(np.asarray(inputs[k])[b]) for k in PER_BATCH}
        m.update(shared)
        m["consts"] = consts
        maps.append(m)
    return maps


def kernel(**inputs):
    nc = build_program()
    res = run_bass_kernel_spmd(nc, make_inmaps(inputs), core_ids=list(range(8)))
    return np.stack([np.asarray(r["out"]) for r in res.results], axis=0)
```

```python
import numpy as np
import concourse.bass as bass
import concourse.mybir as mybir
from concourse.bass_utils import run_bass_kernel_spmd

F32 = mybir.dt.float32
BF16 = mybir.dt.bfloat16
I32 = mybir.dt.int32
AF = mybir.ActivationFunctionType
ALU = mybir.AluOpType
AX = mybir.AxisListType
DTSIZE = {F32: 4, BF16: 2, I32: 4}

COMPUTE = ("pe", "act", "dve", "pool")
NDMASEM = 16

D = 1024
L = 2048
NT = 16
EPS = 1e-6
S5W = 512
NG = 32
IN_W = 4112
OFF_U, OFF_Q, OFF_K, OFF_V, OFF_GK, OFF_R, OFF_GA, OFF_GB = 0, 512, 768, 1024, 1536, 1552, 2064, 3088
NEXP = 256
TWO_PI = 6.283185307179586


class Reg:
    __slots__ = ("name", "lw", "rd")

    def __init__(self, name, inherit=None):
        self.name = name
        self.lw = {}
        self.rd = dict(inherit) if inherit else {}


class Prog:
    def __init__(self, nc):
        self.nc = nc
        self.q = {e: [] for e in COMPUTE + ("sp",)}
        self.cnt = {e: 0 for e in COMPUTE}
        self.known = {e: {} for e in COMPUTE + ("sp",)}
        self.targets = {e: set() for e in COMPUTE}
        self.ndma = {e: 0 for e in ("sp", "act", "pool")}
        self.sems = {}
        self.dmasems = {}

    def _deps(self, eng, reads, writes, wacc=()):
        deps = {}
        for r in reads:
            for k, v in r.lw.items():
                if deps.get(k, 0) < v:
                    deps[k] = v
        for w in writes:
            for k, v in w.lw.items():
                if deps.get(k, 0) < v:
                    deps[k] = v
            for k, v in w.rd.items():
                if deps.get(k, 0) < v:
                    deps[k] = v
        for w in wacc:
            for k, v in w.rd.items():
                if deps.get(k, 0) < v:
                    deps[k] = v
        waits = []
        kn = self.known[eng]
        for k, v in deps.items():
            if k == "pe" and eng == "pe" and not getattr(self, "pe_selfsync", False):
                continue
            if kn.get(k, 0) >= v:
                continue
            kn[k] = v
            waits.append((k, v))
            if k in COMPUTE:
                self.targets[k].add(v)
        return waits

    def _mark(self, tok, reads, writes, wacc=()):
        k, v = tok
        for r in reads:
            if r.rd.get(k, 0) < v:
                r.rd[k] = v
        for w in writes:
            w.lw = {k: v}
            w.rd = {}
        for w in wacc:
            if w.lw.get(k, 0) < v:
                w.lw[k] = v

    def op(self, eng, fn, reads=(), writes=(), wacc=(), pmode="full"):
        waits = self._deps(eng, reads, writes, wacc)
        if eng == "pe":
            if pmode != getattr(self, "last_pmode", "full") and self.cnt["pe"] > 0:
                prev = self.cnt["pe"]
                if self.known["pe"].get("pe", 0) < prev:
                    self.known["pe"]["pe"] = prev
                    waits.append(("pe", prev))
                    self.targets["pe"].add(prev)
            self.last_pmode = pmode
        self.cnt[eng] += 1
        idx = self.cnt[eng]
        self.q[eng].append(("op", waits, fn, idx))
        self._mark((eng, idx), reads, writes, wacc)
        return idx

    def dma(self, eng, fn, reads=(), writes=(), wacc=()):
        waits = self._deps(eng, reads, writes, wacc)
        d = self.ndma[eng]
        self.ndma[eng] += 1
        si = d % NDMASEM
        key = ("dma", eng, si)
        val = 16 * (d // NDMASEM + 1)
        if d >= NDMASEM:
            pv = val - 16
            if self.known[eng].get(key, 0) < pv:
                self.known[eng][key] = pv
                waits.append((key, pv))
        self.q[eng].append(("dma", waits, fn, (key, val)))
        self._mark((key, val), reads, writes, wacc)
        return (key, val)

    def wait_all_dma(self, eng):
        waits = []
        for qe, n in self.ndma.items():
            for si in range(min(n, NDMASEM)):
                cntsi = (n - 1 - si) // NDMASEM + 1
                waits.append((("dma", qe, si), 16 * cntsi))
        self.q[eng].append(("wait", waits, None, None))

    def build(self):
        nc = self.nc
        for e in COMPUTE:
            self.sems[e] = nc.alloc_semaphore(name=f"sem_{e}")
        for qe, n in self.ndma.items():
            for si in range(min(n, NDMASEM)):
                self.dmasems[("dma", qe, si)] = nc.alloc_semaphore(name=f"dsem_{qe}_{si}")
        rank = {}
        for e in COMPUTE:
            rank[e] = {v: i + 1 for i, v in enumerate(sorted(self.targets[e]))}
        handles = {"pe": "tensor", "act": "scalar", "dve": "vector", "pool": "gpsimd", "sp": "sync"}

        def replay(e, h):
            for kind, waits, fn, tok in self.q[e]:
                for k, v in waits:
                    if k in COMPUTE:
                        h.wait_ge(self.sems[k], rank[k][v])
                    else:
                        h.wait_ge(self.dmasems[k], v)
                if kind == "op":
                    ins = fn(h)
                    if tok in rank[e]:
                        ins.then_inc(self.sems[e], 1)
                elif kind == "dma":
                    ins = fn(h)
                    ins.then_inc(self.dmasems[tok[0]], 16)

        with nc.Block() as block:
            for e in COMPUTE + ("sp",):
                if not self.q[e]:
                    continue

                def mk(e):
                    def f(h):
                        replay(e, h)
                    return f
                getattr(block, handles[e])(mk(e))


class Tile:
    def __init__(self, t, name, inherit):
        self.t = t
        self.name = name
        self.inherit = inherit
        self.regs = {}

    def r(self, key=None):
        if key not in self.regs:
            self.regs[key] = Reg(f"{self.name}:{key}", self.inherit)
        return self.regs[key]

    def __getitem__(self, idx):
        return self.t[idx]


class Arena:
    def __init__(self, nc, base=16512, top=229344):
        self.nc = nc
        self.ptr = base
        self.top = top
        self.hist = []
        self.n = 0

    def alloc(self, name, shape, dt, at=None):
        size = int(np.prod(shape[1:])) * DTSIZE[dt]
        size = (size + 31) // 32 * 32
        if at is None:
            start, end = self.ptr, self.ptr + size
            self.ptr = end
        else:
            start, end = at, at + size
        assert end <= self.top, f"SBUF overflow allocating {name}: {end} > {self.top}"
        self.n += 1
        t = self.nc.alloc_sbuf_tensor_at(f"{name}_{self.n}", list(shape), dt, offset=start)
        inherit = {}
        keep = []
        for (s, e, old) in self.hist:
            if s < end and start < e:
                for rg in old.regs.values():
                    for k, v in rg.lw.items():
                        inherit[k] = max(inherit.get(k, 0), v)
                    for k, v in rg.rd.items():
                        inherit[k] = max(inherit.get(k, 0), v)
                for k, v in old.inherit.items():
                    inherit[k] = max(inherit.get(k, 0), v)
            keep.append((s, e, old))
        T = Tile(t, name, inherit)
        self.hist = keep + [(start, end, T)]
        return T

    def mark(self):
        return self.ptr

    def release(self, m):
        self.ptr = m


def bcast_free(ap, n):
    return bass.AP(ap.tensor, ap.offset, [list(ap.ap[0]), [0, n]])


def build_program(stage=99, n_exp=NEXP + 1):
    nc = bass.Bass("TRN2", target_bir_lowering=False)
    P = Prog(nc)
    A = Arena(nc)

    def din(name, shape, dt=F32):
        return nc.dram_tensor(name, list(shape), dt, kind="ExternalInput").ap()

    x_d = din("x", [L, D])
    c_d = din("c", [D])
    adaw_d = din("ada_w", [D, 6 * D])
    adab_d = din("ada_b", [6 * D])
    n1g_d = din("norm1_g", [D])
    win_d = din("w_in", [D, IN_W])
    consts_d = din("consts", [128, 1024])
    lamre_d = din("s5_lam_re", [NG, 64])
    lamim_d = din("s5_lam_im", [NG, 64])
    logdt_d = din("s5_log_dt", [NG])
    bre_d = din("s5_b_re", [NG, 64, 16])
    bim_d = din("s5_b_im", [NG, 64, 16])
    cre_d = din("s5_c_re", [NG * 16, 64])
    cim_d = din("s5_c_im", [NG * 16, 64])
    s5d_d = din("s5_d", [S5W])
    wglu_d = din("s5_w_glu", [S5W, S5W])
    bglu_d = din("s5_b_glu", [S5W])
    wpa_d = din("w_proj_a", [S5W, D])
    wgk2_d = din("gla_w_gk2", [16, 256])
    bgk2_d = din("gla_b_gk2", [256])
    gng_d = din("gla_norm_g", [128])
    wpb_d = din("w_proj_b", [512, D])
    wout_d = din("w_out", [D, D])
    n2g_d = din("norm2_g", [D])
    rw_d = din("router_w", [D, NEXP])
    rb_d = din("router_bias", [NEXP])
    if stage >= 7:
        ewg_d = din("exp_w_gate", [n_exp * 128, 2048])
        ewu_d = din("exp_w_up", [n_exp * 128, 2048])
        ewd_d = din("exp_w_down", [n_exp * 128, 2048])
    fg_d = din("final_g", [D])
    out_d = nc.dram_tensor("out", [L, D], F32, kind="ExternalOutput").ap()
    dbg = {}

    def dbg_out(name, shape, dt=F32):
        dbg[name] = nc.dram_tensor(name, list(shape), dt, kind="ExternalOutput").ap()
        return dbg[name]

    banks = []
    for i in range(8):
        pt = nc.alloc_psum_tensor(f"bank{i}", [128, 512], F32)
        banks.append(Tile(pt, f"bank{i}", {}))
    bank_i = [0]

    def bank():
        b = banks[bank_i[0] % 8]
        bank_i[0] += 1
        return b

    cst = A.alloc("cst", [128, 1024], F32)
    P.dma("sp", lambda e: e.dma_start(out=cst[:], in_=consts_d), writes=[cst.r()])
    identf = cst[:, 0:128]
    identb_t = A.alloc("identb", [128, 128], BF16)
    P.op("dve", lambda e: e.tensor_copy(out=identb_t[:], in_=identf), reads=[cst.r()], writes=[identb_t.r()])

    cT = A.alloc("cT", [128, 8], F32)
    P.dma("sp", lambda e: e.dma_start(out=cT[:], in_=c_d.rearrange("(p k) -> p k", k=8)), writes=[cT.r()])
    scT = A.alloc("scT", [128, 8], F32)
    P.op("act", lambda e: e.activation(out=scT[:], in_=cT[:], func=AF.Silu), reads=[cT.r()], writes=[scT.r()])
    colp = A.alloc("colp", [128, 8, 8], F32)
    P.dma("sp", lambda e: e.dma_start(out=colp[:, 0, :], in_=n1g_d.rearrange("(k p) -> p k", p=128), allow_slow_non_contiguous=True), writes=[colp.r()])
    P.dma("sp", lambda e: e.dma_start(out=colp[:, 3, :], in_=n2g_d.rearrange("(k p) -> p k", p=128), allow_slow_non_contiguous=True), writes=[colp.r()])
    for j, off in ((1, 0), (2, D), (4, 3 * D), (5, 4 * D)):
        P.dma("sp", (lambda j, off: lambda e: e.dma_start(
            out=colp[:, j, :], in_=adab_d[off:off + D].rearrange("(k p) -> p k", p=128), allow_slow_non_contiguous=True))(j, off), writes=[colp.r()])
    modcol = A.alloc("modcol", [128, 4, 8], F32)
    g12 = A.alloc("g12", [128, 2, 1024], F32)
    m_ada = A.mark()
    rowb = A.alloc("rowb", [128, 2, 1024], F32)
    for j, off in ((0, 2 * D), (1, 5 * D)):
        src = adab_d[off:off + D]
        srcb = bass.AP(src.tensor, src.offset, [[0, 128], [1, D]])
        P.dma("sp", (lambda j, srcb: lambda e: e.dma_start(out=rowb[:, j, :], in_=srcb))(j, srcb), writes=[rowb.r(j)])
    adaw_v = adaw_d.rearrange("(p k) n -> p k n", k=8)
    colidx = {0: 0, 1: 1, 3: 2, 4: 3}
    rowidx = {2: 0, 5: 1}
    wblk = [A.alloc(f"adaw{i}", [128, 8, 512], F32) for i in range(2)]
    nb = 0
    for blk in range(6):
        for half in range(2):
            wb = wblk[nb % 2]
            nb += 1
            n0 = blk * 1024 + half * 512
            P.dma("sp", (lambda wb, n0: lambda e: e.dma_start(out=wb[:], in_=adaw_v[:, :, n0:n0 + 512]))(wb, n0),
                  writes=[wb.r()])
            if blk in rowidx:
                ps = bank()
                for k in range(8):
                    P.op("pe", (lambda ps, wb, k: lambda e: e.matmul(
                        ps[:], lhsT=bcast_free(scT[:, k:k + 1], 128), rhs=wb[:, k, :], start=(k == 0), stop=(k == 7)))(ps, wb, k),
                        reads=[scT.r(), wb.r()], writes=[ps.r()], pmode="f32")
                j = rowidx[blk]
                P.op("dve", (lambda ps, j, half: lambda e: e.tensor_tensor(
                    out=g12[:, j, half * 512:(half + 1) * 512], in0=ps[:], in1=rowb[:, j, half * 512:(half + 1) * 512], op=ALU.add))(ps, j, half),
                    reads=[ps.r(), rowb.r(j)], writes=[g12.r(j)])
            else:
                ps = bank()
                j = colidx[blk]
                for cc in range(4):
                    for k in range(8):
                        P.op("pe", (lambda ps, wb, cc, k: lambda e: e.matmul(
                            ps[:, cc:cc + 1], lhsT=wb[:, k, cc * 128:(cc + 1) * 128], rhs=scT[:, k:k + 1],
                            start=(k == 0), stop=(k == 7)))(ps, wb, cc, k),
                            reads=[scT.r(), wb.r()], writes=[ps.r()], pmode="f32")
                jb = {0: 1, 1: 2, 2: 4, 3: 5}[j]
                P.op("dve", (lambda ps, j, jb, half: lambda e: e.tensor_tensor(
                    out=modcol[:, j, half * 4:(half + 1) * 4], in0=ps[:, 0:4], in1=colp[:, jb, half * 4:(half + 1) * 4], op=ALU.add))(ps, j, jb, half),
                    reads=[ps.r(), colp.r()], writes=[modcol.r()])
    A.release(m_ada)
    AB = A.alloc("AB", [128, 2, 8], F32)
    for j, (jg, jsc) in enumerate(((0, 1), (3, 3))):
        P.op("dve", (lambda j, jg, jsc: lambda e: e.scalar_tensor_tensor(
            out=AB[:, j, :], in0=modcol[:, jsc, :], scalar=1.0, in1=colp[:, jg, :], op0=ALU.add, op1=ALU.mult))(j, jg, jsc),
            reads=[modcol.r(), colp.r()], writes=[AB.r()])

    if stage == 0:
        o1 = dbg_out("d_modcol", [128, 32]); o2 = dbg_out("d_g12", [128, 2048]); o3 = dbg_out("d_AB", [128, 16])
        P.dma("sp", lambda e: e.dma_start(out=o1, in_=modcol[:].rearrange("p a b -> p (a b)")), reads=[modcol.r()])
        P.dma("sp", lambda e: e.dma_start(out=o2, in_=g12[:].rearrange("p a b -> p (a b)")), reads=[g12.r(0), g12.r(1)])
        P.dma("sp", lambda e: e.dma_start(out=o3, in_=AB[:].rearrange("p a b -> p (a b)")), reads=[AB.r()])
        P.wait_all_dma("sp")
        P.build()
        return nc


    import math

    def fv(tile, col0, dims, p0=0, np_=128):
        a = tile[p0:p0 + np_, col0:col0 + 1]
        return bass.AP(a.tensor, a.offset, [list(a.ap[0])] + [[st, ct] for st, ct in dims])

    ev_i = [0]

    def evac_copy(out_ap, in_ap, reads, writes):
        ev_i[0] += 1
        if ev_i[0] % 2:
            P.op("dve", lambda e: e.tensor_copy(out=out_ap, in_=in_ap), reads=reads, writes=writes)
        else:
            P.op("act", lambda e: e.copy(out=out_ap, in_=in_ap), reads=reads, writes=writes)

    sq = A.alloc("sq", [128, D], F32)
    ssq = A.alloc("ssq", [128, 16], F32)
    rstd = A.alloc("rstd", [128, 16], F32)
    hT_off = A.mark()
    hT = A.alloc("hT", [128, 8, L], BF16)
    hT_regs = lambda n: [hT.r((k, n)) for k in range(8)]
    m_B = A.mark()
    xin = [A.alloc(f"xin{i}", [128, 4, D], F32) for i in range(2)]
    xnb = [A.alloc(f"xnb{i}", [128, 4, D], BF16) for i in range(2)]
    x_v = x_d.rearrange("(n p) d -> p n d", p=128)
    for blk in range(4):
        xi = xin[blk % 2]
        xb = xnb[blk % 2]
        P.dma("sp", lambda e, xi=xi, blk=blk: e.dma_start(out=xi[:], in_=x_v[:, blk * 4:(blk + 1) * 4, :]), writes=[xi.r()])
        for j in range(4):
            P.op("act", lambda e, xi=xi, j=j, blk=blk: e.activation(out=sq[:], in_=xi[:, j, :], func=AF.Square,
                                                                     accum_out=ssq[:, blk * 4 + j:blk * 4 + j + 1]),
                 reads=[xi.r()], writes=[sq.r(), ssq.r(blk)])
        rs = rstd[:, blk * 4:(blk + 1) * 4]
        P.op("dve", lambda e, rs=rs, blk=blk: e.tensor_scalar(out=rs, in0=ssq[:, blk * 4:(blk + 1) * 4], scalar1=1.0 / D, scalar2=EPS,
                                                               op0=ALU.mult, op1=ALU.add), reads=[ssq.r(blk)], writes=[rstd.r(blk)])
        P.op("act", lambda e, rs=rs: e.activation(out=rs, in_=rs, func=AF.Sqrt), reads=[rstd.r(blk)], writes=[rstd.r(blk)])
        P.op("dve", lambda e, rs=rs: e.reciprocal(out=rs, in_=rs), reads=[rstd.r(blk)], writes=[rstd.r(blk)])
        for j in range(4):
            P.op("act", lambda e, xi=xi, xb=xb, j=j, blk=blk: e.activation(out=xb[:, j, :], in_=xi[:, j, :], func=AF.Copy,
                                                                           scale=rstd[:, blk * 4 + j:blk * 4 + j + 1]),
                 reads=[xi.r(), rstd.r(blk)], writes=[xb.r(j)])
        for k in range(8):
            ps = bank()
            psb = ps.t[:].bitcast(BF16)
            for j in range(4):
                P.op("pe", lambda e, psb=psb, xb=xb, j=j, k=k: e.transpose(psb[:, j * 128:(j + 1) * 128], xb[:, j, k * 128:(k + 1) * 128], identb_t[:]),
                     reads=[xb.r(j), identb_t.r()], writes=[ps.r()])
            P.op("dve", lambda e, psb=psb, k=k, blk=blk: e.tensor_scalar(
                out=hT[:, k, blk * 512:(blk + 1) * 512], in0=psb[:, 0:512], scalar1=AB[:, 0, k:k + 1], scalar2=modcol[:, 0, k:k + 1],
                op0=ALU.mult, op1=ALU.add), reads=[ps.r(), AB.r(), modcol.r()], writes=[hT.r((k, blk))])
    A.release(m_B)

    if stage == 1:
        o1 = dbg_out("d_hT", [128, 8 * L], BF16)
        P.dma("sp", lambda e: e.dma_start(out=o1, in_=hT[:].rearrange("p a b -> p (a b)")), reads=[hT.r((k, n)) for k in range(8) for n in range(4)])
        P.wait_all_dma("sp")
        P.build()
        return nc

    m_C = A.mark()
    mixedT_off = A.mark()
    mixedT = A.alloc("mixedT", [128, 8, L], BF16)
    m_C2 = A.mark()
    win_v = win_d.rearrange("(k p) n -> p k n", p=128)
    wu = A.alloc("wu", [128, 8, S5W], BF16)
    P.dma("pool", lambda e: e.dma_start(out=wu[:], in_=win_v[:, :, OFF_U:OFF_U + S5W]), writes=[wu.r()])
    dcol = A.alloc("dcol", [128, 8], F32)
    P.dma("sp", lambda e: e.dma_start(out=dcol[:, 0:4], in_=s5d_d.rearrange("(k p) -> p k", p=128), allow_slow_non_contiguous=True), writes=[dcol.r()])
    P.dma("sp", lambda e: e.dma_start(out=dcol[:, 4:8], in_=bglu_d.rearrange("(k p) -> p k", p=128), allow_slow_non_contiguous=True), writes=[dcol.r()])

    uT = A.alloc("uT", [128, 4, L], BF16)
    for m in range(4):
        for n in range(4):
            ps = bank()
            for k in range(8):
                P.op("pe", lambda e, ps=ps, m=m, n=n, k=k: e.matmul(ps[:], lhsT=wu[:, k, m * 128:(m + 1) * 128], rhs=hT[:, k, n * 512:(n + 1) * 512],
                                                                   start=(k == 0), stop=(k == 7)),
                     reads=[wu.r(), hT.r((k, n))], writes=[ps.r()])
            evac_copy(uT[:, m, n * 512:(n + 1) * 512], ps[:], [ps.r()], [uT.r((m, n))])

    NE = 11
    NW = 32 * NE
    PR = A.alloc("PR", [128, NW], F32)
    PIs = A.alloc("PIs", [128, NW], F32)
    BT = A.alloc("BT", [128, 32, 128], BF16)
    Cpad = A.alloc("Cpad", [128, 32 * 128], F32)
    yT = A.alloc("yT", [128, 4, L], BF16)
    m_tmp = A.mark()
    lam_in = A.alloc("lam_in", [32, 2, 128], F32)
    for j, src in enumerate((lamre_d, lamim_d)):
        for hh in range(2):
            P.dma("sp", lambda e, j=j, hh=hh, src=src: e.dma_start(out=lam_in[:, j, hh * 64:(hh + 1) * 64], in_=src), writes=[lam_in.r()])
    LRLI = A.alloc("LRLI", [128, 64], F32)
    for j in range(2):
        ps = bank()
        P.op("pe", lambda e, ps=ps, j=j: e.transpose(ps[:, 0:32], lam_in[:, j, :], cst[0:32, 0:32]), reads=[lam_in.r(), cst.r()], writes=[ps.r()], pmode="k32")
        P.op("dve", lambda e, ps=ps, j=j: e.tensor_copy(out=LRLI[:, j * 32:(j + 1) * 32], in_=ps[:, 0:32]), reads=[ps.r()], writes=[LRLI.r()])
    DT = A.alloc("DT", [128, 32], F32)
    P.dma("sp", lambda e: e.dma_start(out=DT[:], in_=bass.AP(logdt_d.tensor, logdt_d.offset, [[0, 128], [1, 32]])), writes=[DT.r()])
    P.op("act", lambda e: e.activation(out=DT[:], in_=DT[:], func=AF.Exp), reads=[DT.r()], writes=[DT.r()])
    LDT = A.alloc("LDT", [128, 64], F32)
    for j in range(2):
        P.op("dve", lambda e, j=j: e.tensor_tensor(out=LDT[:, j * 32:(j + 1) * 32], in0=LRLI[:, j * 32:(j + 1) * 32], in1=DT[:], op=ALU.mult),
             reads=[LRLI.r(), DT.r()], writes=[LDT.r()])
    E_b = fv(cst, 272, [(0, 32), (1, NE)])
    ARG = A.alloc("ARG", [128, NW], F32)
    ANG = A.alloc("ANG", [128, NW], F32)
    P.op("dve", lambda e: e.tensor_tensor(out=fv(ARG, 0, [(NE, 32), (1, NE)]), in0=fv(LDT, 0, [(1, 32), (0, NE)]), in1=E_b, op=ALU.mult),
         reads=[LDT.r(), cst.r()], writes=[ARG.r()])
    P.op("dve", lambda e: e.tensor_tensor(out=fv(ANG, 0, [(NE, 32), (1, NE)]), in0=fv(LDT, 32, [(1, 32), (0, NE)]), in1=E_b, op=ALU.mult),
         reads=[LDT.r(), cst.r()], writes=[ANG.r()])
    MAG = A.alloc("MAG", [128, NW], F32)
    P.op("act", lambda e: e.activation(out=MAG[:], in_=ARG[:], func=AF.Exp), reads=[ARG.r()], writes=[MAG.r()])
    tmpf = A.alloc("tmpf", [128, NW], F32)
    tmpm = A.alloc("tmpm", [128, NW], F32)
    tmpi = A.alloc("tmpi", [128, NW], I32)
    SIN = A.alloc("SIN", [128, NW], F32)
    COS = A.alloc("COS", [128, NW], F32)

    def sin_of(shift, out_t):
        rt = [tmpf.r()]
        P.op("dve", lambda e: e.tensor_scalar(out=tmpf[:], in0=ANG[:], scalar1=shift, scalar2=1.0 / TWO_PI, op0=ALU.add, op1=ALU.mult),
             reads=[ANG.r()], writes=rt)
        P.op("dve", lambda e: e.tensor_copy(out=tmpi[:], in_=tmpf[:]), reads=rt, writes=[tmpi.r()])
        P.op("dve", lambda e: e.tensor_copy(out=tmpf[:], in_=tmpi[:]), reads=[tmpi.r()], writes=rt)
        P.op("dve", lambda e: e.scalar_tensor_tensor(out=tmpf[:], in0=tmpf[:], scalar=-TWO_PI, in1=ANG[:], op0=ALU.mult, op1=ALU.add),
             reads=rt + [ANG.r()], writes=rt)
        if shift != 0.0:
            P.op("dve", lambda e: e.tensor_scalar(out=tmpf[:], in0=tmpf[:], scalar1=shift, scalar2=None, op0=ALU.add), reads=rt, writes=rt)
        P.op("dve", lambda e: e.tensor_scalar(out=tmpm[:], in0=tmpf[:], scalar1=math.pi, scalar2=-TWO_PI, op0=ALU.is_gt, op1=ALU.mult),
             reads=rt, writes=[tmpm.r()])
        P.op("dve", lambda e: e.tensor_tensor(out=tmpf[:], in0=tmpf[:], in1=tmpm[:], op=ALU.add), reads=rt + [tmpm.r()], writes=rt)
        P.op("dve", lambda e: e.tensor_scalar(out=tmpm[:], in0=tmpf[:], scalar1=-math.pi, scalar2=TWO_PI, op0=ALU.is_lt, op1=ALU.mult),
             reads=rt, writes=[tmpm.r()])
        P.op("dve", lambda e: e.tensor_tensor(out=tmpf[:], in0=tmpf[:], in1=tmpm[:], op=ALU.add), reads=rt + [tmpm.r()], writes=rt)
        P.op("dve", lambda e: e.tensor_scalar(out=tmpf[:], in0=tmpf[:], scalar1=3.14159, scalar2=-3.14159, op0=ALU.min, op1=ALU.max), reads=rt, writes=rt)
        P.op("act", lambda e: e.activation(out=out_t[:], in_=tmpf[:], func=AF.Sin), reads=rt, writes=[out_t.r()])

    sin_of(0.0, SIN)
    sin_of(math.pi / 2, COS)
    PIu = A.alloc("PIu", [128, NW], F32)
    P.op("dve", lambda e: e.tensor_tensor(out=PR[:], in0=MAG[:], in1=COS[:], op=ALU.mult), reads=[MAG.r(), COS.r()], writes=[PR.r()])
    P.op("dve", lambda e: e.tensor_tensor(out=PIu[:], in0=MAG[:], in1=SIN[:], op=ALU.mult), reads=[MAG.r(), SIN.r()], writes=[PIu.r()])
    P.op("dve", lambda e: e.tensor_scalar(out=PIs[:], in0=PIu[:], scalar1=cst[:, 256:257], scalar2=None, op0=ALU.mult), reads=[PIu.r(), cst.r()], writes=[PIs.r()])
    cf = A.alloc("cf", [128, 256], F32)
    ar = fv(PR, 0, [(NE, 32)])
    ai = fv(PIu, 0, [(NE, 32)])
    lr_ = LRLI[:, 0:32]
    li_ = LRLI[:, 32:64]
    cfr = [cf.r()]
    P.op("dve", lambda e: e.tensor_scalar(out=cf[:, 0:32], in0=ar, scalar1=-1.0, scalar2=None, op0=ALU.add), reads=[PR.r()], writes=cfr)
    P.op("dve", lambda e: e.tensor_tensor(out=cf[:, 32:64], in0=lr_, in1=lr_, op=ALU.mult), reads=[LRLI.r()], writes=cfr)
    P.op("dve", lambda e: e.tensor_tensor(out=cf[:, 64:96], in0=li_, in1=li_, op=ALU.mult), reads=[LRLI.r()], writes=cfr)
    P.op("dve", lambda e: e.tensor_tensor(out=cf[:, 32:64], in0=cf[:, 32:64], in1=cf[:, 64:96], op=ALU.add), reads=cfr, writes=cfr)
    P.op("dve", lambda e: e.reciprocal(out=cf[:, 32:64], in_=cf[:, 32:64]), reads=cfr, writes=cfr)
    P.op("dve", lambda e: e.tensor_tensor(out=cf[:, 64:96], in0=cf[:, 0:32], in1=lr_, op=ALU.mult), reads=cfr + [LRLI.r()], writes=cfr)
    P.op("dve", lambda e: e.tensor_tensor(out=cf[:, 96:128], in0=ai, in1=li_, op=ALU.mult), reads=[PIu.r(), LRLI.r()], writes=cfr)
    P.op("dve", lambda e: e.tensor_tensor(out=cf[:, 64:96], in0=cf[:, 64:96], in1=cf[:, 96:128], op=ALU.add), reads=cfr, writes=cfr)
    P.op("dve", lambda e: e.tensor_tensor(out=cf[:, 128:160], in0=cf[:, 64:96], in1=cf[:, 32:64], op=ALU.mult), reads=cfr, writes=cfr)
    P.op("dve", lambda e: e.tensor_tensor(out=cf[:, 64:96], in0=ai, in1=lr_, op=ALU.mult), reads=[PIu.r(), LRLI.r()], writes=cfr)
    P.op("dve", lambda e: e.tensor_tensor(out=cf[:, 96:128], in0=cf[:, 0:32], in1=li_, op=ALU.mult), reads=cfr + [LRLI.r()], writes=cfr)
    P.op("dve", lambda e: e.tensor_tensor(out=cf[:, 64:96], in0=cf[:, 64:96], in1=cf[:, 96:128], op=ALU.subtract), reads=cfr, writes=cfr)
    P.op("dve", lambda e: e.tensor_tensor(out=cf[:, 160:192], in0=cf[:, 64:96], in1=cf[:, 32:64], op=ALU.mult), reads=cfr, writes=cfr)
    P.op("dve", lambda e: e.tensor_scalar(out=cf[:, 160:192], in0=cf[:, 160:192], scalar1=cst[:, 257:258], scalar2=None, op0=ALU.mult), reads=cfr + [cst.r()], writes=cfr)
    RB = A.alloc("RB", [128, 2, 32, 16], F32)
    bre_v = bre_d.rearrange("g p h -> p g h")
    bim_v = bim_d.rearrange("g p h -> p g h")
    for (p0, var, src) in ((0, 0, bre_v), (64, 0, bim_v), (0, 1, bim_v), (64, 1, bre_v)):
        P.dma("sp", lambda e, p0=p0, var=var, src=src: e.dma_start(out=RB[p0:p0 + 64, var, :, :], in_=src), writes=[RB.r()])
    B1 = A.alloc("B1", [128, 512], F32)
    B1v = fv(B1, 0, [(16, 32), (1, 16)])
    P.op("dve", lambda e: e.tensor_tensor(out=B1v, in0=RB[:, 0, :, :], in1=fv(cf, 4 * 32, [(1, 32), (0, 16)]), op=ALU.mult), reads=[RB.r()] + cfr, writes=[B1.r()])
    P.op("dve", lambda e: e.tensor_tensor(out=RB[:, 1, :, :], in0=RB[:, 1, :, :], in1=fv(cf, 5 * 32, [(1, 32), (0, 16)]), op=ALU.mult), reads=[RB.r()] + cfr, writes=[RB.r()])
    P.op("dve", lambda e: e.tensor_tensor(out=B1v, in0=B1v, in1=RB[:, 1, :, :], op=ALU.add), reads=[RB.r(), B1.r()], writes=[B1.r()])
    for ch in range(4):
        ps = bank()
        P.op("pe", lambda e, ps=ps, ch=ch: e.transpose(ps[:, 0:128], B1[:, ch * 128:(ch + 1) * 128], cst[:, 0:128]), reads=[B1.r(), cst.r()], writes=[ps.r()], pmode="f32")
        for gl in range(8):
            P.op("dve", lambda e, ps=ps, ch=ch, gl=gl: e.tensor_scalar(out=BT[:, ch * 8 + gl, :], in0=ps[:, 0:128], scalar1=cst[:, 264 + gl:265 + gl], scalar2=None, op0=ALU.mult),
                 reads=[ps.r(), cst.r()], writes=[BT.r()])
    Cin = A.alloc("Cin", [128, 4, 128], F32)
    P.dma("sp", lambda e: e.dma_start(out=Cin[:, :, 0:64], in_=cre_d.rearrange("(k r) p -> r k p", r=128)), writes=[Cin.r()])
    P.dma("sp", lambda e: e.dma_start(out=Cin[:, :, 64:128], in_=cim_d.rearrange("(k r) p -> r k p", r=128)), writes=[Cin.r()])
    CT1 = A.alloc("CT1", [128, 512], F32)
    for r4 in range(4):
        ps = bank()
        P.op("pe", lambda e, ps=ps, r4=r4: e.transpose(ps[:, 0:128], Cin[:, r4, :], cst[:, 0:128]), reads=[Cin.r(), cst.r()], writes=[ps.r()], pmode="f32")
        P.op("dve", lambda e, ps=ps, r4=r4: e.tensor_scalar(out=CT1[:, r4 * 128:(r4 + 1) * 128], in0=ps[:, 0:128], scalar1=cst[:, 256:257], scalar2=None, op0=ALU.mult),
             reads=[ps.r(), cst.r()], writes=[CT1.r()])
    P.op("dve", lambda e: e.tensor_tensor(out=fv(Cpad, 0, [(128, 32), (16, 8), (1, 16)]), in0=fv(CT1, 0, [(16, 32), (0, 8), (1, 16)]),
                                          in1=fv(cst, 288, [(8, 32), (1, 8), (0, 16)]), op=ALU.mult), reads=[CT1.r(), cst.r()], writes=[Cpad.r()])

    A.release(m_tmp)
    m_scan = A.mark()
    F32R = mybir.dt.float32r
    r32 = lambda ap: ap.bitcast(F32R)
    PAD = 1024
    identR = A.alloc("identR", [128, 128], F32)
    P.op("dve", lambda e: e.tensor_copy(out=r32(identR[:]), in_=cst[:, 0:128]), reads=[cst.r()], writes=[identR.r()])
    SAB = [A.alloc(f"S{i}", [128, PAD + L], F32) for i in range(2)]
    for t_ in SAB:
        P.op("dve", lambda e, t_=t_: e.tensor_scalar(out=r32(t_[:, 0:PAD]), in0=cst[:, 0:PAD], scalar1=0.0, scalar2=None, op0=ALU.mult), reads=[cst.r()], writes=[t_.r("pad")])
    Rt = [A.alloc(f"R{i}", [128, NE, 128], F32) for i in range(2)]
    ytmp = [A.alloc(f"ytmp{i}", [128, 512], F32) for i in range(2)]
    ybanks = banks[4:8]
    rot_i = [0]

    def rbank():
        b = banks[rot_i[0] % 4]
        rot_i[0] += 1
        return b

    def sregs(t_, lo):
        rr = []
        if lo < 0:
            rr.append(t_.r("pad"))
        for n in range(4):
            if lo < (n + 1) * 512 and n * 512 < lo + 512:
                rr.append(t_.r(n))
        return rr

    for g in range(NG):
        chunk, gl = divmod(g, 8)
        R = Rt[g % 2]
        for k in range(NE):
            P.op("act", lambda e, R=R, k=k, g=g: e.activation(out=r32(R[:, k, :]), in_=cst[:, 0:128], func=AF.Copy, scale=PR[:, g * NE + k:g * NE + k + 1]),
                 reads=[cst.r(), PR.r()], writes=[R.r(k)])
            P.op("dve", lambda e, R=R, k=k, g=g: e.scalar_tensor_tensor(out=r32(R[:, k, :]), in0=cst[:, 128:256], scalar=PIs[:, g * NE + k:g * NE + k + 1], in1=R[:, k, :],
                                                                       op0=ALU.mult, op1=ALU.add), reads=[cst.r(), PIs.r(), R.r(k)], writes=[R.r(k)])
        cur, nxt = SAB
        for n in range(4):
            ps = rbank()
            P.op("pe", lambda e, ps=ps, g=g, chunk=chunk, n=n: e.matmul(ps[:], lhsT=BT[:, g, :], rhs=uT[:, chunk, n * 512:(n + 1) * 512], start=True, stop=True),
                 reads=[BT.r(), uT.r((chunk, n))], writes=[ps.r()])
            evac_copy(r32(cur[:, PAD + n * 512:PAD + (n + 1) * 512]), ps[:], [ps.r()], [cur.r(n)])
        for k in range(NE):
            sh = 1 << k
            for n in range(4):
                lo = n * 512 - sh
                dst = nxt[:, PAD + n * 512:PAD + (n + 1) * 512]
                srcn = cur[:, PAD + n * 512:PAD + (n + 1) * 512]
                if lo + 512 <= 0:
                    P.op("pool", lambda e, dst=dst, srcn=srcn: e.tensor_copy(out=r32(dst), in_=srcn), reads=[cur.r(n)], writes=[nxt.r(n)])
                    continue
                ps = rbank()
                P.op("pe", lambda e, ps=ps, srcn=srcn: e.matmul(ps[:], lhsT=r32(identR[:]), rhs=r32(srcn), start=True, stop=False),
                     reads=[identR.r(), cur.r(n)], writes=[ps.r()], pmode="f32")
                P.op("pe", lambda e, ps=ps, R=R, k=k, cur=cur, lo=lo: e.matmul(ps[:], lhsT=r32(R[:, k, :]), rhs=r32(cur[:, PAD + lo:PAD + lo + 512]), start=False, stop=True),
                     reads=[R.r(k)] + sregs(cur, lo), writes=[ps.r()], pmode="f32")
                evac_copy(r32(dst), ps[:], [ps.r()], [nxt.r(n)])
            cur, nxt = nxt, cur
        for n in range(4):
            yb = ybanks[n]
            P.op("pe", lambda e, yb=yb, g=g, cur=cur, n=n, gl=gl: e.matmul(yb[:], lhsT=Cpad[:, g * 128:(g + 1) * 128], rhs=cur[:, PAD + n * 512:PAD + (n + 1) * 512],
                                                                          start=(gl == 0), stop=(gl == 7)),
                 reads=[Cpad.r(), cur.r(n)], writes=[yb.r()], pmode="f32")
        if gl == 7:
            for n in range(4):
                yb = ybanks[n]
                yt = ytmp[n % 2]
                P.op("dve", lambda e, yb=yb, yt=yt, chunk=chunk, n=n: e.scalar_tensor_tensor(out=yt[:], in0=uT[:, chunk, n * 512:(n + 1) * 512], scalar=dcol[:, chunk:chunk + 1],
                                                                                           in1=yb[:], op0=ALU.mult, op1=ALU.add),
                     reads=[uT.r((chunk, n)), dcol.r(), yb.r()], writes=[yt.r()])
                P.op("act", lambda e, yt=yt, chunk=chunk, n=n: e.activation(out=yT[:, chunk, n * 512:(n + 1) * 512], in_=yt[:], func=AF.Gelu_apprx_tanh),
                     reads=[yt.r()], writes=[yT.r((chunk, n))])

    if stage == 2:
        o1 = dbg_out("d_yT", [128, 4 * L], BF16)
        o2 = dbg_out("d_uT", [128, 4 * L], BF16)
        P.dma("sp", lambda e: e.dma_start(out=o1, in_=yT[:].rearrange("p a b -> p (a b)")), reads=[yT.r((k, n)) for k in range(4) for n in range(4)])
        P.dma("sp", lambda e: e.dma_start(out=o2, in_=uT[:].rearrange("p a b -> p (a b)")), reads=[uT.r((k, n)) for k in range(4) for n in range(4)])
        P.wait_all_dma("sp")
        P.build()
        return nc


    A.release(m_scan)
    wglu = A.alloc("wglu", [128, 4, S5W], BF16)
    P.dma("pool", lambda e: e.dma_start(out=wglu[:], in_=wglu_d.rearrange("(k p) n -> p k n", p=128)), writes=[wglu.r()])
    wpa = A.alloc("wpa", [128, 4, D], BF16)
    P.dma("pool", lambda e: e.dma_start(out=wpa[:], in_=wpa_d.rearrange("(k p) n -> p k n", p=128)), writes=[wpa.r()])
    wga = A.alloc("wga", [128, 8, D], BF16)
    P.dma("pool", lambda e: e.dma_start(out=wga[:], in_=win_v[:, :, OFF_GA:OFF_GA + D]), writes=[wga.r()])
    yaT = A.alloc("yaT", [128, 4, L], BF16)
    sgb = [A.alloc(f"sgb{i}", [128, 512], BF16) for i in range(2)]
    sgf = [A.alloc(f"sgf{i}", [128, 512], F32) for i in range(2)]
    it = 0
    for m in range(4):
        for n in range(4):
            ps = bank()
            for k in range(4):
                P.op("pe", lambda e, ps=ps, m=m, n=n, k=k: e.matmul(ps[:], lhsT=wglu[:, k, m * 128:(m + 1) * 128], rhs=yT[:, k, n * 512:(n + 1) * 512],
                                                                   start=(k == 0), stop=(k == 3)), reads=[wglu.r(), yT.r((k, n))], writes=[ps.r()])
            sg = sgb[it % 2]
            it += 1
            P.op("act", lambda e, ps=ps, sg=sg, m=m: e.activation(out=sg[:], in_=ps[:], func=AF.Sigmoid, bias=dcol[:, 4 + m:5 + m]),
                 reads=[ps.r(), dcol.r()], writes=[sg.r()])
            P.op("dve", lambda e, sg=sg, m=m, n=n: e.tensor_tensor(out=yaT[:, m, n * 512:(n + 1) * 512], in0=yT[:, m, n * 512:(n + 1) * 512], in1=sg[:], op=ALU.mult),
                 reads=[sg.r(), yT.r((m, n))], writes=[yaT.r((m, n))])

    def proj_gate(wp, wg, srcT, first):
        it = 0
        for m in range(8):
            for n in range(4):
                ps1 = bank()
                for k in range(4):
                    P.op("pe", lambda e, ps1=ps1, m=m, n=n, k=k: e.matmul(ps1[:], lhsT=wp[:, k, m * 128:(m + 1) * 128], rhs=srcT[:, k, n * 512:(n + 1) * 512],
                                                                         start=(k == 0), stop=(k == 3)), reads=[wp.r(), srcT.r((k, n))], writes=[ps1.r()])
                ps2 = bank()
                for k in range(8):
                    P.op("pe", lambda e, ps2=ps2, m=m, n=n, k=k: e.matmul(ps2[:], lhsT=wg[:, k, m * 128:(m + 1) * 128], rhs=hT[:, k, n * 512:(n + 1) * 512],
                                                                         start=(k == 0), stop=(k == 7)), reads=[wg.r(), hT.r((k, n))], writes=[ps2.r()])
                sg = sgf[it % 2]
                it += 1
                P.op("act", lambda e, ps2=ps2, sg=sg: e.activation(out=sg[:], in_=ps2[:], func=AF.Sigmoid), reads=[ps2.r()], writes=[sg.r()])
                dst = mixedT[:, m, n * 512:(n + 1) * 512]
                if first:
                    P.op("dve", lambda e, ps1=ps1, sg=sg, dst=dst: e.tensor_tensor(out=dst, in0=ps1[:], in1=sg[:], op=ALU.mult),
                         reads=[ps1.r(), sg.r()], writes=[mixedT.r((m, n))])
                else:
                    P.op("dve", lambda e, ps1=ps1, sg=sg: e.tensor_tensor(out=sg[:], in0=ps1[:], in1=sg[:], op=ALU.mult),
                         reads=[ps1.r(), sg.r()], writes=[sg.r()])
                    P.op("pool", lambda e, sg=sg, dst=dst: e.tensor_tensor(out=dst, in0=dst, in1=sg[:], op=ALU.add),
                         reads=[sg.r(), mixedT.r((m, n))], writes=[mixedT.r((m, n))])

    proj_gate(wpa, wga, yaT, True)
    A.release(m_C2)

    P.pe_selfsync = True
    qtT = A.alloc("qtT", [128, 2, L], BF16)
    ktT = A.alloc("ktT", [128, 2, L], BF16)
    vtok = A.alloc("vtok", [128, NT, 512], BF16)
    rstok = A.alloc("rstok", [128, NT, 512], BF16)
    ktok = A.alloc("ktok", [128, NT, 256], BF16)
    eblast = A.alloc("eblast", [128, 2, NT], F32)
    gsm = A.alloc("gsm", [128, 512], F32)
    m_G = A.mark()
    wgla = A.alloc("wgla", [128, 8, 1552], BF16)
    P.dma("pool", lambda e: e.dma_start(out=wgla[:], in_=win_v[:, :, OFF_Q:OFF_Q + 1552]), writes=[wgla.r()])
    WQ, WK, WV, WGK, WR = 0, 256, 512, 1024, 1040
    wgk2 = A.alloc("wgk2", [16, 256], BF16)
    P.dma("pool", lambda e: e.dma_start(out=wgk2[:], in_=wgk2_d), writes=[wgk2.r()])
    P.dma("sp", lambda e: e.dma_start(out=gsm[:, 0:2], in_=bgk2_d.rearrange("(k p) -> p k", p=128), allow_slow_non_contiguous=True), writes=[gsm.r()])
    P.dma("sp", lambda e: e.dma_start(out=gsm[:, 128:256], in_=bass.AP(gng_d.tensor, gng_d.offset, [[0, 128], [1, 128]])), writes=[gsm.r()])
    P.op("dve", lambda e: e.tensor_scalar(out=gsm[:, 0:2], in0=gsm[:, 0:2], scalar1=-1.0, scalar2=None, op0=ALU.mult), reads=[gsm.r()], writes=[gsm.r()])
    P.op("pool", lambda e: e.memset(gsm[:, 256:384], 1.0), writes=[gsm.r()])
    gkT = A.alloc("gkT", [16, L], BF16)
    for n in range(4):
        ps = bank()
        for k in range(8):
            P.op("pe", lambda e, ps=ps, n=n, k=k: e.matmul(ps[0:16, :], lhsT=wgla[:, k, WGK:WGK + 16], rhs=hT[:, k, n * 512:(n + 1) * 512], start=(k == 0), stop=(k == 7)),
                 reads=[wgla.r(), hT.r((k, n))], writes=[ps.r()], pmode="m16")
        evac_copy(gkT[:, n * 512:(n + 1) * 512], ps[0:16, :], [ps.r()], [gkT.r(n)])
    cum = [A.alloc(f"cum{i}", [128, 512], F32) for i in range(2)]
    ebt = [A.alloc(f"ebt{i}", [128, 512], F32) for i in range(2)]
    enbt = [A.alloc(f"enbt{i}", [128, 512], F32) for i in range(2)]
    it = 0
    for n in range(4):
        for t2 in range(2):
            cu, eb_, enb_ = cum[it % 2], ebt[it % 2], enbt[it % 2]
            it += 1
            ps = bank()
            P.op("pe", lambda e, ps=ps, n=n, t2=t2: e.matmul(ps[:], lhsT=wgk2[:, t2 * 128:(t2 + 1) * 128], rhs=gkT[:, n * 512:(n + 1) * 512], start=True, stop=True),
                 reads=[wgk2.r(), gkT.r(n)], writes=[ps.r()], pmode="k16")
            P.op("act", lambda e, ps=ps, cu=cu, t2=t2: e.activation(out=cu[:], in_=ps[:], func=AF.Exp, scale=-1.0, bias=gsm[:, t2:t2 + 1]), reads=[ps.r(), gsm.r()], writes=[cu.r()])
            P.op("act", lambda e, cu=cu: e.activation(out=cu[:], in_=cu[:], func=AF.Ln, bias=1.0), reads=[cu.r()], writes=[cu.r()])
            for c4 in range(4):
                P.op("dve", lambda e, cu=cu, c4=c4: e.tensor_tensor_scan(out=cu[:, c4 * 128:(c4 + 1) * 128], data0=gsm[:, 256:384], data1=cu[:, c4 * 128:(c4 + 1) * 128],
                                                                        initial=0.0, op0=ALU.mult, op1=ALU.add), reads=[cu.r(), gsm.r()], writes=[cu.r()])
            P.op("act", lambda e, cu=cu, eb_=eb_: e.activation(out=eb_[:], in_=cu[:], func=AF.Exp, scale=-1.0 / 16.0), reads=[cu.r()], writes=[eb_.r()])
            P.op("act", lambda e, cu=cu, enb_=enb_: e.activation(out=enb_[:], in_=cu[:], func=AF.Exp, scale=1.0 / 16.0), reads=[cu.r()], writes=[enb_.r()])
            P.op("pool", lambda e, eb_=eb_, n=n, t2=t2: e.tensor_copy(out=eblast[:, t2, n * 4:(n + 1) * 4], in_=fv(eb_, 127, [(128, 4)])), reads=[eb_.r()], writes=[eblast.r()])
            psq = bank()
            for k in range(8):
                P.op("pe", lambda e, psq=psq, n=n, t2=t2, k=k: e.matmul(psq[:], lhsT=wgla[:, k, WQ + t2 * 128:WQ + (t2 + 1) * 128], rhs=hT[:, k, n * 512:(n + 1) * 512],
                                                                       start=(k == 0), stop=(k == 7)), reads=[wgla.r(), hT.r((k, n))], writes=[psq.r()])
            P.op("dve", lambda e, psq=psq, eb_=eb_, n=n, t2=t2: e.scalar_tensor_tensor(out=qtT[:, t2, n * 512:(n + 1) * 512], in0=psq[:], scalar=0.125, in1=eb_[:],
                                                                                      op0=ALU.mult, op1=ALU.mult), reads=[psq.r(), eb_.r()], writes=[qtT.r((t2, n))])
            psk = bank()
            for k in range(8):
                P.op("pe", lambda e, psk=psk, n=n, t2=t2, k=k: e.matmul(psk[:], lhsT=wgla[:, k, WK + t2 * 128:WK + (t2 + 1) * 128], rhs=hT[:, k, n * 512:(n + 1) * 512],
                                                                       start=(k == 0), stop=(k == 7)), reads=[wgla.r(), hT.r((k, n))], writes=[psk.r()])
            P.op("dve", lambda e, psk=psk, enb_=enb_, n=n, t2=t2: e.tensor_tensor(out=ktT[:, t2, n * 512:(n + 1) * 512], in0=psk[:], in1=enb_[:], op=ALU.mult),
                 reads=[psk.r(), enb_.r()], writes=[ktT.r((t2, n))])
    for c in range(NT):
        n = c // 4
        psv = bank()
        for k in range(8):
            P.op("pe", lambda e, psv=psv, c=c, k=k: e.matmul(psv[:], lhsT=hT[:, k, c * 128:(c + 1) * 128], rhs=wgla[:, k, WV:WV + 512], start=(k == 0), stop=(k == 7)),
                 reads=[wgla.r(), hT.r((k, n))], writes=[psv.r()])
        evac_copy(vtok[:, c, :], psv[:], [psv.r()], [vtok.r(c)])
        psr = bank()
        for k in range(8):
            P.op("pe", lambda e, psr=psr, c=c, k=k: e.matmul(psr[:], lhsT=hT[:, k, c * 128:(c + 1) * 128], rhs=wgla[:, k, WR:WR + 512], start=(k == 0), stop=(k == 7)),
                 reads=[wgla.r(), hT.r((k, n))], writes=[psr.r()])
        P.op("act", lambda e, psr=psr, c=c: e.activation(out=rstok[:, c, :], in_=psr[:], func=AF.Silu), reads=[psr.r()], writes=[rstok.r(c)])
        pst = bank()
        pstb = pst.t[:].bitcast(BF16)
        for t2 in range(2):
            P.op("pe", lambda e, pstb=pstb, c=c, t2=t2: e.transpose(pstb[:, t2 * 128:(t2 + 1) * 128], ktT[:, t2, c * 128:(c + 1) * 128], identb_t[:]),
                 reads=[ktT.r((t2, n)), identb_t.r()], writes=[pst.r()])
        evac_copy(ktok[:, c, :], pstb[:, 0:256], [pst.r()], [ktok.r(c)])
    A.release(m_G)
    wpb = A.alloc("wpb", [128, 4, D], BF16)
    P.dma("pool", lambda e: e.dma_start(out=wpb[:], in_=wpb_d.rearrange("(k p) n -> p k n", p=128)), writes=[wpb.r()])
    wgb = A.alloc("wgb", [128, 8, D], BF16)
    P.dma("pool", lambda e: e.dma_start(out=wgb[:], in_=win_v[:, :, OFF_GB:OFF_GB + D]), writes=[wgb.r()])
    ybT = A.alloc("ybT", [128, 4, L], BF16)
    S32 = A.alloc("S32", [128, 2, 128], F32)
    Sbf = A.alloc("Sbf", [128, 2, 128], BF16)
    PT = [A.alloc(f"PT{i}", [128, 4, 128], BF16) for i in range(2)]
    otok = [A.alloc(f"otok{i}", [128, 512], F32) for i in range(2)]
    onrm = [A.alloc(f"onrm{i}", [128, 512], F32) for i in range(2)]
    ybtok = [A.alloc(f"ybtok{i}", [128, 512], BF16) for i in range(2)]
    oss = [A.alloc(f"oss{i}", [128, 8], F32) for i in range(2)]
    osq = A.alloc("osq", [128, 128], F32)
    P.op("pool", lambda e: e.memset(S32[:], 0.0), writes=[S32.r()])
    maskb = fv(cst, 544, [(0, 4), (1, 128)])
    for c in range(NT):
        n = c // 4
        cs = slice(c * 128, (c + 1) * 128)
        pss = bank()
        for hd in range(4):
            t2, po = hd // 2, (hd % 2) * 64
            P.op("pe", lambda e, pss=pss, hd=hd, t2=t2, po=po, cs=cs: e.matmul(pss[:, hd * 128:(hd + 1) * 128], lhsT=ktT[po:po + 64, t2, cs], rhs=qtT[po:po + 64, t2, cs],
                                                                              start=True, stop=True), reads=[ktT.r((t2, n)), qtT.r((t2, n))], writes=[pss.r()], pmode="k64")
        pt = PT[c % 2]
        P.op("dve", lambda e, pss=pss, pt=pt: e.tensor_tensor(out=pt[:], in0=pss[:].rearrange("p (a b) -> p a b", a=4), in1=maskb, op=ALU.mult),
             reads=[pss.r(), cst.r()], writes=[pt.r()])
        pso = bank()
        for hd in range(4):
            t2, po = hd // 2, (hd % 2) * 64
            P.op("pe", lambda e, pso=pso, pt=pt, hd=hd, c=c: e.matmul(pso[:, hd * 128:(hd + 1) * 128], lhsT=pt[:, hd, :], rhs=vtok[:, c, hd * 128:(hd + 1) * 128],
                                                                     start=True, stop=(c == 0)), reads=[pt.r(), vtok.r(c)], writes=[pso.r()])
            if c > 0:
                P.op("pe", lambda e, pso=pso, hd=hd, t2=t2, po=po, cs=cs: e.matmul(pso[:, hd * 128:(hd + 1) * 128], lhsT=qtT[po:po + 64, t2, cs], rhs=Sbf[po:po + 64, t2, :],
                                                                                  start=False, stop=True), reads=[qtT.r((t2, n)), Sbf.r()], writes=[pso.r()], pmode="k64")
        ot, on_, yb_, os_ = otok[c % 2], onrm[c % 2], ybtok[c % 2], oss[c % 2]
        P.op("act", lambda e, pso=pso, ot=ot: e.copy(out=ot[:], in_=pso[:]), reads=[pso.r()], writes=[ot.r()])
        for hd in range(4):
            P.op("act", lambda e, ot=ot, os_=os_, hd=hd: e.activation(out=osq[:], in_=ot[:, hd * 128:(hd + 1) * 128], func=AF.Square, accum_out=os_[:, hd:hd + 1]),
                 reads=[ot.r()], writes=[osq.r(), os_.r()])
        P.op("dve", lambda e, os_=os_: e.tensor_scalar(out=os_[:, 4:8], in0=os_[:, 0:4], scalar1=1.0 / 128.0, scalar2=EPS, op0=ALU.mult, op1=ALU.add), reads=[os_.r()], writes=[os_.r()])
        P.op("act", lambda e, os_=os_: e.activation(out=os_[:, 4:8], in_=os_[:, 4:8], func=AF.Sqrt), reads=[os_.r()], writes=[os_.r()])
        P.op("dve", lambda e, os_=os_: e.reciprocal(out=os_[:, 4:8], in_=os_[:, 4:8]), reads=[os_.r()], writes=[os_.r()])
        for hd in range(4):
            P.op("dve", lambda e, ot=ot, on_=on_, os_=os_, hd=hd: e.scalar_tensor_tensor(out=on_[:, hd * 128:(hd + 1) * 128], in0=ot[:, hd * 128:(hd + 1) * 128], scalar=os_[:, 4 + hd:5 + hd],
                                                                                        in1=gsm[:, 128:256], op0=ALU.mult, op1=ALU.mult), reads=[ot.r(), os_.r(), gsm.r()], writes=[on_.r()])
        P.op("pool", lambda e, on_=on_, yb_=yb_, c=c: e.tensor_tensor(out=yb_[:], in0=on_[:], in1=rstok[:, c, :], op=ALU.mult), reads=[on_.r(), rstok.r(c)], writes=[yb_.r()])
        pst = bank()
        pstb = pst.t[:].bitcast(BF16)
        for m in range(4):
            P.op("pe", lambda e, pstb=pstb, yb_=yb_, m=m: e.transpose(pstb[:, m * 128:(m + 1) * 128], yb_[:, m * 128:(m + 1) * 128], identb_t[:]),
                 reads=[yb_.r(), identb_t.r()], writes=[pst.r()])
        evac_copy(ybT[:, :, cs], pstb[:, 0:512].rearrange("p (a b) -> p a b", a=4), [pst.r()], [ybT.r((m, n)) for m in range(4)])
        if c < NT - 1:
            for t2 in range(2):
                psu = bank()
                P.op("pe", lambda e, psu=psu, c=c, t2=t2: e.matmul(psu[:, 0:256], lhsT=ktok[:, c, t2 * 128:(t2 + 1) * 128], rhs=vtok[:, c, t2 * 256:(t2 + 1) * 256], start=True, stop=True),
                     reads=[ktok.r(c), vtok.r(c)], writes=[psu.r()])
                for hl in range(2):
                    rows = slice(hl * 64, (hl + 1) * 64)
                    P.op("dve", lambda e, psu=psu, t2=t2, hl=hl, rows=rows: e.tensor_tensor(out=S32[rows, t2, :], in0=psu[rows, hl * 128:(hl + 1) * 128], in1=S32[rows, t2, :], op=ALU.add),
                         reads=[psu.r(), S32.r()], writes=[S32.r()])
                    P.op("act", lambda e, t2=t2, rows=rows, c=c: e.activation(out=S32[rows, t2, :], in_=S32[rows, t2, :], func=AF.Copy, scale=eblast[rows, t2, c:c + 1]),
                         reads=[S32.r(), eblast.r()], writes=[S32.r()])
                    P.op("dve", lambda e, t2=t2, rows=rows: e.tensor_copy(out=Sbf[rows, t2, :], in_=S32[rows, t2, :]), reads=[S32.r()], writes=[Sbf.r()])

    if stage == 3:
        o1 = dbg_out("d_ybT", [128, 4 * L], BF16)
        P.dma("sp", lambda e: e.dma_start(out=o1, in_=ybT[:].rearrange("p a b -> p (a b)")), reads=[ybT.r((k, n)) for k in range(4) for n in range(4)])
        o2 = dbg_out("d_yaT", [128, 4 * L], BF16)
        P.dma("sp", lambda e: e.dma_start(out=o2, in_=yaT[:].rearrange("p a b -> p (a b)")), reads=[yaT.r((k, n)) for k in range(4) for n in range(4)])
        P.wait_all_dma("sp")
        P.build()
        return nc

    P.pe_selfsync = False
    proj_gate(wpb, wgb, ybT, False)

    if stage == 4:
        o1 = dbg_out("d_mixedT", [128, 8 * L], BF16)
        P.dma("sp", lambda e: e.dma_start(out=o1, in_=mixedT[:].rearrange("p a b -> p (a b)")), reads=[mixedT.r((k, n)) for k in range(8) for n in range(4)])
        P.wait_all_dma("sp")
        P.build()
        return nc


    A.release(m_C2)
    acc = A.alloc("acc", [128, NT, D], F32)
    m_E = A.mark()
    wout = A.alloc("wout", [128, 8, D], BF16)
    P.dma("pool", lambda e: e.dma_start(out=wout[:], in_=wout_d.rearrange("(k p) n -> p k n", p=128)), writes=[wout.r()])
    for k in range(8):
        eng = "dve" if k % 2 == 0 else "pool"
        P.op(eng, lambda e, k=k: e.tensor_tensor(out=wout[:, k, :], in0=wout[:, k, :], in1=g12[:, 0, :], op=ALU.mult), reads=[wout.r(), g12.r(0)], writes=[wout.r()])
    xin2 = [A.alloc(f"xin2_{i}", [128, 4, D], F32) for i in range(2)]
    for blk in range(4):
        xi = xin2[blk % 2]
        P.dma("sp", lambda e, xi=xi, blk=blk: e.dma_start(out=xi[:], in_=x_v[:, blk * 4:(blk + 1) * 4, :]), writes=[xi.r()])
        for j in range(4):
            c = blk * 4 + j
            for half in range(2):
                ps = bank()
                for k in range(8):
                    P.op("pe", lambda e, ps=ps, c=c, k=k, half=half: e.matmul(ps[:], lhsT=mixedT[:, k, c * 128:(c + 1) * 128], rhs=wout[:, k, half * 512:(half + 1) * 512],
                                                                             start=(k == 0), stop=(k == 7)), reads=[mixedT.r((k, blk)), wout.r()], writes=[ps.r()])
                P.op("dve", lambda e, ps=ps, xi=xi, j=j, c=c, half=half: e.tensor_tensor(out=acc[:, c, half * 512:(half + 1) * 512], in0=ps[:], in1=xi[:, j, half * 512:(half + 1) * 512], op=ALU.add),
                     reads=[ps.r(), xi.r()], writes=[acc.r((c, half))])
    A.release(m_E)

    if stage == 5:
        o1 = dbg_out("d_x1", [L, D], F32)
        P.dma("sp", lambda e: e.dma_start(out=o1.rearrange("(n p) d -> p n d", p=128), in_=acc[:]), reads=[acc.r((c, h_)) for c in range(NT) for h_ in range(2)])
        P.wait_all_dma("sp")
        P.build()
        return nc

    NB = 384
    NSLOT = NB * 128
    xn2_d = nc.dram_tensor("xn2_scr", [L, D], BF16).ap()
    slottab_d = nc.dram_tensor("slottab_scr", [NSLOT, 2], I32).ap()
    yslots_d = nc.dram_tensor("yslots_scr", [NSLOT, D], F32).ap()
    bexp_d = nc.dram_tensor("bexp_scr", [NB], I32).ap()
    r_xn2, r_slottab, r_yslots, r_bexp = Reg("xn2"), Reg("slottab"), Reg("yslots"), Reg("bexp")
    h2T = A.alloc("h2T", [128, 8, L], BF16, at=hT_off)
    Msk = A.alloc("Msk", [128, NT, NEXP], BF16, at=mixedT_off + 17664)
    Wr = A.alloc("Wr", [128, NT, NEXP + 1], F32, at=mixedT_off)
    rbias = A.alloc("rbias", [128, NEXP], F32, at=mixedT_off + 16512)
    m_F = A.mark()
    wrt = A.alloc("wrt", [128, 8, NEXP], F32)
    P.dma("sp", lambda e: e.dma_start(out=wrt[:], in_=rw_d.rearrange("(k p) n -> p k n", p=128)), writes=[wrt.r()])
    P.dma("sp", lambda e: e.dma_start(out=rbias[:], in_=bass.AP(rb_d.tensor, rb_d.offset, [[0, 128], [1, NEXP]])), writes=[rbias.r()])
    P.op("pool", lambda e: e.memset(Wr[:, :, NEXP:NEXP + 1], 1.0), writes=[Wr.r("sh")])
    xnf = A.alloc("xnf", [128, 4, D], F32)
    h2f = A.alloc("h2f", [128, 8, 512], F32)
    rt = A.alloc("rt", [128, 2048], F32)
    SC, BI, MB, SEL = 0, 256, 512, 768
    M8 = A.alloc("M8", [128, 128], F32)
    for blk in range(4):
        for j in range(4):
            c = blk * 4 + j
            P.op("act", lambda e, c=c, blk=blk: e.activation(out=sq[:], in_=acc[:, c, :], func=AF.Square, accum_out=ssq[:, c:c + 1]),
                 reads=[acc.r((c, 0)), acc.r((c, 1))], writes=[sq.r(), ssq.r(blk)])
        rs = rstd[:, blk * 4:(blk + 1) * 4]
        P.op("dve", lambda e, rs=rs, blk=blk: e.tensor_scalar(out=rs, in0=ssq[:, blk * 4:(blk + 1) * 4], scalar1=1.0 / D, scalar2=EPS, op0=ALU.mult, op1=ALU.add),
             reads=[ssq.r(blk)], writes=[rstd.r(blk)])
        P.op("act", lambda e, rs=rs: e.activation(out=rs, in_=rs, func=AF.Sqrt), reads=[rstd.r(blk)], writes=[rstd.r(blk)])
        P.op("dve", lambda e, rs=rs: e.reciprocal(out=rs, in_=rs), reads=[rstd.r(blk)], writes=[rstd.r(blk)])
        for j in range(4):
            c = blk * 4 + j
            P.op("act", lambda e, j=j, c=c: e.activation(out=xnf[:, j, :], in_=acc[:, c, :], func=AF.Copy, scale=rstd[:, c:c + 1]),
                 reads=[acc.r((c, 0)), acc.r((c, 1)), rstd.r(blk)], writes=[xnf.r(j)])
            P.dma("pool", lambda e, j=j, c=c: e.dma_start(out=xn2_d[c * 128:(c + 1) * 128, :], in_=xnf[:, j, :]), reads=[xnf.r(j)], wacc=[r_xn2])
        for k in range(8):
            ps = bank()
            for j in range(4):
                P.op("pe", lambda e, ps=ps, j=j, k=k: e.transpose(ps[:, j * 128:(j + 1) * 128], xnf[:, j, k * 128:(k + 1) * 128], cst[:, 0:128]),
                     reads=[xnf.r(j), cst.r()], writes=[ps.r()], pmode="f32")
            P.op("dve", lambda e, ps=ps, k=k, blk=blk: e.tensor_scalar(out=h2T[:, k, blk * 512:(blk + 1) * 512], in0=ps[:], scalar1=AB[:, 1, k:k + 1], scalar2=modcol[:, 2, k:k + 1],
                                                                      op0=ALU.mult, op1=ALU.add), reads=[ps.r(), AB.r(), modcol.r()], writes=[h2T.r((k, blk))])
            P.op("dve", lambda e, ps=ps, k=k: e.tensor_scalar(out=h2f[:, k, :], in0=ps[:], scalar1=AB[:, 1, k:k + 1], scalar2=modcol[:, 2, k:k + 1], op0=ALU.mult, op1=ALU.add),
                 reads=[ps.r(), AB.r(), modcol.r()], writes=[h2f.r(k)])
        for j in range(4):
            c = blk * 4 + j
            ps = bank()
            for k in range(8):
                P.op("pe", lambda e, ps=ps, j=j, k=k: e.matmul(ps[:, 0:NEXP], lhsT=h2f[:, k, j * 128:(j + 1) * 128], rhs=wrt[:, k, :], start=(k == 0), stop=(k == 7)),
                     reads=[h2f.r(k), wrt.r()], writes=[ps.r()], pmode="f32")
            rr = [rt.r()]
            mr = [M8.r()]
            P.op("act", lambda e, ps=ps: e.activation(out=rt[:, SC:SC + 256], in_=ps[:, 0:NEXP], func=AF.Sigmoid), reads=[ps.r()], writes=rr)
            P.op("dve", lambda e: e.tensor_tensor(out=rt[:, BI:BI + 256], in0=rt[:, SC:SC + 256], in1=rbias[:], op=ALU.add), reads=rr + [rbias.r()], writes=rr)
            for gq in range(8):
                P.op("dve", lambda e, gq=gq: e.max(out=M8[:, gq * 8:(gq + 1) * 8], in_=rt[:, BI + gq * 32:BI + (gq + 1) * 32]), reads=rr, writes=mr)
            P.op("dve", lambda e: e.tensor_tensor(out=M8[:, 64:72], in0=fv(M8, 0, [(8, 8)]), in1=fv(M8, 1, [(8, 8)]), op=ALU.add), reads=mr, writes=mr)
            P.op("dve", lambda e: e.max(out=M8[:, 72:80], in_=M8[:, 64:72]), reads=mr, writes=mr)
            P.op("dve", lambda e: e.tensor_scalar(out=M8[:, 80:88], in0=M8[:, 64:72], scalar1=M8[:, 75:76], scalar2=None, op0=ALU.is_ge), reads=mr, writes=mr)
            P.op("dve", lambda e: e.scalar_tensor_tensor(out=fv(rt, MB, [(32, 8), (1, 32)]), in0=fv(rt, BI, [(32, 8), (1, 32)]), scalar=2.0, in1=fv(M8, 80, [(1, 8), (0, 32)]),
                                                         op0=ALU.add, op1=ALU.mult), reads=rr + mr, writes=rr)
            P.op("dve", lambda e: e.max(out=M8[:, 88:96], in_=rt[:, MB:MB + 256]), reads=rr, writes=mr)
            P.op("dve", lambda e: e.tensor_scalar(out=rt[:, SEL:SEL + 256], in0=rt[:, MB:MB + 256], scalar1=M8[:, 95:96], scalar2=None, op0=ALU.is_ge), reads=rr + mr, writes=rr)
            P.op("pool", lambda e, c=c: e.tensor_copy(out=Msk[:, c, :], in_=rt[:, SEL:SEL + 256]), reads=rr, writes=[Msk.r(c)])
            P.op("dve", lambda e: e.tensor_tensor(out=rt[:, SEL:SEL + 256], in0=rt[:, SEL:SEL + 256], in1=rt[:, SC:SC + 256], op=ALU.mult), reads=rr, writes=rr)
            P.op("dve", lambda e: e.tensor_reduce(out=M8[:, 96:97], in_=rt[:, SEL:SEL + 256], axis=AX.X, op=ALU.add), reads=rr, writes=mr)
            P.op("dve", lambda e: e.reciprocal(out=M8[:, 97:98], in_=M8[:, 96:97]), reads=mr, writes=mr)
            P.op("dve", lambda e, c=c: e.tensor_scalar(out=Wr[:, c, 0:NEXP], in0=rt[:, SEL:SEL + 256], scalar1=M8[:, 97:98], scalar2=2.5, op0=ALU.mult, op1=ALU.mult),
                 reads=rr + mr, writes=[Wr.r(c)])
    A.release(m_F)

    if stage == 6:
        o1 = dbg_out("d_W", [L, NEXP + 1], F32)
        P.dma("sp", lambda e: e.dma_start(out=o1.rearrange("(n p) d -> p n d", p=128), in_=Wr[:]), reads=[Wr.r(c) for c in range(NT)] + [Wr.r("sh")])
        o2 = dbg_out("d_h2T", [128, 8 * L], BF16)
        P.dma("sp", lambda e: e.dma_start(out=o2, in_=h2T[:].rearrange("p a b -> p (a b)")), reads=[h2T.r((k, n)) for k in range(8) for n in range(4)])
        P.wait_all_dma("sp")
        P.build()
        return nc

    IOA = bass.IndirectOffsetOnAxis
    regcache = {}

    def breg(e, val):
        if ("b", val) not in regcache:
            regcache[("b", val)] = e.to_reg(val)
        return regcache[("b", val)]
    m_G0 = A.mark()
    wsh = A.alloc("wsh", [128, 2, 2048], BF16)
    wdsh = A.alloc("wdsh", [128, 2048], BF16)
    P.dma("pool", lambda e: e.dma_start(out=wsh[:, 0, :], in_=ewg_d[(n_exp - 1) * 128:n_exp * 128, :]), writes=[wsh.r("g")])
    P.dma("pool", lambda e: e.dma_start(out=wsh[:, 1, :], in_=ewu_d[(n_exp - 1) * 128:n_exp * 128, :]), writes=[wsh.r("u")])
    P.dma("pool", lambda e: e.dma_start(out=wdsh[:], in_=ewd_d[(n_exp - 1) * 128:n_exp * 128, :]), writes=[wdsh.r()])
    aT = [A.alloc(f"aT{i}", [128, 2, 512], BF16) for i in range(2)]
    sgm = [A.alloc(f"sgm{i}", [128, 512], BF16) for i in range(2)]
    for kk in range(2):
        P.op("pool", lambda e, kk=kk: e.tensor_tensor(out=wdsh[:, kk * 1024:(kk + 1) * 1024], in0=wdsh[:, kk * 1024:(kk + 1) * 1024], in1=g12[:, 1, :], op=ALU.mult),
             reads=[wdsh.r(), g12.r(1)], writes=[wdsh.r()])
    for n in range(4):
        at_ = aT[n % 2]
        for f in range(2):
            psg = bank()
            for k in range(8):
                P.op("pe", lambda e, psg=psg, f=f, k=k, n=n: e.matmul(psg[:], lhsT=wsh[:, 0, k * 256 + f * 128:k * 256 + (f + 1) * 128], rhs=h2T[:, k, n * 512:(n + 1) * 512], start=(k == 0), stop=(k == 7)),
                     reads=[wsh.r("g"), h2T.r((k, n))], writes=[psg.r()])
            psu = bank()
            for k in range(8):
                P.op("pe", lambda e, psu=psu, f=f, k=k, n=n: e.matmul(psu[:], lhsT=wsh[:, 1, k * 256 + f * 128:k * 256 + (f + 1) * 128], rhs=h2T[:, k, n * 512:(n + 1) * 512], start=(k == 0), stop=(k == 7)),
                     reads=[wsh.r("u"), h2T.r((k, n))], writes=[psu.r()])
            sg = sgm[f]
            P.op("act", lambda e, psg=psg, sg=sg: e.activation(out=sg[:], in_=psg[:], func=AF.Silu), reads=[psg.r()], writes=[sg.r()])
            P.op("dve", lambda e, psu=psu, sg=sg, at_=at_, f=f: e.tensor_tensor(out=at_[:, f, :], in0=psu[:], in1=sg[:], op=ALU.mult), reads=[psu.r(), sg.r()], writes=[at_.r(f)])
        for j in range(4):
            c = n * 4 + j
            for half in range(2):
                psy = bank()
                for f in range(2):
                    P.op("pe", lambda e, psy=psy, at_=at_, f=f, j=j, half=half: e.matmul(psy[:], lhsT=at_[:, f, j * 128:(j + 1) * 128], rhs=wdsh[:, f * 1024 + half * 512:f * 1024 + (half + 1) * 512],
                                                                                   start=(f == 0), stop=(f == 1)), reads=[at_.r(f), wdsh.r()], writes=[psy.r()])
                dst = acc[:, c, half * 512:(half + 1) * 512]
                P.op("dve", lambda e, psy=psy, dst=dst: e.tensor_tensor(out=dst, in0=psy[:], in1=dst, op=ALU.add), reads=[psy.r(), acc.r((c, half))], writes=[acc.r((c, half))])
    A.release(m_G0)

    onesb = A.alloc("onesb", [128, 128], BF16)
    sutb = A.alloc("sutb", [128, 128], BF16)
    onesf = A.alloc("onesf", [128, 256], F32)
    P.op("pool", lambda e: e.memset(onesb[:], 1.0), writes=[onesb.r()])
    P.op("pool", lambda e: e.memset(onesf[:], 1.0), writes=[onesf.r()])
    P.op("dve", lambda e: e.tensor_copy(out=sutb[:], in_=cst[:, 704:832]), reads=[cst.r()], writes=[sutb.r()])
    zt = A.alloc("zt", [128, 768], I32)
    P.op("pool", lambda e: e.memset(zt[:], 0), writes=[zt.r()])
    P.dma("sp", lambda e: e.dma_start(out=slottab_d.rearrange("(p n) two -> p (n two)", p=128), in_=zt[:]), reads=[zt.r()], writes=[r_slottab])
    cnt = A.alloc("cntt", [128, 1024], F32)
    cr_ = [cnt.r()]
    ps = bank()
    for c in range(NT):
        P.op("pe", lambda e, ps=ps, c=c: e.matmul(ps[:, 0:256], lhsT=onesb[:], rhs=Msk[:, c, :], start=(c == 0), stop=(c == NT - 1)), reads=[onesb.r(), Msk.r(c)], writes=[ps.r()])
    P.op("dve", lambda e, ps=ps: e.tensor_copy(out=cnt[:, 0:256], in_=ps[:, 0:256]), reads=[ps.r()], writes=cr_)
    widx = A.alloc("widx", [128, NB], I32)
    m_cmp = A.mark()
    cmp = A.alloc("cmpt", [128, 4096], F32)
    P.op("dve", lambda e: e.tensor_tensor(out=fv(cmp, 0, [(16, 256), (1, 16)]), in0=fv(cnt, 0, [(1, 256), (0, 16)]), in1=fv(cst, 680, [(0, 256), (1, 16)]), op=ALU.is_gt),
         reads=cr_ + [cst.r()], writes=[cmp.r()])
    P.op("dve", lambda e: e.tensor_reduce(out=cnt[:, 256:512], in_=fv(cmp, 0, [(16, 256), (1, 16)]), axis=AX.X, op=ALU.add), reads=[cmp.r()], writes=cr_)
    P.op("dve", lambda e: e.tensor_tensor_scan(out=cnt[:, 512:768], data0=onesf[:], data1=cnt[:, 256:512], initial=0.0, op0=ALU.mult, op1=ALU.add), reads=cr_ + [onesf.r()], writes=cr_)
    P.op("dve", lambda e: e.tensor_tensor(out=cnt[:, 768:1024], in0=cnt[:, 512:768], in1=cnt[:, 256:512], op=ALU.subtract), reads=cr_, writes=cr_)
    P.op("dve", lambda e: e.tensor_scalar(out=cnt[:, 768:1024], in0=cnt[:, 768:1024], scalar1=128.0, scalar2=None, op0=ALU.mult), reads=cr_, writes=cr_)
    bx = A.alloc("bx", [128, 8], F32)
    bxi = A.alloc("bxi", [128, 4], I32)
    for i in range(3):
        P.op("dve", lambda e, i=i: e.tensor_scalar(out=bx[:, 4 + i:5 + i], in0=cst[:, 840:841], scalar1=float(128 * i), scalar2=None, op0=ALU.add), reads=[cst.r()], writes=[bx.r()])
        P.op("dve", lambda e, i=i: e.tensor_scalar(out=cmp[:, 0:256], in0=cnt[:, 512:768], scalar1=bx[:, 4 + i:5 + i], scalar2=None, op0=ALU.is_le), reads=cr_ + [bx.r()], writes=[cmp.r()])
        P.op("dve", lambda e, i=i: e.tensor_reduce(out=bx[:, i:i + 1], in_=cmp[:, 0:256], axis=AX.X, op=ALU.add), reads=[cmp.r()], writes=[bx.r()])
    onesf128 = A.alloc("onesf128", [128, 128], F32)
    P.op("pool", lambda e: e.memset(onesf128[:], 1.0), writes=[onesf128.r()])
    dg = A.alloc("dg", [128, 3, 128], F32)
    widf = A.alloc("widf", [128, NB], F32)
    wtl = A.alloc("wtl", [128, NB], F32)
    psb_ = bank()
    for i in range(3):
        P.op("dve", lambda e, i=i: e.tensor_scalar(out=dg[:, i, :], in0=cst[:, 0:128], scalar1=bx[:, i:i + 1], scalar2=None, op0=ALU.mult), reads=[cst.r(), bx.r()], writes=[dg.r(i)])
        P.op("pe", lambda e, i=i, psb_=psb_: e.matmul(psb_[:, i * 128:(i + 1) * 128], lhsT=onesf128[:], rhs=dg[:, i, :], start=True, stop=True), reads=[onesf128.r(), dg.r(i)], writes=[psb_.r()], pmode="f32")
    P.op("dve", lambda e, psb_=psb_: e.tensor_scalar(out=widf[:], in0=psb_[:, 0:NB], scalar1=128.0, scalar2=cst[:, 840:841], op0=ALU.mult, op1=ALU.add), reads=[psb_.r(), cst.r()], writes=[widf.r()])
    P.op("dve", lambda e, psb_=psb_: e.tensor_scalar(out=wtl[:], in0=psb_[:, 0:NB], scalar1=float(NEXP), scalar2=1.0e6, op0=ALU.is_ge, op1=ALU.mult), reads=[psb_.r()], writes=[wtl.r()])
    P.op("dve", lambda e: e.tensor_tensor(out=widf[:], in0=widf[:], in1=wtl[:], op=ALU.add), reads=[widf.r(), wtl.r()], writes=[widf.r()])
    P.op("dve", lambda e: e.tensor_copy(out=widx[:], in_=widf[:]), reads=[widf.r()], writes=[widx.r()])
    A.release(m_cmp)
    tokid = A.alloc("tokid", [128, NT], I32)
    P.op("pool", lambda e: e.iota(tokid[:], [[128, NT]], base=0, channel_multiplier=1), writes=[tokid.r()])
    d8 = A.alloc("d8", [128, NT, 8], F32)
    d8i = A.alloc("d8i", [128, NT, 8], I32)
    m_bk = A.mark()
    keyt = [A.alloc(f"key{i}", [128, 256], F32) for i in range(2)]
    eqt = A.alloc("eqt", [128, 256], F32)
    junk = A.alloc("junk", [128, 256], F32)
    w8t = [A.alloc(f"w8_{i}", [128, 8], F32) for i in range(2)]
    ixt = [[A.alloc(f"ix{i}_{j}", [128, 1], I32) for j in range(8)] for i in range(2)]
    rct = [[A.alloc(f"rc{i}_{j}", [128, 2], I32) for j in range(8)] for i in range(2)]
    for c in range(NT):
        ps = bank()
        for c2 in range(c):
            P.op("pe", lambda e, ps=ps, c2=c2: e.matmul(ps[:, 0:256], lhsT=onesb[:], rhs=Msk[:, c2, :], start=(c2 == 0), stop=False), reads=[onesb.r(), Msk.r(c2)], writes=[ps.r()])
        P.op("pe", lambda e, ps=ps, c=c: e.matmul(ps[:, 0:256], lhsT=sutb[:], rhs=Msk[:, c, :], start=(c == 0), stop=True), reads=[sutb.r(), Msk.r(c)], writes=[ps.r()])
        key, w8 = keyt[c % 2], w8t[c % 2]
        P.op("dve", lambda e, ps=ps, key=key: e.scalar_tensor_tensor(out=key[:], in0=ps[:, 0:256], scalar=1.0, in1=cnt[:, 768:1024], op0=ALU.add, op1=ALU.add), reads=[ps.r()] + cr_, writes=[key.r()])
        P.op("dve", lambda e, key=key, c=c: e.tensor_tensor(out=key[:], in0=key[:], in1=Msk[:, c, :], op=ALU.mult), reads=[key.r(), Msk.r(c)], writes=[key.r()])
        P.op("dve", lambda e, key=key, c=c: e.max(out=d8[:, c, :], in_=key[:]), reads=[key.r()], writes=[d8.r(c)])
        for j in range(8):
            P.op("dve", lambda e, key=key, w8=w8, c=c, j=j: e.scalar_tensor_tensor(out=junk[:], in0=key[:], scalar=d8[:, c, j:j + 1], in1=Wr[:, c, 0:NEXP], op0=ALU.is_equal, op1=ALU.mult,
                                                                                   accum_out=w8[:, j:j + 1]), reads=[key.r(), d8.r(c), Wr.r(c)], writes=[junk.r(), w8.r()])
        P.op("dve", lambda e, c=c: e.tensor_scalar(out=d8[:, c, :], in0=d8[:, c, :], scalar1=-1.0, scalar2=None, op0=ALU.add), reads=[d8.r(c)], writes=[d8.r(c)])
        P.op("dve", lambda e, c=c: e.tensor_copy(out=d8i[:, c, :], in_=d8[:, c, :]), reads=[d8.r(c)], writes=[d8i.r(c)])
        for j in range(8):
            ix, rc = ixt[c % 2][j], rct[c % 2][j]
            P.op("dve", lambda e, ix=ix, c=c, j=j: e.tensor_copy(out=ix[:], in_=d8i[:, c, j:j + 1]), reads=[d8i.r(c)], writes=[ix.r()])
            P.op("pool", lambda e, rc=rc, c=c: e.tensor_copy(out=rc[:, 0:1], in_=tokid[:, c:c + 1]), reads=[tokid.r()], writes=[rc.r()])
            P.op("pool", lambda e, rc=rc, w8=w8, j=j: e.tensor_copy(out=rc[:, 1:2], in_=w8[:, j:j + 1].bitcast(I32)), reads=[w8.r()], writes=[rc.r()])
            P.dma("pool", lambda e, ix=ix, rc=rc: e.indirect_dma_start(out=slottab_d, out_offset=IOA(ap=ix[:, :], axis=0), in_=rc[:], in_offset=None,
                                                                     bounds_check=breg(e, NSLOT - 1), oob_is_err=False), reads=[rc.r(), ix.r()], wacc=[r_slottab])
    A.release(m_bk)

    if stage == 7:
        o1 = dbg_out("d_slottab", [NSLOT, 2], I32)
        P.dma("sp", lambda e: e.dma_start(out=o1, in_=slottab_d), reads=[r_slottab])
        o2 = dbg_out("d_d8i", [128, NT * 8], I32)
        P.dma("sp", lambda e: e.dma_start(out=o2, in_=d8i[:].rearrange("p a b -> p (a b)")), reads=[d8i.r(c) for c in range(NT)])
        o3 = dbg_out("d_widx", [128, NB], I32)
        P.dma("sp", lambda e: e.dma_start(out=o3, in_=widx[:]), reads=[widx.r()])
        o5 = dbg_out("d_W", [L, NEXP + 1], F32)
        P.dma("sp", lambda e: e.dma_start(out=o5.rearrange("(n p) d -> p n d", p=128), in_=Wr[:]), reads=[Wr.r(c) for c in range(NT)] + [Wr.r("sh")])
        o4 = dbg_out("d_cnt", [128, 1024], F32)
        P.dma("sp", lambda e: e.dma_start(out=o4, in_=cnt[:]), reads=cr_)
        P.wait_all_dma("sp")
        P.build()
        return nc

    m_blk = A.mark()
    NBUF = 4
    NFE = 6
    wgu = [A.alloc(f"wgu{i}", [128, 2, 2048], BF16, at=hT_off + i * 8192) for i in range(NBUF)]
    ysb = [A.alloc(f"ysb{i}", [128, D], F32) for i in range(2)]
    wixt = [A.alloc(f"wix{i}", [128, 1], I32) for i in range(NBUF)]
    stt = [A.alloc(f"st{i}", [128, 2], I32) for i in range(NFE)]
    stk = [A.alloc(f"stk{i}", [128, 1], I32) for i in range(NFE)]
    Xg = [A.alloc(f"Xg{i}", [128, D], BF16) for i in range(NFE)]
    XgT = [A.alloc(f"XgT{i}", [128, 8, 128], BF16) for i in range(2)]
    sgs = [A.alloc(f"sgs{i}", [128, 256], BF16) for i in range(2)]
    aTk = [A.alloc(f"aTk{i}", [128, 256], BF16) for i in range(2)]
    aTs = [A.alloc(f"aTs{i}", [128, 256], BF16) for i in range(2)]
    a2 = AB[:, 1, 0:1]
    A2b = bass.AP(a2.tensor, a2.offset, [list(a2.ap[0]), [1, 8], [0, 128]])
    b2 = modcol[:, 2, 0:1]
    B2b = bass.AP(b2.tensor, b2.offset, [list(b2.ap[0]), [1, 8], [0, 128]])
    nblk_run = NB if stage >= 99 else 4

    def front_end(b):
        s_, sk, xg = stt[b % NFE], stk[b % NFE], Xg[b % NFE]
        P.dma("sp", lambda e, s_=s_, b=b: e.dma_start(out=s_[:], in_=slottab_d[b * 128:(b + 1) * 128, :]), reads=[r_slottab], writes=[s_.r()])
        P.op("dve", lambda e, sk=sk, s_=s_: e.tensor_copy(out=sk[:], in_=s_[:, 0:1]), reads=[s_.r()], writes=[sk.r()])
        P.dma("pool", lambda e, xg=xg, sk=sk: e.indirect_dma_start(out=xg[:], out_offset=None, in_=xn2_d, in_offset=IOA(ap=sk[:, :], axis=0), bounds_check=breg(e, L - 1), oob_is_err=False),
              reads=[sk.r(), r_xn2], writes=[xg.r()])

    NWD = 5
    wdn = [A.alloc(f"wdn4_{i}", [128, 2048], BF16) for i in range(NWD)]

    def load_blk(b):
        wb, wd_, wx = wgu[b % NBUF], wdn[b % NWD], wixt[b % NBUF]
        P.op("pool", lambda e, wx=wx, b=b: e.tensor_copy(out=wx[:], in_=widx[:, b:b + 1]), reads=[widx.r()], writes=[wx.r()])
        for dst, src_d, rg in ((wb[:, 0, :], ewg_d, wb.r("g")), (wb[:, 1, :], ewu_d, wb.r("u")), (wd_[:], ewd_d, wd_.r())):
            P.dma("pool", lambda e, dst=dst, src_d=src_d, wx=wx: e.indirect_dma_start(out=dst, out_offset=None, in_=src_d, in_offset=IOA(ap=wx[:, :], axis=0),
                                                                                 bounds_check=breg(e, n_exp * 128 - 1), oob_is_err=False), reads=[wx.r()], writes=[rg])

    def st_T(b):
        xg, xt = Xg[b % NFE], XgT[b % 2]
        pst = bank()
        pstb = pst.t[:].bitcast(BF16)
        for k in range(8):
            P.op("pe", lambda e, pstb=pstb, xg=xg, k=k: e.transpose(pstb[:, k * 128:(k + 1) * 128], xg[:, k * 128:(k + 1) * 128], identb_t[:]), reads=[xg.r(), identb_t.r()], writes=[pst.r()])
        P.op("dve", lambda e, pstb=pstb, xt=xt: e.tensor_tensor(out=xt[:], in0=pstb[:, 0:1024].rearrange("p (a b) -> p a b", a=8), in1=A2b, op=ALU.mult), reads=[pst.r(), AB.r()], writes=[xt.r()])
        P.op("dve", lambda e, xt=xt: e.tensor_tensor(out=xt[:], in0=xt[:], in1=B2b, op=ALU.add), reads=[xt.r(), modcol.r()], writes=[xt.r()])

    def st_MM(b):
        wb, s_, xt = wgu[b % NBUF], stt[b % NFE], XgT[b % 2]
        psgu = bank()
        for k in range(8):
            wv = wb[:, 0, k * 256:k * 256 + 1]
            rhs = bass.AP(wv.tensor, wv.offset, [list(wv.ap[0]), [2048, 2], [1, 256]])
            P.op("pe", lambda e, psgu=psgu, xt=xt, k=k, rhs=rhs: e.matmul(psgu[:].rearrange("p (a b) -> p a b", a=2), lhsT=xt[:, k, :], rhs=rhs, start=(k == 0), stop=(k == 7)),
                 reads=[wb.r("g"), wb.r("u"), xt.r()], writes=[psgu.r()])
        sg, ak = sgs[b % 2], aTk[b % 2]
        swc = s_[:, 1:2].bitcast(F32)
        P.op("act", lambda e, psgu=psgu, sg=sg: e.activation(out=sg[:], in_=psgu[:, 0:256], func=AF.Silu), reads=[psgu.r()], writes=[sg.r()])
        P.op("dve", lambda e, psgu=psgu, sg=sg, ak=ak, swc=swc: e.scalar_tensor_tensor(out=ak[:], in0=sg[:], scalar=swc, in1=psgu[:, 256:512], op0=ALU.mult, op1=ALU.mult),
             reads=[psgu.r(), sg.r(), s_.r()], writes=[ak.r()])

    def st_T2(b):
        ak, at_ = aTk[b % 2], aTs[b % 2]
        psa = bank()
        psab = psa.t[:].bitcast(BF16)
        for f in range(2):
            P.op("pe", lambda e, psab=psab, ak=ak, f=f: e.transpose(psab[:, f * 128:(f + 1) * 128], ak[:, f * 128:(f + 1) * 128], identb_t[:]), reads=[ak.r(), identb_t.r()], writes=[psa.r()])
        P.op("act", lambda e, psab=psab, at_=at_: e.copy(out=at_[:], in_=psab[:, 0:256]), reads=[psa.r()], writes=[at_.r()])

    def st_MM2(b):
        at_, wd_, ys = aTs[b % 2], wdn[b % NWD], ysb[b % 2]
        for half in range(2):
            psy = bank()
            for f in range(2):
                P.op("pe", lambda e, psy=psy, at_=at_, wd_=wd_, f=f, half=half: e.matmul(psy[:], lhsT=at_[:, f * 128:(f + 1) * 128], rhs=wd_[:, f * 1024 + half * 512:f * 1024 + (half + 1) * 512],
                                                                                       start=(f == 0), stop=(f == 1)), reads=[at_.r(), wd_.r()], writes=[psy.r()])
            if half == 0:
                P.op("act", lambda e, psy=psy, ys=ys: e.copy(out=ys[:, 0:512], in_=psy[:]), reads=[psy.r()], writes=[ys.r(0)])
            else:
                P.op("dve", lambda e, psy=psy, ys=ys: e.tensor_copy(out=ys[:, 512:1024], in_=psy[:]), reads=[psy.r()], writes=[ys.r(1)])
        P.dma("act", lambda e, ys=ys, b=b: e.dma_start(out=yslots_d[b * 128:(b + 1) * 128, :], in_=ys[:]), reads=[ys.r(0), ys.r(1)], wacc=[r_yslots])

    for b in range(min(4, nblk_run)):
        front_end(b)
    for b in range(min(3, nblk_run)):
        load_blk(b)
    st_T(0)
    for b in range(nblk_run):
        if b + 4 < nblk_run:
            front_end(b + 4)
        if b + 3 < nblk_run:
            load_blk(b + 3)
        st_MM(b)
        if b + 1 < nblk_run:
            st_T(b + 1)
        st_T2(b)
        if b >= 1:
            st_MM2(b - 1)
    st_MM2(nblk_run - 1)

    A.release(m_blk)
    gbs = [[A.alloc(f"gb{s_}_{i}", [128, D], F32, at=(hT_off if s_ == 0 else mixedT_off) + i * 4096) for i in range(8)] for s_ in range(2)]
    ixgs = [[A.alloc(f"ixg{s_}_{j}", [128, 1], I32) for j in range(8)] for s_ in range(2)]
    for c in range(NT if stage >= 99 else 0):
        gb, ixg = gbs[c % 2], ixgs[c % 2]
        for j in range(8):
            P.op("dve", lambda e, c=c, j=j, ixg=ixg: e.tensor_copy(out=ixg[j][:], in_=d8i[:, c, j:j + 1]), reads=[d8i.r(c)], writes=[ixg[j].r()])
            P.dma("pool", lambda e, j=j, gb=gb, ixg=ixg: e.indirect_dma_start(out=gb[j][:], out_offset=None, in_=yslots_d, in_offset=IOA(ap=ixg[j][:, :], axis=0), bounds_check=breg(e, NSLOT - 1), oob_is_err=False),
                  reads=[ixg[j].r(), r_yslots], writes=[gb[j].r()])
        for (a_, b_, eng) in ((0, 1, "dve"), (2, 3, "pool"), (4, 5, "dve"), (6, 7, "pool"), (0, 2, "dve"), (4, 6, "pool"), (0, 4, "dve")):
            P.op(eng, lambda e, a_=a_, b_=b_, gb=gb: e.tensor_tensor(out=gb[a_][:], in0=gb[a_][:], in1=gb[b_][:], op=ALU.add), reads=[gb[a_].r(), gb[b_].r()], writes=[gb[a_].r()])
        P.op("dve", lambda e, gb=gb: e.tensor_tensor(out=gb[0][:], in0=gb[0][:], in1=g12[:, 1, :], op=ALU.mult), reads=[gb[0].r(), g12.r(1)], writes=[gb[0].r()])
        P.op("pool", lambda e, c=c, gb=gb: e.tensor_tensor(out=acc[:, c, :], in0=acc[:, c, :], in1=gb[0][:], op=ALU.add), reads=[gb[0].r(), acc.r((c, 0)), acc.r((c, 1))], writes=[acc.r((c, 0)), acc.r((c, 1))])

    fgb = A.alloc("fgb", [128, D], F32)
    P.dma("sp", lambda e: e.dma_start(out=fgb[:], in_=bass.AP(fg_d.tensor, fg_d.offset, [[0, 128], [1, D]])), writes=[fgb.r()])
    otl = [A.alloc(f"otl{i}", [128, D], F32) for i in range(2)]
    out_v = out_d.rearrange("(n p) d -> p n d", p=128)
    for c in range(NT):
        accr = [acc.r((c, 0)), acc.r((c, 1))]
        P.op("act", lambda e, c=c: e.activation(out=sq[:], in_=acc[:, c, :], func=AF.Square, accum_out=ssq[:, c:c + 1]), reads=accr, writes=[sq.r(), ssq.r(c // 4)])
        rs = rstd[:, c:c + 1]
        P.op("dve", lambda e, rs=rs, c=c: e.tensor_scalar(out=rs, in0=ssq[:, c:c + 1], scalar1=1.0 / D, scalar2=EPS, op0=ALU.mult, op1=ALU.add), reads=[ssq.r(c // 4)], writes=[rstd.r(c // 4)])
        P.op("act", lambda e, rs=rs: e.activation(out=rs, in_=rs, func=AF.Sqrt), reads=[rstd.r(c // 4)], writes=[rstd.r(c // 4)])
        P.op("dve", lambda e, rs=rs: e.reciprocal(out=rs, in_=rs), reads=[rstd.r(c // 4)], writes=[rstd.r(c // 4)])
        ot = otl[c % 2]
        P.op("dve", lambda e, ot=ot, rs=rs, c=c: e.scalar_tensor_tensor(out=ot[:], in0=acc[:, c, :], scalar=rs, in1=fgb[:], op0=ALU.mult, op1=ALU.mult),
             reads=accr + [rstd.r(c // 4), fgb.r()], writes=[ot.r()])
        P.dma("sp", lambda e, ot=ot, c=c: e.dma_start(out=out_v[:, c, :], in_=ot[:]), reads=[ot.r()])

    P.wait_all_dma("sp")
    P.build()
    return nc


def make_consts():
    c = np.zeros((128, 1024), np.float32)
    c[:, 0:128] = np.eye(128, dtype=np.float32)
    k = np.arange(128)
    c[k, 128 + (k + 64) % 128] = 1.0
    c[:64, 256] = 1.0
    c[64:, 256] = -1.0
    c[:64, 257] = -1.0
    c[64:, 257] = 1.0
    for gl in range(8):
        c[gl * 16:(gl + 1) * 16, 264 + gl] = 1.0
    c[:, 272:283] = (2.0 ** np.arange(11))[None, :]
    oh = np.zeros((32, 8), np.float32)
    oh[np.arange(32), np.arange(32) % 8] = 1.0
    c[:, 288:544] = oh.reshape(1, 256)
    j = np.arange(128)[:, None]
    i = np.arange(128)[None, :]
    c[:, 544:672] = (j <= i).astype(np.float32)
    c[:, 680:696] = (128.0 * np.arange(16))[None, :]
    c[:, 704:832] = (j < i).astype(np.float32)
    c[:, 840] = np.arange(128)
    return c


PER_BATCH = ("x", "c")
SHARED = ("ada_w", "ada_b", "norm1_g", "w_in", "s5_lam_re", "s5_lam_im", "s5_log_dt", "s5_b_re", "s5_b_im", "s5_c_re", "s5_c_im", "s5_d", "s5_w_glu", "s5_b_glu", "w_proj_a", "gla_w_gk2", "gla_b_gk2", "gla_norm_g", "w_proj_b", "w_out", "norm2_g", "router_w", "router_bias")
RESHAPE = {"s5_c_re": (512, 64), "s5_c_im": (512, 64), "s5_d": (512,)}


def make_inmaps(inputs, cores=range(8), n_exp=NEXP + 1):
    consts = make_consts()
    shared = {k: np.ascontiguousarray(np.asarray(inputs[k])[0]) for k in SHARED}
    for k, shp in RESHAPE.items():
        shared[k] = shared[k].reshape(shp)
    shared["final_g"] = np.ascontiguousarray(np.asarray(inputs["final_g"]))
    if "exp_w_gate" in inputs:
        for k, ks in (("exp_w_gate", "sh_w_gate"), ("exp_w_up", "sh_w_up"), ("exp_w_down", "sh_w_down")):
            full = np.concatenate([np.asarray(inputs[k])[0], np.asarray(inputs[ks])], axis=0) if n_exp == NEXP + 1 else np.asarray(inputs[k])[0]
            full = full[:n_exp]
            ne, kd, nn = full.shape
            shared[k] = np.ascontiguousarray(full.reshape(ne, kd // 128, 128, nn).transpose(0, 2, 1, 3)).reshape(ne * 128, (kd // 128) * nn)
    maps = []
    for b in cores:
        m = {k: np.ascontiguousarray(np.asarray(inputs[k])[b]) for k in PER_BATCH}
        m.update(shared)
        m["consts"] = consts
        maps.append(m)
    return maps


def kernel(**inputs):
    nc = build_program()
    res = run_bass_kernel_spmd(nc, make_inmaps(inputs), core_ids=list(range(8)))
    return np.stack([np.asarray(r["out"]) for r in res.results], axis=0)
```

```python
import numpy as np
import concourse.bass as bass
import concourse.mybir as mybir
from concourse.bass_utils import run_bass_kernel_spmd

F32 = mybir.dt.float32
BF16 = mybir.dt.bfloat16
I32 = mybir.dt.int32
AF = mybir.ActivationFunctionType
ALU = mybir.AluOpType
AX = mybir.AxisListType
DTSIZE = {F32: 4, BF16: 2, I32: 4}

COMPUTE = ("pe", "act", "dve", "pool")
NDMASEM = 16

D = 1024
L = 2048
NT = 16
EPS = 1e-6
S5W = 512
NG = 32
IN_W = 4112
OFF_U, OFF_Q, OFF_K, OFF_V, OFF_GK, OFF_R, OFF_GA, OFF_GB = 0, 512, 768, 1024, 1536, 1552, 2064, 3088
NEXP = 256
TWO_PI = 6.283185307179586


class Reg:
    __slots__ = ("name", "lw", "rd")

    def __init__(self, name, inherit=None):
        self.name = name
        self.lw = {}
        self.rd = dict(inherit) if inherit else {}


class Prog:
    def __init__(self, nc):
        self.nc = nc
        self.q = {e: [] for e in COMPUTE + ("sp",)}
        self.cnt = {e: 0 for e in COMPUTE}
        self.known = {e: {} for e in COMPUTE + ("sp",)}
        self.targets = {e: set() for e in COMPUTE}
        self.ndma = {e: 0 for e in ("sp", "act", "pool")}
        self.sems = {}
        self.dmasems = {}

    def _deps(self, eng, reads, writes, wacc=()):
        deps = {}
        for r in reads:
            for k, v in r.lw.items():
                if deps.get(k, 0) < v:
                    deps[k] = v
        for w in writes:
            for k, v in w.lw.items():
                if deps.get(k, 0) < v:
                    deps[k] = v
            for k, v in w.rd.items():
                if deps.get(k, 0) < v:
                    deps[k] = v
        for w in wacc:
            for k, v in w.rd.items():
                if deps.get(k, 0) < v:
                    deps[k] = v
        waits = []
        kn = self.known[eng]
        for k, v in deps.items():
            if k == "pe" and eng == "pe" and not getattr(self, "pe_selfsync", False):
                continue
            if kn.get(k, 0) >= v:
                continue
            kn[k] = v
            waits.append((k, v))
            if k in COMPUTE:
                self.targets[k].add(v)
        return waits

    def _mark(self, tok, reads, writes, wacc=()):
        k, v = tok
        for r in reads:
            if r.rd.get(k, 0) < v:
                r.rd[k] = v
        for w in writes:
            w.lw = {k: v}
            w.rd = {}
        for w in wacc:
            if w.lw.get(k, 0) < v:
                w.lw[k] = v

    def op(self, eng, fn, reads=(), writes=(), wacc=(), pmode="full"):
        waits = self._deps(eng, reads, writes, wacc)
        if eng == "pe":
            if pmode != getattr(self, "last_pmode", "full") and self.cnt["pe"] > 0:
                prev = self.cnt["pe"]
                if self.known["pe"].get("pe", 0) < prev:
                    self.known["pe"]["pe"] = prev
                    waits.append(("pe", prev))
                    self.targets["pe"].add(prev)
            self.last_pmode = pmode
        self.cnt[eng] += 1
        idx = self.cnt[eng]
        self.q[eng].append(("op", waits, fn, idx))
        self._mark((eng, idx), reads, writes, wacc)
        return idx

    def dma(self, eng, fn, reads=(), writes=(), wacc=()):
        waits = self._deps(eng, reads, writes, wacc)
        d = self.ndma[eng]
        self.ndma[eng] += 1
        si = d % NDMASEM
        key = ("dma", eng, si)
        val = 16 * (d // NDMASEM + 1)
        if d >= NDMASEM:
            pv = val - 16
            if self.known[eng].get(key, 0) < pv:
                self.known[eng][key] = pv
                waits.append((key, pv))
        self.q[eng].append(("dma", waits, fn, (key, val)))
        self._mark((key, val), reads, writes, wacc)
        return (key, val)

    def wait_all_dma(self, eng):
        waits = []
        for qe, n in self.ndma.items():
            for si in range(min(n, NDMASEM)):
                cntsi = (n - 1 - si) // NDMASEM + 1
                waits.append((("dma", qe, si), 16 * cntsi))
        self.q[eng].append(("wait", waits, None, None))

    def build(self):
        nc = self.nc
        for e in COMPUTE:
            self.sems[e] = nc.alloc_semaphore(name=f"sem_{e}")
        for qe, n in self.ndma.items():
            for si in range(min(n, NDMASEM)):
                self.dmasems[("dma", qe, si)] = nc.alloc_semaphore(name=f"dsem_{qe}_{si}")
        rank = {}
        for e in COMPUTE:
            rank[e] = {v: i + 1 for i, v in enumerate(sorted(self.targets[e]))}
        handles = {"pe": "tensor", "act": "scalar", "dve": "vector", "pool": "gpsimd", "sp": "sync"}

        def replay(e, h):
            for kind, waits, fn, tok in self.q[e]:
                for k, v in waits:
                    if k in COMPUTE:
                        h.wait_ge(self.sems[k], rank[k][v])
                    else:
                        h.wait_ge(self.dmasems[k], v)
                if kind == "op":
                    ins = fn(h)
                    if tok in rank[e]:
                        ins.then_inc(self.sems[e], 1)
                elif kind == "dma":
                    ins = fn(h)
                    ins.then_inc(self.dmasems[tok[0]], 16)

        with nc.Block() as block:
            for e in COMPUTE + ("sp",):
                if not self.q[e]:
                    continue

                def mk(e):
                    def f(h):
                        replay(e, h)
                    return f
                getattr(block, handles[e])(mk(e))


class Tile:
    def __init__(self, t, name, inherit):
        self.t = t
        self.name = name
        self.inherit = inherit
        self.regs = {}

    def r(self, key=None):
        if key not in self.regs:
            self.regs[key] = Reg(f"{self.name}:{key}", self.inherit)
        return self.regs[key]

    def __getitem__(self, idx):
        return self.t[idx]


class Arena:
    def __init__(self, nc, base=16512, top=229344):
        self.nc = nc
        self.ptr = base
        self.top = top
        self.hist = []
        self.n = 0

    def alloc(self, name, shape, dt, at=None):
        size = int(np.prod(shape[1:])) * DTSIZE[dt]
        size = (size + 31) // 32 * 32
        if at is None:
            start, end = self.ptr, self.ptr + size
            self.ptr = end
        else:
            start, end = at, at + size
        assert end <= self.top, f"SBUF overflow allocating {name}: {end} > {self.top}"
        self.n += 1
        t = self.nc.alloc_sbuf_tensor_at(f"{name}_{self.n}", list(shape), dt, offset=start)
        inherit = {}
        keep = []
        for (s, e, old) in self.hist:
            if s < end and start < e:
                for rg in old.regs.values():
                    for k, v in rg.lw.items():
                        inherit[k] = max(inherit.get(k, 0), v)
                    for k, v in rg.rd.items():
                        inherit[k] = max(inherit.get(k, 0), v)
                for k, v in old.inherit.items():
                    inherit[k] = max(inherit.get(k, 0), v)
            keep.append((s, e, old))
        T = Tile(t, name, inherit)
        self.hist = keep + [(start, end, T)]
        return T

    def mark(self):
        return self.ptr

    def release(self, m):
        self.ptr = m


def bcast_free(ap, n):
    return bass.AP(ap.tensor, ap.offset, [list(ap.ap[0]), [0, n]])


def build_program(stage=99, n_exp=NEXP + 1):
    nc = bass.Bass("TRN2", target_bir_lowering=False)
    P = Prog(nc)
    A = Arena(nc)

    def din(name, shape, dt=F32):
        return nc.dram_tensor(name, list(shape), dt, kind="ExternalInput").ap()

    x_d = din("x", [L, D])
    c_d = din("c", [D])
    adaw_d = din("ada_w", [D, 6 * D])
    adab_d = din("ada_b", [6 * D])
    n1g_d = din("norm1_g", [D])
    win_d = din("w_in", [D, IN_W])
    consts_d = din("consts", [128, 1024])
    lamre_d = din("s5_lam_re", [NG, 64])
    lamim_d = din("s5_lam_im", [NG, 64])
    logdt_d = din("s5_log_dt", [NG])
    bre_d = din("s5_b_re", [NG, 64, 16])
    bim_d = din("s5_b_im", [NG, 64, 16])
    cre_d = din("s5_c_re", [NG * 16, 64])
    cim_d = din("s5_c_im", [NG * 16, 64])
    s5d_d = din("s5_d", [S5W])
    wglu_d = din("s5_w_glu", [S5W, S5W])
    bglu_d = din("s5_b_glu", [S5W])
    wpa_d = din("w_proj_a", [S5W, D])
    wgk2_d = din("gla_w_gk2", [16, 256])
    bgk2_d = din("gla_b_gk2", [256])
    gng_d = din("gla_norm_g", [128])
    wpb_d = din("w_proj_b", [512, D])
    wout_d = din("w_out", [D, D])
    n2g_d = din("norm2_g", [D])
    rw_d = din("router_w", [D, NEXP])
    rb_d = din("router_bias", [NEXP])
    if stage >= 7:
        ewg_d = din("exp_w_gate", [n_exp * 128, 2048])
        ewu_d = din("exp_w_up", [n_exp * 128, 2048])
        ewd_d = din("exp_w_down", [n_exp * 128, 2048])
    fg_d = din("final_g", [D])
    out_d = nc.dram_tensor("out", [L, D], F32, kind="ExternalOutput").ap()
    dbg = {}

    def dbg_out(name, shape, dt=F32):
        dbg[name] = nc.dram_tensor(name, list(shape), dt, kind="ExternalOutput").ap()
        return dbg[name]

    banks = []
    for i in range(8):
        pt = nc.alloc_psum_tensor(f"bank{i}", [128, 512], F32)
        banks.append(Tile(pt, f"bank{i}", {}))
    bank_i = [0]

    def bank():
        b = banks[bank_i[0] % 8]
        bank_i[0] += 1
        return b

    cst = A.alloc("cst", [128, 1024], F32)
    P.dma("sp", lambda e: e.dma_start(out=cst[:], in_=consts_d), writes=[cst.r()])
    identf = cst[:, 0:128]
    identb_t = A.alloc("identb", [128, 128], BF16)
    P.op("dve", lambda e: e.tensor_copy(out=identb_t[:], in_=identf), reads=[cst.r()], writes=[identb_t.r()])

    cT = A.alloc("cT", [128, 8], F32)
    P.dma("sp", lambda e: e.dma_start(out=cT[:], in_=c_d.rearrange("(p k) -> p k", k=8)), writes=[cT.r()])
    scT = A.alloc("scT", [128, 8], F32)
    P.op("act", lambda e: e.activation(out=scT[:], in_=cT[:], func=AF.Silu), reads=[cT.r()], writes=[scT.r()])
    colp = A.alloc("colp", [128, 8, 8], F32)
    P.dma("sp", lambda e: e.dma_start(out=colp[:, 0, :], in_=n1g_d.rearrange("(k p) -> p k", p=128), allow_slow_non_contiguous=True), writes=[colp.r()])
    P.dma("sp", lambda e: e.dma_start(out=colp[:, 3, :], in_=n2g_d.rearrange("(k p) -> p k", p=128), allow_slow_non_contiguous=True), writes=[colp.r()])
    for j, off in ((1, 0), (2, D), (4, 3 * D), (5, 4 * D)):
        P.dma("sp", (lambda j, off: lambda e: e.dma_start(
            out=colp[:, j, :], in_=adab_d[off:off + D].rearrange("(k p) -> p k", p=128), allow_slow_non_contiguous=True))(j, off), writes=[colp.r()])
    modcol = A.alloc("modcol", [128, 4, 8], F32)
    g12 = A.alloc("g12", [128, 2, 1024], F32)
    m_ada = A.mark()
    rowb = A.alloc("rowb", [128, 2, 1024], F32)
    for j, off in ((0, 2 * D), (1, 5 * D)):
        src = adab_d[off:off + D]
        srcb = bass.AP(src.tensor, src.offset, [[0, 128], [1, D]])
        P.dma("sp", (lambda j, srcb: lambda e: e.dma_start(out=rowb[:, j, :], in_=srcb))(j, srcb), writes=[rowb.r(j)])
    adaw_v = adaw_d.rearrange("(p k) n -> p k n", k=8)
    colidx = {0: 0, 1: 1, 3: 2, 4: 3}
    rowidx = {2: 0, 5: 1}
    wblk = [A.alloc(f"adaw{i}", [128, 8, 512], F32) for i in range(2)]
    nb = 0
    for blk in range(6):
        for half in range(2):
            wb = wblk[nb % 2]
            nb += 1
            n0 = blk * 1024 + half * 512
            P.dma("sp", (lambda wb, n0: lambda e: e.dma_start(out=wb[:], in_=adaw_v[:, :, n0:n0 + 512]))(wb, n0),
                  writes=[wb.r()])
            if blk in rowidx:
                ps = bank()
                for k in range(8):
                    P.op("pe", (lambda ps, wb, k: lambda e: e.matmul(
                        ps[:], lhsT=bcast_free(scT[:, k:k + 1], 128), rhs=wb[:, k, :], start=(k == 0), stop=(k == 7)))(ps, wb, k),
                        reads=[scT.r(), wb.r()], writes=[ps.r()], pmode="f32")
                j = rowidx[blk]
                P.op("dve", (lambda ps, j, half: lambda e: e.tensor_tensor(
                    out=g12[:, j, half * 512:(half + 1) * 512], in0=ps[:], in1=rowb[:, j, half * 512:(half + 1) * 512], op=ALU.add))(ps, j, half),
                    reads=[ps.r(), rowb.r(j)], writes=[g12.r(j)])
            else:
                ps = bank()
                j = colidx[blk]
                for cc in range(4):
                    for k in range(8):
                        P.op("pe", (lambda ps, wb, cc, k: lambda e: e.matmul(
                            ps[:, cc:cc + 1], lhsT=wb[:, k, cc * 128:(cc + 1) * 128], rhs=scT[:, k:k + 1],
                            start=(k == 0), stop=(k == 7)))(ps, wb, cc, k),
                            reads=[scT.r(), wb.r()], writes=[ps.r()], pmode="f32")
                jb = {0: 1, 1: 2, 2: 4, 3: 5}[j]
                P.op("dve", (lambda ps, j, jb, half: lambda e: e.tensor_tensor(
                    out=modcol[:, j, half * 4:(half + 1) * 4], in0=ps[:, 0:4], in1=colp[:, jb, half * 4:(half + 1) * 4], op=ALU.add))(ps, j, jb, half),
                    reads=[ps.r(), colp.r()], writes=[modcol.r()])
    A.release(m_ada)
    AB = A.alloc("AB", [128, 2, 8], F32)
    for j, (jg, jsc) in enumerate(((0, 1), (3, 3))):
        P.op("dve", (lambda j, jg, jsc: lambda e: e.scalar_tensor_tensor(
            out=AB[:, j, :], in0=modcol[:, jsc, :], scalar=1.0, in1=colp[:, jg, :], op0=ALU.add, op1=ALU.mult))(j, jg, jsc),
            reads=[modcol.r(), colp.r()], writes=[AB.r()])

    if stage == 0:
        o1 = dbg_out("d_modcol", [128, 32]); o2 = dbg_out("d_g12", [128, 2048]); o3 = dbg_out("d_AB", [128, 16])
        P.dma("sp", lambda e: e.dma_start(out=o1, in_=modcol[:].rearrange("p a b -> p (a b)")), reads=[modcol.r()])
        P.dma("sp", lambda e: e.dma_start(out=o2, in_=g12[:].rearrange("p a b -> p (a b)")), reads=[g12.r(0), g12.r(1)])
        P.dma("sp", lambda e: e.dma_start(out=o3, in_=AB[:].rearrange("p a b -> p (a b)")), reads=[AB.r()])
        P.wait_all_dma("sp")
        P.build()
        return nc


    import math

    def fv(tile, col0, dims, p0=0, np_=128):
        a = tile[p0:p0 + np_, col0:col0 + 1]
        return bass.AP(a.tensor, a.offset, [list(a.ap[0])] + [[st, ct] for st, ct in dims])

    ev_i = [0]

    def evac_copy(out_ap, in_ap, reads, writes):
        ev_i[0] += 1
        if ev_i[0] % 2:
            P.op("dve", lambda e: e.tensor_copy(out=out_ap, in_=in_ap), reads=reads, writes=writes)
        else:
            P.op("act", lambda e: e.copy(out=out_ap, in_=in_ap), reads=reads, writes=writes)

    sq = A.alloc("sq", [128, D], F32)
    ssq = A.alloc("ssq", [128, 16], F32)
    rstd = A.alloc("rstd", [128, 16], F32)
    hT_off = A.mark()
    hT = A.alloc("hT", [128, 8, L], BF16)
    hT_regs = lambda n: [hT.r((k, n)) for k in range(8)]
    m_B = A.mark()
    xin = [A.alloc(f"xin{i}", [128, 4, D], F32) for i in range(2)]
    xnb = [A.alloc(f"xnb{i}", [128, 4, D], BF16) for i in range(2)]
    x_v = x_d.rearrange("(n p) d -> p n d", p=128)
    for blk in range(4):
        xi = xin[blk % 2]
        xb = xnb[blk % 2]
        P.dma("sp", lambda e, xi=xi, blk=blk: e.dma_start(out=xi[:], in_=x_v[:, blk * 4:(blk + 1) * 4, :]), writes=[xi.r()])
        for j in range(4):
            P.op("act", lambda e, xi=xi, j=j, blk=blk: e.activation(out=sq[:], in_=xi[:, j, :], func=AF.Square,
                                                                     accum_out=ssq[:, blk * 4 + j:blk * 4 + j + 1]),
                 reads=[xi.r()], writes=[sq.r(), ssq.r(blk)])
        rs = rstd[:, blk * 4:(blk + 1) * 4]
        P.op("dve", lambda e, rs=rs, blk=blk: e.tensor_scalar(out=rs, in0=ssq[:, blk * 4:(blk + 1) * 4], scalar1=1.0 / D, scalar2=EPS,
                                                               op0=ALU.mult, op1=ALU.add), reads=[ssq.r(blk)], writes=[rstd.r(blk)])
        P.op("act", lambda e, rs=rs: e.activation(out=rs, in_=rs, func=AF.Sqrt), reads=[rstd.r(blk)], writes=[rstd.r(blk)])
        P.op("dve", lambda e, rs=rs: e.reciprocal(out=rs, in_=rs), reads=[rstd.r(blk)], writes=[rstd.r(blk)])
        for j in range(4):
            P.op("act", lambda e, xi=xi, xb=xb, j=j, blk=blk: e.activation(out=xb[:, j, :], in_=xi[:, j, :], func=AF.Copy,
                                                                           scale=rstd[:, blk * 4 + j:blk * 4 + j + 1]),
                 reads=[xi.r(), rstd.r(blk)], writes=[xb.r(j)])
        for k in range(8):
            ps = bank()
            psb = ps.t[:].bitcast(BF16)
            for j in range(4):
                P.op("pe", lambda e, psb=psb, xb=xb, j=j, k=k: e.transpose(psb[:, j * 128:(j + 1) * 128], xb[:, j, k * 128:(k + 1) * 128], identb_t[:]),
                     reads=[xb.r(j), identb_t.r()], writes=[ps.r()])
            P.op("dve", lambda e, psb=psb, k=k, blk=blk: e.tensor_scalar(
                out=hT[:, k, blk * 512:(blk + 1) * 512], in0=psb[:, 0:512], scalar1=AB[:, 0, k:k + 1], scalar2=modcol[:, 0, k:k + 1],
                op0=ALU.mult, op1=ALU.add), reads=[ps.r(), AB.r(), modcol.r()], writes=[hT.r((k, blk))])
    A.release(m_B)

    if stage == 1:
        o1 = dbg_out("d_hT", [128, 8 * L], BF16)
        P.dma("sp", lambda e: e.dma_start(out=o1, in_=hT[:].rearrange("p a b -> p (a b)")), reads=[hT.r((k, n)) for k in range(8) for n in range(4)])
        P.wait_all_dma("sp")
        P.build()
        return nc

    m_C = A.mark()
    mixedT_off = A.mark()
    mixedT = A.alloc("mixedT", [128, 8, L], BF16)
    m_C2 = A.mark()
    win_v = win_d.rearrange("(k p) n -> p k n", p=128)
    wu = A.alloc("wu", [128, 8, S5W], BF16)
    P.dma("pool", lambda e: e.dma_start(out=wu[:], in_=win_v[:, :, OFF_U:OFF_U + S5W]), writes=[wu.r()])
    dcol = A.alloc("dcol", [128, 8], F32)
    P.dma("sp", lambda e: e.dma_start(out=dcol[:, 0:4], in_=s5d_d.rearrange("(k p) -> p k", p=128), allow_slow_non_contiguous=True), writes=[dcol.r()])
    P.dma("sp", lambda e: e.dma_start(out=dcol[:, 4:8], in_=bglu_d.rearrange("(k p) -> p k", p=128), allow_slow_non_contiguous=True), writes=[dcol.r()])

    uT = A.alloc("uT", [128, 4, L], BF16)
    for m in range(4):
        for n in range(4):
            ps = bank()
            for k in range(8):
                P.op("pe", lambda e, ps=ps, m=m, n=n, k=k: e.matmul(ps[:], lhsT=wu[:, k, m * 128:(m + 1) * 128], rhs=hT[:, k, n * 512:(n + 1) * 512],
                                                                   start=(k == 0), stop=(k == 7)),
                     reads=[wu.r(), hT.r((k, n))], writes=[ps.r()])
            evac_copy(uT[:, m, n * 512:(n + 1) * 512], ps[:], [ps.r()], [uT.r((m, n))])

    NE = 11
    NW = 32 * NE
    PR = A.alloc("PR", [128, NW], F32)
    PIs = A.alloc("PIs", [128, NW], F32)
    BT = A.alloc("BT", [128, 32, 128], BF16)
    Cpad = A.alloc("Cpad", [128, 32 * 128], F32)
    yT = A.alloc("yT", [128, 4, L], BF16)
    m_tmp = A.mark()
    lam_in = A.alloc("lam_in", [32, 2, 128], F32)
    for j, src in enumerate((lamre_d, lamim_d)):
        for hh in range(2):
            P.dma("sp", lambda e, j=j, hh=hh, src=src: e.dma_start(out=lam_in[:, j, hh * 64:(hh + 1) * 64], in_=src), writes=[lam_in.r()])
    LRLI = A.alloc("LRLI", [128, 64], F32)
    for j in range(2):
        ps = bank()
        P.op("pe", lambda e, ps=ps, j=j: e.transpose(ps[:, 0:32], lam_in[:, j, :], cst[0:32, 0:32]), reads=[lam_in.r(), cst.r()], writes=[ps.r()], pmode="k32")
        P.op("dve", lambda e, ps=ps, j=j: e.tensor_copy(out=LRLI[:, j * 32:(j + 1) * 32], in_=ps[:, 0:32]), reads=[ps.r()], writes=[LRLI.r()])
    DT = A.alloc("DT", [128, 32], F32)
    P.dma("sp", lambda e: e.dma_start(out=DT[:], in_=bass.AP(logdt_d.tensor, logdt_d.offset, [[0, 128], [1, 32]])), writes=[DT.r()])
    P.op("act", lambda e: e.activation(out=DT[:], in_=DT[:], func=AF.Exp), reads=[DT.r()], writes=[DT.r()])
    LDT = A.alloc("LDT", [128, 64], F32)
    for j in range(2):
        P.op("dve", lambda e, j=j: e.tensor_tensor(out=LDT[:, j * 32:(j + 1) * 32], in0=LRLI[:, j * 32:(j + 1) * 32], in1=DT[:], op=ALU.mult),
             reads=[LRLI.r(), DT.r()], writes=[LDT.r()])
    E_b = fv(cst, 272, [(0, 32), (1, NE)])
    ARG = A.alloc("ARG", [128, NW], F32)
    ANG = A.alloc("ANG", [128, NW], F32)
    P.op("dve", lambda e: e.tensor_tensor(out=fv(ARG, 0, [(NE, 32), (1, NE)]), in0=fv(LDT, 0, [(1, 32), (0, NE)]), in1=E_b, op=ALU.mult),
         reads=[LDT.r(), cst.r()], writes=[ARG.r()])
    P.op("dve", lambda e: e.tensor_tensor(out=fv(ANG, 0, [(NE, 32), (1, NE)]), in0=fv(LDT, 32, [(1, 32), (0, NE)]), in1=E_b, op=ALU.mult),
         reads=[LDT.r(), cst.r()], writes=[ANG.r()])
    MAG = A.alloc("MAG", [128, NW], F32)
    P.op("act", lambda e: e.activation(out=MAG[:], in_=ARG[:], func=AF.Exp), reads=[ARG.r()], writes=[MAG.r()])
    tmpf = A.alloc("tmpf", [128, NW], F32)
    tmpm = A.alloc("tmpm", [128, NW], F32)
    tmpi = A.alloc("tmpi", [128, NW], I32)
    SIN = A.alloc("SIN", [128, NW], F32)
    COS = A.alloc("COS", [128, NW], F32)

    def sin_of(shift, out_t):
        rt = [tmpf.r()]
        P.op("dve", lambda e: e.tensor_scalar(out=tmpf[:], in0=ANG[:], scalar1=shift, scalar2=1.0 / TWO_PI, op0=ALU.add, op1=ALU.mult),
             reads=[ANG.r()], writes=rt)
        P.op("dve", lambda e: e.tensor_copy(out=tmpi[:], in_=tmpf[:]), reads=rt, writes=[tmpi.r()])
        P.op("dve", lambda e: e.tensor_copy(out=tmpf[:], in_=tmpi[:]), reads=[tmpi.r()], writes=rt)
        P.op("dve", lambda e: e.scalar_tensor_tensor(out=tmpf[:], in0=tmpf[:], scalar=-TWO_PI, in1=ANG[:], op0=ALU.mult, op1=ALU.add),
             reads=rt + [ANG.r()], writes=rt)
        if shift != 0.0:
            P.op("dve", lambda e: e.tensor_scalar(out=tmpf[:], in0=tmpf[:], scalar1=shift, scalar2=None, op0=ALU.add), reads=rt, writes=rt)
        P.op("dve", lambda e: e.tensor_scalar(out=tmpm[:], in0=tmpf[:], scalar1=math.pi, scalar2=-TWO_PI, op0=ALU.is_gt, op1=ALU.mult),
             reads=rt, writes=[tmpm.r()])
        P.op("dve", lambda e: e.tensor_tensor(out=tmpf[:], in0=tmpf[:], in1=tmpm[:], op=ALU.add), reads=rt + [tmpm.r()], writes=rt)
        P.op("dve", lambda e: e.tensor_scalar(out=tmpm[:], in0=tmpf[:], scalar1=-math.pi, scalar2=TWO_PI, op0=ALU.is_lt, op1=ALU.mult),
             reads=rt, writes=[tmpm.r()])
        P.op("dve", lambda e: e.tensor_tensor(out=tmpf[:], in0=tmpf[:], in1=tmpm[:], op=ALU.add), reads=rt + [tmpm.r()], writes=rt)
        P.op("dve", lambda e: e.tensor_scalar(out=tmpf[:], in0=tmpf[:], scalar1=3.14159, scalar2=-3.14159, op0=ALU.min, op1=ALU.max), reads=rt, writes=rt)
        P.op("act", lambda e: e.activation(out=out_t[:], in_=tmpf[:], func=AF.Sin), reads=rt, writes=[out_t.r()])

    sin_of(0.0, SIN)
    sin_of(math.pi / 2, COS)
    PIu = A.alloc("PIu", [128, NW], F32)
    P.op("dve", lambda e: e.tensor_tensor(out=PR[:], in0=MAG[:], in1=COS[:], op=ALU.mult), reads=[MAG.r(), COS.r()], writes=[PR.r()])
    P.op("dve", lambda e: e.tensor_tensor(out=PIu[:], in0=MAG[:], in1=SIN[:], op=ALU.mult), reads=[MAG.r(), SIN.r()], writes=[PIu.r()])
    P.op("dve", lambda e: e.tensor_scalar(out=PIs[:], in0=PIu[:], scalar1=cst[:, 256:257], scalar2=None, op0=ALU.mult), reads=[PIu.r(), cst.r()], writes=[PIs.r()])
    cf = A.alloc("cf", [128, 256], F32)
    ar = fv(PR, 0, [(NE, 32)])
    ai = fv(PIu, 0, [(NE, 32)])
    lr_ = LRLI[:, 0:32]
    li_ = LRLI[:, 32:64]
    cfr = [cf.r()]
    P.op("dve", lambda e: e.tensor_scalar(out=cf[:, 0:32], in0=ar, scalar1=-1.0, scalar2=None, op0=ALU.add), reads=[PR.r()], writes=cfr)
    P.op("dve", lambda e: e.tensor_tensor(out=cf[:, 32:64], in0=lr_, in1=lr_, op=ALU.mult), reads=[LRLI.r()], writes=cfr)
    P.op("dve", lambda e: e.tensor_tensor(out=cf[:, 64:96], in0=li_, in1=li_, op=ALU.mult), reads=[LRLI.r()], writes=cfr)
    P.op("dve", lambda e: e.tensor_tensor(out=cf[:, 32:64], in0=cf[:, 32:64], in1=cf[:, 64:96], op=ALU.add), reads=cfr, writes=cfr)
    P.op("dve", lambda e: e.reciprocal(out=cf[:, 32:64], in_=cf[:, 32:64]), reads=cfr, writes=cfr)
    P.op("dve", lambda e: e.tensor_tensor(out=cf[:, 64:96], in0=cf[:, 0:32], in1=lr_, op=ALU.mult), reads=cfr + [LRLI.r()], writes=cfr)
    P.op("dve", lambda e: e.tensor_tensor(out=cf[:, 96:128], in0=ai, in1=li_, op=ALU.mult), reads=[PIu.r(), LRLI.r()], writes=cfr)
    P.op("dve", lambda e: e.tensor_tensor(out=cf[:, 64:96], in0=cf[:, 64:96], in1=cf[:, 96:128], op=ALU.add), reads=cfr, writes=cfr)
    P.op("dve", lambda e: e.tensor_tensor(out=cf[:, 128:160], in0=cf[:, 64:96], in1=cf[:, 32:64], op=ALU.mult), reads=cfr, writes=cfr)
    P.op("dve", lambda e: e.tensor_tensor(out=cf[:, 64:96], in0=ai, in1=lr_, op=ALU.mult), reads=[PIu.r(), LRLI.r()], writes=cfr)
    P.op("dve", lambda e: e.tensor_tensor(out=cf[:, 96:128], in0=cf[:, 0:32], in1=li_, op=ALU.mult), reads=cfr + [LRLI.r()], writes=cfr)
    P.op("dve", lambda e: e.tensor_tensor(out=cf[:, 64:96], in0=cf[:, 64:96], in1=cf[:, 96:128], op=ALU.subtract), reads=cfr, writes=cfr)
    P.op("dve", lambda e: e.tensor_tensor(out=cf[:, 160:192], in0=cf[:, 64:96], in1=cf[:, 32:64], op=ALU.mult), reads=cfr, writes=cfr)
    P.op("dve", lambda e: e.tensor_scalar(out=cf[:, 160:192], in0=cf[:, 160:192], scalar1=cst[:, 257:258], scalar2=None, op0=ALU.mult), reads=cfr + [cst.r()], writes=cfr)
    RB = A.alloc("RB", [128, 2, 32, 16], F32)
    bre_v = bre_d.rearrange("g p h -> p g h")
    bim_v = bim_d.rearrange("g p h -> p g h")
    for (p0, var, src) in ((0, 0, bre_v), (64, 0, bim_v), (0, 1, bim_v), (64, 1, bre_v)):
        P.dma("sp", lambda e, p0=p0, var=var, src=src: e.dma_start(out=RB[p0:p0 + 64, var, :, :], in_=src), writes=[RB.r()])
    B1 = A.alloc("B1", [128, 512], F32)
    B1v = fv(B1, 0, [(16, 32), (1, 16)])
    P.op("dve", lambda e: e.tensor_tensor(out=B1v, in0=RB[:, 0, :, :], in1=fv(cf, 4 * 32, [(1, 32), (0, 16)]), op=ALU.mult), reads=[RB.r()] + cfr, writes=[B1.r()])
    P.op("dve", lambda e: e.tensor_tensor(out=RB[:, 1, :, :], in0=RB[:, 1, :, :], in1=fv(cf, 5 * 32, [(1, 32), (0, 16)]), op=ALU.mult), reads=[RB.r()] + cfr, writes=[RB.r()])
    P.op("dve", lambda e: e.tensor_tensor(out=B1v, in0=B1v, in1=RB[:, 1, :, :], op=ALU.add), reads=[RB.r(), B1.r()], writes=[B1.r()])
    for ch in range(4):
        ps = bank()
        P.op("pe", lambda e, ps=ps, ch=ch: e.transpose(ps[:, 0:128], B1[:, ch * 128:(ch + 1) * 128], cst[:, 0:128]), reads=[B1.r(), cst.r()], writes=[ps.r()], pmode="f32")
        for gl in range(8):
            P.op("dve", lambda e, ps=ps, ch=ch, gl=gl: e.tensor_scalar(out=BT[:, ch * 8 + gl, :], in0=ps[:, 0:128], scalar1=cst[:, 264 + gl:265 + gl], scalar2=None, op0=ALU.mult),
                 reads=[ps.r(), cst.r()], writes=[BT.r()])
    Cin = A.alloc("Cin", [128, 4, 128], F32)
    P.dma("sp", lambda e: e.dma_start(out=Cin[:, :, 0:64], in_=cre_d.rearrange("(k r) p -> r k p", r=128)), writes=[Cin.r()])
    P.dma("sp", lambda e: e.dma_start(out=Cin[:, :, 64:128], in_=cim_d.rearrange("(k r) p -> r k p", r=128)), writes=[Cin.r()])
    CT1 = A.alloc("CT1", [128, 512], F32)
    for r4 in range(4):
        ps = bank()
        P.op("pe", lambda e, ps=ps, r4=r4: e.transpose(ps[:, 0:128], Cin[:, r4, :], cst[:, 0:128]), reads=[Cin.r(), cst.r()], writes=[ps.r()], pmode="f32")
        P.op("dve", lambda e, ps=ps, r4=r4: e.tensor_scalar(out=CT1[:, r4 * 128:(r4 + 1) * 128], in0=ps[:, 0:128], scalar1=cst[:, 256:257], scalar2=None, op0=ALU.mult),
             reads=[ps.r(), cst.r()], writes=[CT1.r()])
    P.op("dve", lambda e: e.tensor_tensor(out=fv(Cpad, 0, [(128, 32), (16, 8), (1, 16)]), in0=fv(CT1, 0, [(16, 32), (0, 8), (1, 16)]),
                                          in1=fv(cst, 288, [(8, 32), (1, 8), (0, 16)]), op=ALU.mult), reads=[CT1.r(), cst.r()], writes=[Cpad.r()])

    A.release(m_tmp)
    m_scan = A.mark()
    F32R = mybir.dt.float32r
    r32 = lambda ap: ap.bitcast(F32R)
    PAD = 1024
    identR = A.alloc("identR", [128, 128], F32)
    P.op("dve", lambda e: e.tensor_copy(out=r32(identR[:]), in_=cst[:, 0:128]), reads=[cst.r()], writes=[identR.r()])
    SAB = [A.alloc(f"S{i}", [128, PAD + L], F32) for i in range(2)]
    for t_ in SAB:
        P.op("dve", lambda e, t_=t_: e.tensor_scalar(out=r32(t_[:, 0:PAD]), in0=cst[:, 0:PAD], scalar1=0.0, scalar2=None, op0=ALU.mult), reads=[cst.r()], writes=[t_.r("pad")])
    Rt = [A.alloc(f"R{i}", [128, NE, 128], F32) for i in range(2)]
    ytmp = [A.alloc(f"ytmp{i}", [128, 512], F32) for i in range(2)]
    ybanks = banks[4:8]
    rot_i = [0]

    def rbank():
        b = banks[rot_i[0] % 4]
        rot_i[0] += 1
        return b

    def sregs(t_, lo):
        rr = []
        if lo < 0:
            rr.append(t_.r("pad"))
        for n in range(4):
            if lo < (n + 1) * 512 and n * 512 < lo + 512:
                rr.append(t_.r(n))
        return rr

    for g in range(NG):
        chunk, gl = divmod(g, 8)
        R = Rt[g % 2]
        for k in range(NE):
            P.op("act", lambda e, R=R, k=k, g=g: e.activation(out=r32(R[:, k, :]), in_=cst[:, 0:128], func=AF.Copy, scale=PR[:, g * NE + k:g * NE + k + 1]),
                 reads=[cst.r(), PR.r()], writes=[R.r(k)])
            P.op("dve", lambda e, R=R, k=k, g=g: e.scalar_tensor_tensor(out=r32(R[:, k, :]), in0=cst[:, 128:256], scalar=PIs[:, g * NE + k:g * NE + k + 1], in1=R[:, k, :],
                                                                       op0=ALU.mult, op1=ALU.add), reads=[cst.r(), PIs.r(), R.r(k)], writes=[R.r(k)])
        cur, nxt = SAB
        for n in range(4):
            ps = rbank()
            P.op("pe", lambda e, ps=ps, g=g, chunk=chunk, n=n: e.matmul(ps[:], lhsT=BT[:, g, :], rhs=uT[:, chunk, n * 512:(n + 1) * 512], start=True, stop=True),
                 reads=[BT.r(), uT.r((chunk, n))], writes=[ps.r()])
            evac_copy(r32(cur[:, PAD + n * 512:PAD + (n + 1) * 512]), ps[:], [ps.r()], [cur.r(n)])
        for k in range(NE):
            sh = 1 << k
            for n in range(4):
                lo = n * 512 - sh
                dst = nxt[:, PAD + n * 512:PAD + (n + 1) * 512]
                srcn = cur[:, PAD + n * 512:PAD + (n + 1) * 512]
                if lo + 512 <= 0:
                    P.op("pool", lambda e, dst=dst, srcn=srcn: e.tensor_copy(out=r32(dst), in_=srcn), reads=[cur.r(n)], writes=[nxt.r(n)])
                    continue
                ps = rbank()
                P.op("pe", lambda e, ps=ps, srcn=srcn: e.matmul(ps[:], lhsT=r32(identR[:]), rhs=r32(srcn), start=True, stop=False),
                     reads=[identR.r(), cur.r(n)], writes=[ps.r()], pmode="f32")
                P.op("pe", lambda e, ps=ps, R=R, k=k, cur=cur, lo=lo: e.matmul(ps[:], lhsT=r32(R[:, k, :]), rhs=r32(cur[:, PAD + lo:PAD + lo + 512]), start=False, stop=True),
                     reads=[R.r(k)] + sregs(cur, lo), writes=[ps.r()], pmode="f32")
                evac_copy(r32(dst), ps[:], [ps.r()], [nxt.r(n)])
            cur, nxt = nxt, cur
        for n in range(4):
            yb = ybanks[n]
            P.op("pe", lambda e, yb=yb, g=g, cur=cur, n=n, gl=gl: e.matmul(yb[:], lhsT=Cpad[:, g * 128:(g + 1) * 128], rhs=cur[:, PAD + n * 512:PAD + (n + 1) * 512],
                                                                          start=(gl == 0), stop=(gl == 7)),
                 reads=[Cpad.r(), cur.r(n)], writes=[yb.r()], pmode="f32")
        if gl == 7:
            for n in range(4):
                yb = ybanks[n]
                yt = ytmp[n % 2]
                P.op("dve", lambda e, yb=yb, yt=yt, chunk=chunk, n=n: e.scalar_tensor_tensor(out=yt[:], in0=uT[:, chunk, n * 512:(n + 1) * 512], scalar=dcol[:, chunk:chunk + 1],
                                                                                           in1=yb[:], op0=ALU.mult, op1=ALU.add),
                     reads=[uT.r((chunk, n)), dcol.r(), yb.r()], writes=[yt.r()])
                P.op("act", lambda e, yt=yt, chunk=chunk, n=n: e.activation(out=yT[:, chunk, n * 512:(n + 1) * 512], in_=yt[:], func=AF.Gelu_apprx_tanh),
                     reads=[yt.r()], writes=[yT.r((chunk, n))])

    if stage == 2:
        o1 = dbg_out("d_yT", [128, 4 * L], BF16)
        o2 = dbg_out("d_uT", [128, 4 * L], BF16)
        P.dma("sp", lambda e: e.dma_start(out=o1, in_=yT[:].rearrange("p a b -> p (a b)")), reads=[yT.r((k, n)) for k in range(4) for n in range(4)])
        P.dma("sp", lambda e: e.dma_start(out=o2, in_=uT[:].rearrange("p a b -> p (a b)")), reads=[uT.r((k, n)) for k in range(4) for n in range(4)])
        P.wait_all_dma("sp")
        P.build()
        return nc


    A.release(m_scan)
    wglu = A.alloc("wglu", [128, 4, S5W], BF16)
    P.dma("pool", lambda e: e.dma_start(out=wglu[:], in_=wglu_d.rearrange("(k p) n -> p k n", p=128)), writes=[wglu.r()])
    wpa = A.alloc("wpa", [128, 4, D], BF16)
    P.dma("pool", lambda e: e.dma_start(out=wpa[:], in_=wpa_d.rearrange("(k p) n -> p k n", p=128)), writes=[wpa.r()])
    wga = A.alloc("wga", [128, 8, D], BF16)
    P.dma("pool", lambda e: e.dma_start(out=wga[:], in_=win_v[:, :, OFF_GA:OFF_GA + D]), writes=[wga.r()])
    yaT = A.alloc("yaT", [128, 4, L], BF16)
    sgb = [A.alloc(f"sgb{i}", [128, 512], BF16) for i in range(2)]
    sgf = [A.alloc(f"sgf{i}", [128, 512], F32) for i in range(2)]
    it = 0
    for m in range(4):
        for n in range(4):
            ps = bank()
            for k in range(4):
                P.op("pe", lambda e, ps=ps, m=m, n=n, k=k: e.matmul(ps[:], lhsT=wglu[:, k, m * 128:(m + 1) * 128], rhs=yT[:, k, n * 512:(n + 1) * 512],
                                                                   start=(k == 0), stop=(k == 3)), reads=[wglu.r(), yT.r((k, n))], writes=[ps.r()])
            sg = sgb[it % 2]
            it += 1
            P.op("act", lambda e, ps=ps, sg=sg, m=m: e.activation(out=sg[:], in_=ps[:], func=AF.Sigmoid, bias=dcol[:, 4 + m:5 + m]),
                 reads=[ps.r(), dcol.r()], writes=[sg.r()])
            P.op("dve", lambda e, sg=sg, m=m, n=n: e.tensor_tensor(out=yaT[:, m, n * 512:(n + 1) * 512], in0=yT[:, m, n * 512:(n + 1) * 512], in1=sg[:], op=ALU.mult),
                 reads=[sg.r(), yT.r((m, n))], writes=[yaT.r((m, n))])

    def proj_gate(wp, wg, srcT, first):
        it = 0
        for m in range(8):
            for n in range(4):
                ps1 = bank()
                for k in range(4):
                    P.op("pe", lambda e, ps1=ps1, m=m, n=n, k=k: e.matmul(ps1[:], lhsT=wp[:, k, m * 128:(m + 1) * 128], rhs=srcT[:, k, n * 512:(n + 1) * 512],
                                                                         start=(k == 0), stop=(k == 3)), reads=[wp.r(), srcT.r((k, n))], writes=[ps1.r()])
                ps2 = bank()
                for k in range(8):
                    P.op("pe", lambda e, ps2=ps2, m=m, n=n, k=k: e.matmul(ps2[:], lhsT=wg[:, k, m * 128:(m + 1) * 128], rhs=hT[:, k, n * 512:(n + 1) * 512],
                                                                         start=(k == 0), stop=(k == 7)), reads=[wg.r(), hT.r((k, n))], writes=[ps2.r()])
                sg = sgf[it % 2]
                it += 1
                P.op("act", lambda e, ps2=ps2, sg=sg: e.activation(out=sg[:], in_=ps2[:], func=AF.Sigmoid), reads=[ps2.r()], writes=[sg.r()])
                dst = mixedT[:, m, n * 512:(n + 1) * 512]
                if first:
                    P.op("dve", lambda e, ps1=ps1, sg=sg, dst=dst: e.tensor_tensor(out=dst, in0=ps1[:], in1=sg[:], op=ALU.mult),
                         reads=[ps1.r(), sg.r()], writes=[mixedT.r((m, n))])
                else:
                    P.op("dve", lambda e, ps1=ps1, sg=sg: e.tensor_tensor(out=sg[:], in0=ps1[:], in1=sg[:], op=ALU.mult),
                         reads=[ps1.r(), sg.r()], writes=[sg.r()])
                    P.op("pool", lambda e, sg=sg, dst=dst: e.tensor_tensor(out=dst, in0=dst, in1=sg[:], op=ALU.add),
                         reads=[sg.r(), mixedT.r((m, n))], writes=[mixedT.r((m, n))])

    proj_gate(wpa, wga, yaT, True)
    A.release(m_C2)

    P.pe_selfsync = True
    qtT = A.alloc("qtT", [128, 2, L], BF16)
    ktT = A.alloc("ktT", [128, 2, L], BF16)
    vtok = A.alloc("vtok", [128, NT, 512], BF16)
    rstok = A.alloc("rstok", [128, NT, 512], BF16)
    ktok = A.alloc("ktok", [128, NT, 256], BF16)
    eblast = A.alloc("eblast", [128, 2, NT], F32)
    gsm = A.alloc("gsm", [128, 512], F32)
    m_G = A.mark()
    wgla = A.alloc("wgla", [128, 8, 1552], BF16)
    P.dma("pool", lambda e: e.dma_start(out=wgla[:], in_=win_v[:, :, OFF_Q:OFF_Q + 1552]), writes=[wgla.r()])
    WQ, WK, WV, WGK, WR = 0, 256, 512, 1024, 1040
    wgk2 = A.alloc("wgk2", [16, 256], BF16)
    P.dma("pool", lambda e: e.dma_start(out=wgk2[:], in_=wgk2_d), writes=[wgk2.r()])
    P.dma("sp", lambda e: e.dma_start(out=gsm[:, 0:2], in_=bgk2_d.rearrange("(k p) -> p k", p=128), allow_slow_non_contiguous=True), writes=[gsm.r()])
    P.dma("sp", lambda e: e.dma_start(out=gsm[:, 128:256], in_=bass.AP(gng_d.tensor, gng_d.offset, [[0, 128], [1, 128]])), writes=[gsm.r()])
    P.op("dve", lambda e: e.tensor_scalar(out=gsm[:, 0:2], in0=gsm[:, 0:2], scalar1=-1.0, scalar2=None, op0=ALU.mult), reads=[gsm.r()], writes=[gsm.r()])
    P.op("pool", lambda e: e.memset(gsm[:, 256:384], 1.0), writes=[gsm.r()])
    gkT = A.alloc("gkT", [16, L], BF16)
    for n in range(4):
        ps = bank()
        for k in range(8):
            P.op("pe", lambda e, ps=ps, n=n, k=k: e.matmul(ps[0:16, :], lhsT=wgla[:, k, WGK:WGK + 16], rhs=hT[:, k, n * 512:(n + 1) * 512], start=(k == 0), stop=(k == 7)),
                 reads=[wgla.r(), hT.r((k, n))], writes=[ps.r()], pmode="m16")
        evac_copy(gkT[:, n * 512:(n + 1) * 512], ps[0:16, :], [ps.r()], [gkT.r(n)])
    cum = [A.alloc(f"cum{i}", [128, 512], F32) for i in range(2)]
    ebt = [A.alloc(f"ebt{i}", [128, 512], F32) for i in range(2)]
    enbt = [A.alloc(f"enbt{i}", [128, 512], F32) for i in range(2)]
    it = 0
    for n in range(4):
        for t2 in range(2):
            cu, eb_, enb_ = cum[it % 2], ebt[it % 2], enbt[it % 2]
            it += 1
            ps = bank()
            P.op("pe", lambda e, ps=ps, n=n, t2=t2: e.matmul(ps[:], lhsT=wgk2[:, t2 * 128:(t2 + 1) * 128], rhs=gkT[:, n * 512:(n + 1) * 512], start=True, stop=True),
                 reads=[wgk2.r(), gkT.r(n)], writes=[ps.r()], pmode="k16")
            P.op("act", lambda e, ps=ps, cu=cu, t2=t2: e.activation(out=cu[:], in_=ps[:], func=AF.Exp, scale=-1.0, bias=gsm[:, t2:t2 + 1]), reads=[ps.r(), gsm.r()], writes=[cu.r()])
            P.op("act", lambda e, cu=cu: e.activation(out=cu[:], in_=cu[:], func=AF.Ln, bias=1.0), reads=[cu.r()], writes=[cu.r()])
            for c4 in range(4):
                P.op("dve", lambda e, cu=cu, c4=c4: e.tensor_tensor_scan(out=cu[:, c4 * 128:(c4 + 1) * 128], data0=gsm[:, 256:384], data1=cu[:, c4 * 128:(c4 + 1) * 128],
                                                                        initial=0.0, op0=ALU.mult, op1=ALU.add), reads=[cu.r(), gsm.r()], writes=[cu.r()])
            P.op("act", lambda e, cu=cu, eb_=eb_: e.activation(out=eb_[:], in_=cu[:], func=AF.Exp, scale=-1.0 / 16.0), reads=[cu.r()], writes=[eb_.r()])
            P.op("act", lambda e, cu=cu, enb_=enb_: e.activation(out=enb_[:], in_=cu[:], func=AF.Exp, scale=1.0 / 16.0), reads=[cu.r()], writes=[enb_.r()])
            P.op("pool", lambda e, eb_=eb_, n=n, t2=t2: e.tensor_copy(out=eblast[:, t2, n * 4:(n + 1) * 4], in_=fv(eb_, 127, [(128, 4)])), reads=[eb_.r()], writes=[eblast.r()])
            psq = bank()
            for k in range(8):
                P.op("pe", lambda e, psq=psq, n=n, t2=t2, k=k: e.matmul(psq[:], lhsT=wgla[:, k, WQ + t2 * 128:WQ + (t2 + 1) * 128], rhs=hT[:, k, n * 512:(n + 1) * 512],
                                                                       start=(k == 0), stop=(k == 7)), reads=[wgla.r(), hT.r((k, n))], writes=[psq.r()])
            P.op("dve", lambda e, psq=psq, eb_=eb_, n=n, t2=t2: e.scalar_tensor_tensor(out=qtT[:, t2, n * 512:(n + 1) * 512], in0=psq[:], scalar=0.125, in1=eb_[:],
                                                                                      op0=ALU.mult, op1=ALU.mult), reads=[psq.r(), eb_.r()], writes=[qtT.r((t2, n))])
            psk = bank()
            for k in range(8):
                P.op("pe", lambda e, psk=psk, n=n, t2=t2, k=k: e.matmul(psk[:], lhsT=wgla[:, k, WK + t2 * 128:WK + (t2 + 1) * 128], rhs=hT[:, k, n * 512:(n + 1) * 512],
                                                                       start=(k == 0), stop=(k == 7)), reads=[wgla.r(), hT.r((k, n))], writes=[psk.r()])
            P.op("dve", lambda e, psk=psk, enb_=enb_, n=n, t2=t2: e.tensor_tensor(out=ktT[:, t2, n * 512:(n + 1) * 512], in0=psk[:], in1=enb_[:], op=ALU.mult),
                 reads=[psk.r(), enb_.r()], writes=[ktT.r((t2, n))])
    for c in range(NT):
        n = c // 4
        psv = bank()
        for k in range(8):
            P.op("pe", lambda e, psv=psv, c=c, k=k: e.matmul(psv[:], lhsT=hT[:, k, c * 128:(c + 1) * 128], rhs=wgla[:, k, WV:WV + 512], start=(k == 0), stop=(k == 7)),
                 reads=[wgla.r(), hT.r((k, n))], writes=[psv.r()])
        evac_copy(vtok[:, c, :], psv[:], [psv.r()], [vtok.r(c)])
        psr = bank()
        for k in range(8):
            P.op("pe", lambda e, psr=psr, c=c, k=k: e.matmul(psr[:], lhsT=hT[:, k, c * 128:(c + 1) * 128], rhs=wgla[:, k, WR:WR + 512], start=(k == 0), stop=(k == 7)),
                 reads=[wgla.r(), hT.r((k, n))], writes=[psr.r()])
        P.op("act", lambda e, psr=psr, c=c: e.activation(out=rstok[:, c, :], in_=psr[:], func=AF.Silu), reads=[psr.r()], writes=[rstok.r(c)])
        pst = bank()
        pstb = pst.t[:].bitcast(BF16)
        for t2 in range(2):
            P.op("pe", lambda e, pstb=pstb, c=c, t2=t2: e.transpose(pstb[:, t2 * 128:(t2 + 1) * 128], ktT[:, t2, c * 128:(c + 1) * 128], identb_t[:]),
                 reads=[ktT.r((t2, n)), identb_t.r()], writes=[pst.r()])
        evac_copy(ktok[:, c, :], pstb[:, 0:256], [pst.r()], [ktok.r(c)])
    A.release(m_G)
    wpb = A.alloc("wpb", [128, 4, D], BF16)
    P.dma("pool", lambda e: e.dma_start(out=wpb[:], in_=wpb_d.rearrange("(k p) n -> p k n", p=128)), writes=[wpb.r()])
    wgb = A.alloc("wgb", [128, 8, D], BF16)
    P.dma("pool", lambda e: e.dma_start(out=wgb[:], in_=win_v[:, :, OFF_GB:OFF_GB + D]), writes=[wgb.r()])
    ybT = A.alloc("ybT", [128, 4, L], BF16)
    S32 = A.alloc("S32", [128, 2, 128], F32)
    Sbf = A.alloc("Sbf", [128, 2, 128], BF16)
    PT = [A.alloc(f"PT{i}", [128, 4, 128], BF16) for i in range(2)]
    otok = [A.alloc(f"otok{i}", [128, 512], F32) for i in range(2)]
    onrm = [A.alloc(f"onrm{i}", [128, 512], F32) for i in range(2)]
    ybtok = [A.alloc(f"ybtok{i}", [128, 512], BF16) for i in range(2)]
    oss = [A.alloc(f"oss{i}", [128, 8], F32) for i in range(2)]
    osq = A.alloc("osq", [128, 128], F32)
    P.op("pool", lambda e: e.memset(S32[:], 0.0), writes=[S32.r()])
    maskb = fv(cst, 544, [(0, 4), (1, 128)])
    for c in range(NT):
        n = c // 4
        cs = slice(c * 128, (c + 1) * 128)
        pss = bank()
        for hd in range(4):
            t2, po = hd // 2, (hd % 2) * 64
            P.op("pe", lambda e, pss=pss, hd=hd, t2=t2, po=po, cs=cs: e.matmul(pss[:, hd * 128:(hd + 1) * 128], lhsT=ktT[po:po + 64, t2, cs], rhs=qtT[po:po + 64, t2, cs],
                                                                              start=True, stop=True), reads=[ktT.r((t2, n)), qtT.r((t2, n))], writes=[pss.r()], pmode="k64")
        pt = PT[c % 2]
        P.op("dve", lambda e, pss=pss, pt=pt: e.tensor_tensor(out=pt[:], in0=pss[:].rearrange("p (a b) -> p a b", a=4), in1=maskb, op=ALU.mult),
             reads=[pss.r(), cst.r()], writes=[pt.r()])
        pso = bank()
        for hd in range(4):
            t2, po = hd // 2, (hd % 2) * 64
            P.op("pe", lambda e, pso=pso, pt=pt, hd=hd, c=c: e.matmul(pso[:, hd * 128:(hd + 1) * 128], lhsT=pt[:, hd, :], rhs=vtok[:, c, hd * 128:(hd + 1) * 128],
                                                                     start=True, stop=(c == 0)), reads=[pt.r(), vtok.r(c)], writes=[pso.r()])
            if c > 0:
                P.op("pe", lambda e, pso=pso, hd=hd, t2=t2, po=po, cs=cs: e.matmul(pso[:, hd * 128:(hd + 1) * 128], lhsT=qtT[po:po + 64, t2, cs], rhs=Sbf[po:po + 64, t2, :],
                                                                                  start=False, stop=True), reads=[qtT.r((t2, n)), Sbf.r()], writes=[pso.r()], pmode="k64")
        ot, on_, yb_, os_ = otok[c % 2], onrm[c % 2], ybtok[c % 2], oss[c % 2]
        P.op("act", lambda e, pso=pso, ot=ot: e.copy(out=ot[:], in_=pso[:]), reads=[pso.r()], writes=[ot.r()])
        for hd in range(4):
            P.op("act", lambda e, ot=ot, os_=os_, hd=hd: e.activation(out=osq[:], in_=ot[:, hd * 128:(hd + 1) * 128], func=AF.Square, accum_out=os_[:, hd:hd + 1]),
                 reads=[ot.r()], writes=[osq.r(), os_.r()])
        P.op("dve", lambda e, os_=os_: e.tensor_scalar(out=os_[:, 4:8], in0=os_[:, 0:4], scalar1=1.0 / 128.0, scalar2=EPS, op0=ALU.mult, op1=ALU.add), reads=[os_.r()], writes=[os_.r()])
        P.op("act", lambda e, os_=os_: e.activation(out=os_[:, 4:8], in_=os_[:, 4:8], func=AF.Sqrt), reads=[os_.r()], writes=[os_.r()])
        P.op("dve", lambda e, os_=os_: e.reciprocal(out=os_[:, 4:8], in_=os_[:, 4:8]), reads=[os_.r()], writes=[os_.r()])
        for hd in range(4):
            P.op("dve", lambda e, ot=ot, on_=on_, os_=os_, hd=hd: e.scalar_tensor_tensor(out=on_[:, hd * 128:(hd + 1) * 128], in0=ot[:, hd * 128:(hd + 1) * 128], scalar=os_[:, 4 + hd:5 + hd],
                                                                                        in1=gsm[:, 128:256], op0=ALU.mult, op1=ALU.mult), reads=[ot.r(), os_.r(), gsm.r()], writes=[on_.r()])
        P.op("pool", lambda e, on_=on_, yb_=yb_, c=c: e.tensor_tensor(out=yb_[:], in0=on_[:], in1=rstok[:, c, :], op=ALU.mult), reads=[on_.r(), rstok.r(c)], writes=[yb_.r()])
        pst = bank()
        pstb = pst.t[:].bitcast(BF16)
        for m in range(4):
            P.op("pe", lambda e, pstb=pstb, yb_=yb_, m=m: e.transpose(pstb[:, m * 128:(m + 1) * 128], yb_[:, m * 128:(m + 1) * 128], identb_t[:]),
                 reads=[yb_.r(), identb_t.r()], writes=[pst.r()])
        evac_copy(ybT[:, :, cs], pstb[:, 0:512].rearrange("p (a b) -> p a b", a=4), [pst.r()], [ybT.r((m, n)) for m in range(4)])
        if c < NT - 1:
            for t2 in range(2):
                psu = bank()
                P.op("pe", lambda e, psu=psu, c=c, t2=t2: e.matmul(psu[:, 0:256], lhsT=ktok[:, c, t2 * 128:(t2 + 1) * 128], rhs=vtok[:, c, t2 * 256:(t2 + 1) * 256], start=True, stop=True),
                     reads=[ktok.r(c), vtok.r(c)], writes=[psu.r()])
                for hl in range(2):
                    rows = slice(hl * 64, (hl + 1) * 64)
                    P.op("dve", lambda e, psu=psu, t2=t2, hl=hl, rows=rows: e.tensor_tensor(out=S32[rows, t2, :], in0=psu[rows, hl * 128:(hl + 1) * 128], in1=S32[rows, t2, :], op=ALU.add),
                         reads=[psu.r(), S32.r()], writes=[S32.r()])
                    P.op("act", lambda e, t2=t2, rows=rows, c=c: e.activation(out=S32[rows, t2, :], in_=S32[rows, t2, :], func=AF.Copy, scale=eblast[rows, t2, c:c + 1]),
                         reads=[S32.r(), eblast.r()], writes=[S32.r()])
                    P.op("dve", lambda e, t2=t2, rows=rows: e.tensor_copy(out=Sbf[rows, t2, :], in_=S32[rows, t2, :]), reads=[S32.r()], writes=[Sbf.r()])

    if stage == 3:
        o1 = dbg_out("d_ybT", [128, 4 * L], BF16)
        P.dma("sp", lambda e: e.dma_start(out=o1, in_=ybT[:].rearrange("p a b -> p (a b)")), reads=[ybT.r((k, n)) for k in range(4) for n in range(4)])
        o2 = dbg_out("d_yaT", [128, 4 * L], BF16)
        P.dma("sp", lambda e: e.dma_start(out=o2, in_=yaT[:].rearrange("p a b -> p (a b)")), reads=[yaT.r((k, n)) for k in range(4) for n in range(4)])
        P.wait_all_dma("sp")
        P.build()
        return nc

    P.pe_selfsync = False
    proj_gate(wpb, wgb, ybT, False)

    if stage == 4:
        o1 = dbg_out("d_mixedT", [128, 8 * L], BF16)
        P.dma("sp", lambda e: e.dma_start(out=o1, in_=mixedT[:].rearrange("p a b -> p (a b)")), reads=[mixedT.r((k, n)) for k in range(8) for n in range(4)])
        P.wait_all_dma("sp")
        P.build()
        return nc


    A.release(m_C2)
    acc = A.alloc("acc", [128, NT, D], F32)
    m_E = A.mark()
    wout = A.alloc("wout", [128, 8, D], BF16)
    P.dma("pool", lambda e: e.dma_start(out=wout[:], in_=wout_d.rearrange("(k p) n -> p k n", p=128)), writes=[wout.r()])
    for k in range(8):
        eng = "dve" if k % 2 == 0 else "pool"
        P.op(eng, lambda e, k=k: e.tensor_tensor(out=wout[:, k, :], in0=wout[:, k, :], in1=g12[:, 0, :], op=ALU.mult), reads=[wout.r(), g12.r(0)], writes=[wout.r()])
    xin2 = [A.alloc(f"xin2_{i}", [128, 4, D], F32) for i in range(2)]
    for blk in range(4):
        xi = xin2[blk % 2]
        P.dma("sp", lambda e, xi=xi, blk=blk: e.dma_start(out=xi[:], in_=x_v[:, blk * 4:(blk + 1) * 4, :]), writes=[xi.r()])
        for j in range(4):
            c = blk * 4 + j
            for half in range(2):
                ps = bank()
                for k in range(8):
                    P.op("pe", lambda e, ps=ps, c=c, k=k, half=half: e.matmul(ps[:], lhsT=mixedT[:, k, c * 128:(c + 1) * 128], rhs=wout[:, k, half * 512:(half + 1) * 512],
                                                                             start=(k == 0), stop=(k == 7)), reads=[mixedT.r((k, blk)), wout.r()], writes=[ps.r()])
                P.op("dve", lambda e, ps=ps, xi=xi, j=j, c=c, half=half: e.tensor_tensor(out=acc[:, c, half * 512:(half + 1) * 512], in0=ps[:], in1=xi[:, j, half * 512:(half + 1) * 512], op=ALU.add),
                     reads=[ps.r(), xi.r()], writes=[acc.r((c, half))])
    A.release(m_E)

    if stage == 5:
        o1 = dbg_out("d_x1", [L, D], F32)
        P.dma("sp", lambda e: e.dma_start(out=o1.rearrange("(n p) d -> p n d", p=128), in_=acc[:]), reads=[acc.r((c, h_)) for c in range(NT) for h_ in range(2)])
        P.wait_all_dma("sp")
        P.build()
        return nc

    NB = 384
    NSLOT = NB * 128
    xn2_d = nc.dram_tensor("xn2_scr", [L, D], BF16).ap()
    slottab_d = nc.dram_tensor("slottab_scr", [NSLOT, 2], I32).ap()
    yslots_d = nc.dram_tensor("yslots_scr", [NSLOT, D], F32).ap()
    bexp_d = nc.dram_tensor("bexp_scr", [NB], I32).ap()
    r_xn2, r_slottab, r_yslots, r_bexp = Reg("xn2"), Reg("slottab"), Reg("yslots"), Reg("bexp")
    h2T = A.alloc("h2T", [128, 8, L], BF16, at=hT_off)
    Msk = A.alloc("Msk", [128, NT, NEXP], BF16, at=mixedT_off + 17664)
    Wr = A.alloc("Wr", [128, NT, NEXP + 1], F32, at=mixedT_off)
    rbias = A.alloc("rbias", [128, NEXP], F32, at=mixedT_off + 16512)
    m_F = A.mark()
    wrt = A.alloc("wrt", [128, 8, NEXP], F32)
    P.dma("sp", lambda e: e.dma_start(out=wrt[:], in_=rw_d.rearrange("(k p) n -> p k n", p=128)), writes=[wrt.r()])
    P.dma("sp", lambda e: e.dma_start(out=rbias[:], in_=bass.AP(rb_d.tensor, rb_d.offset, [[0, 128], [1, NEXP]])), writes=[rbias.r()])
    P.op("pool", lambda e: e.memset(Wr[:, :, NEXP:NEXP + 1], 1.0), writes=[Wr.r("sh")])
    xnf = A.alloc("xnf", [128, 4, D], F32)
    h2f = A.alloc("h2f", [128, 8, 512], F32)
    rt = A.alloc("rt", [128, 2048], F32)
    SC, BI, MB, SEL = 0, 256, 512, 768
    M8 = A.alloc("M8", [128, 128], F32)
    for blk in range(4):
        for j in range(4):
            c = blk * 4 + j
            P.op("act", lambda e, c=c, blk=blk: e.activation(out=sq[:], in_=acc[:, c, :], func=AF.Square, accum_out=ssq[:, c:c + 1]),
                 reads=[acc.r((c, 0)), acc.r((c, 1))], writes=[sq.r(), ssq.r(blk)])
        rs = rstd[:, blk * 4:(blk + 1) * 4]
        P.op("dve", lambda e, rs=rs, blk=blk: e.tensor_scalar(out=rs, in0=ssq[:, blk * 4:(blk + 1) * 4], scalar1=1.0 / D, scalar2=EPS, op0=ALU.mult, op1=ALU.add),
             reads=[ssq.r(blk)], writes=[rstd.r(blk)])
        P.op("act", lambda e, rs=rs: e.activation(out=rs, in_=rs, func=AF.Sqrt), reads=[rstd.r(blk)], writes=[rstd.r(blk)])
        P.op("dve", lambda e, rs=rs: e.reciprocal(out=rs, in_=rs), reads=[rstd.r(blk)], writes=[rstd.r(blk)])
        for j in range(4):
            c = blk * 4 + j
            P.op("act", lambda e, j=j, c=c: e.activation(out=xnf[:, j, :], in_=acc[:, c, :], func=AF.Copy, scale=rstd[:, c:c + 1]),
                 reads=[acc.r((c, 0)), acc.r((c, 1)), rstd.r(blk)], writes=[xnf.r(j)])
            P.dma("pool", lambda e, j=j, c=c: e.dma_start(out=xn2_d[c * 128:(c + 1) * 128, :], in_=xnf[:, j, :]), reads=[xnf.r(j)], wacc=[r_xn2])
        for k in range(8):
            ps = bank()
            for j in range(4):
                P.op("pe", lambda e, ps=ps, j=j, k=k: e.transpose(ps[:, j * 128:(j + 1) * 128], xnf[:, j, k * 128:(k + 1) * 128], cst[:, 0:128]),
                     reads=[xnf.r(j), cst.r()], writes=[ps.r()], pmode="f32")
            P.op("dve", lambda e, ps=ps, k=k, blk=blk: e.tensor_scalar(out=h2T[:, k, blk * 512:(blk + 1) * 512], in0=ps[:], scalar1=AB[:, 1, k:k + 1], scalar2=modcol[:, 2, k:k + 1],
                                                                      op0=ALU.mult, op1=ALU.add), reads=[ps.r(), AB.r(), modcol.r()], writes=[h2T.r((k, blk))])
            P.op("dve", lambda e, ps=ps, k=k: e.tensor_scalar(out=h2f[:, k, :], in0=ps[:], scalar1=AB[:, 1, k:k + 1], scalar2=modcol[:, 2, k:k + 1], op0=ALU.mult, op1=ALU.add),
                 reads=[ps.r(), AB.r(), modcol.r()], writes=[h2f.r(k)])
        for j in range(4):
            c = blk * 4 + j
            ps = bank()
            for k in range(8):
                P.op("pe", lambda e, ps=ps, j=j, k=k: e.matmul(ps[:, 0:NEXP], lhsT=h2f[:, k, j * 128:(j + 1) * 128], rhs=wrt[:, k, :], start=(k == 0), stop=(k == 7)),
                     reads=[h2f.r(k), wrt.r()], writes=[ps.r()], pmode="f32")
            rr = [rt.r()]
            mr = [M8.r()]
            P.op("act", lambda e, ps=ps: e.activation(out=rt[:, SC:SC + 256], in_=ps[:, 0:NEXP], func=AF.Sigmoid), reads=[ps.r()], writes=rr)
            P.op("dve", lambda e: e.tensor_tensor(out=rt[:, BI:BI + 256], in0=rt[:, SC:SC + 256], in1=rbias[:], op=ALU.add), reads=rr + [rbias.r()], writes=rr)
            for gq in range(8):
                P.op("dve", lambda e, gq=gq: e.max(out=M8[:, gq * 8:(gq + 1) * 8], in_=rt[:, BI + gq * 32:BI + (gq + 1) * 32]), reads=rr, writes=mr)
            P.op("dve", lambda e: e.tensor_tensor(out=M8[:, 64:72], in0=fv(M8, 0, [(8, 8)]), in1=fv(M8, 1, [(8, 8)]), op=ALU.add), reads=mr, writes=mr)
            P.op("dve", lambda e: e.max(out=M8[:, 72:80], in_=M8[:, 64:72]), reads=mr, writes=mr)
            P.op("dve", lambda e: e.tensor_scalar(out=M8[:, 80:88], in0=M8[:, 64:72], scalar1=M8[:, 75:76], scalar2=None, op0=ALU.is_ge), reads=mr, writes=mr)
            P.op("dve", lambda e: e.scalar_tensor_tensor(out=fv(rt, MB, [(32, 8), (1, 32)]), in0=fv(rt, BI, [(32, 8), (1, 32)]), scalar=2.0, in1=fv(M8, 80, [(1, 8), (0, 32)]),
                                                         op0=ALU.add, op1=ALU.mult), reads=rr + mr, writes=rr)
            P.op("dve", lambda e: e.max(out=M8[:, 88:96], in_=rt[:, MB:MB + 256]), reads=rr, writes=mr)
            P.op("dve", lambda e: e.tensor_scalar(out=rt[:, SEL:SEL + 256], in0=rt[:, MB:MB + 256], scalar1=M8[:, 95:96], scalar2=None, op0=ALU.is_ge), reads=rr + mr, writes=rr)
            P.op("pool", lambda e, c=c: e.tensor_copy(out=Msk[:, c, :], in_=rt[:, SEL:SEL + 256]), reads=rr, writes=[Msk.r(c)])
            P.op("dve", lambda e: e.tensor_tensor(out=rt[:, SEL:SEL + 256], in0=rt[:, SEL:SEL + 256], in1=rt[:, SC:SC + 256], op=ALU.mult), reads=rr, writes=rr)
            P.op("dve", lambda e: e.tensor_reduce(out=M8[:, 96:97], in_=rt[:, SEL:SEL + 256], axis=AX.X, op=ALU.add), reads=rr, writes=mr)
            P.op("dve", lambda e: e.reciprocal(out=M8[:, 97:98], in_=M8[:, 96:97]), reads=mr, writes=mr)
            P.op("dve", lambda e, c=c: e.tensor_scalar(out=Wr[:, c, 0:NEXP], in0=rt[:, SEL:SEL + 256], scalar1=M8[:, 97:98], scalar2=2.5, op0=ALU.mult, op1=ALU.mult),
                 reads=rr + mr, writes=[Wr.r(c)])
    A.release(m_F)

    if stage == 6:
        o1 = dbg_out("d_W", [L, NEXP + 1], F32)
        P.dma("sp", lambda e: e.dma_start(out=o1.rearrange("(n p) d -> p n d", p=128), in_=Wr[:]), reads=[Wr.r(c) for c in range(NT)] + [Wr.r("sh")])
        o2 = dbg_out("d_h2T", [128, 8 * L], BF16)
        P.dma("sp", lambda e: e.dma_start(out=o2, in_=h2T[:].rearrange("p a b -> p (a b)")), reads=[h2T.r((k, n)) for k in range(8) for n in range(4)])
        P.wait_all_dma("sp")
        P.build()
        return nc

    IOA = bass.IndirectOffsetOnAxis
    regcache = {}

    def breg(e, val):
        if ("b", val) not in regcache:
            regcache[("b", val)] = e.to_reg(val)
        return regcache[("b", val)]
    m_G0 = A.mark()
    wsh = A.alloc("wsh", [128, 2, 2048], BF16)
    wdsh = A.alloc("wdsh", [128, 2048], BF16)
    P.dma("pool", lambda e: e.dma_start(out=wsh[:, 0, :], in_=ewg_d[(n_exp - 1) * 128:n_exp * 128, :]), writes=[wsh.r("g")])
    P.dma("pool", lambda e: e.dma_start(out=wsh[:, 1, :], in_=ewu_d[(n_exp - 1) * 128:n_exp * 128, :]), writes=[wsh.r("u")])
    P.dma("pool", lambda e: e.dma_start(out=wdsh[:], in_=ewd_d[(n_exp - 1) * 128:n_exp * 128, :]), writes=[wdsh.r()])
    aT = [A.alloc(f"aT{i}", [128, 2, 512], BF16) for i in range(2)]
    sgm = [A.alloc(f"sgm{i}", [128, 512], BF16) for i in range(2)]
    for kk in range(2):
        P.op("pool", lambda e, kk=kk: e.tensor_tensor(out=wdsh[:, kk * 1024:(kk + 1) * 1024], in0=wdsh[:, kk * 1024:(kk + 1) * 1024], in1=g12[:, 1, :], op=ALU.mult),
             reads=[wdsh.r(), g12.r(1)], writes=[wdsh.r()])
    for n in range(4):
        at_ = aT[n % 2]
        for f in range(2):
            psg = bank()
            for k in range(8):
                P.op("pe", lambda e, psg=psg, f=f, k=k, n=n: e.matmul(psg[:], lhsT=wsh[:, 0, k * 256 + f * 128:k * 256 + (f + 1) * 128], rhs=h2T[:, k, n * 512:(n + 1) * 512], start=(k == 0), stop=(k == 7)),
                     reads=[wsh.r("g"), h2T.r((k, n))], writes=[psg.r()])
            psu = bank()
            for k in range(8):
                P.op("pe", lambda e, psu=psu, f=f, k=k, n=n: e.matmul(psu[:], lhsT=wsh[:, 1, k * 256 + f * 128:k * 256 + (f + 1) * 128], rhs=h2T[:, k, n * 512:(n + 1) * 512], start=(k == 0), stop=(k == 7)),
                     reads=[wsh.r("u"), h2T.r((k, n))], writes=[psu.r()])
            sg = sgm[f]
            P.op("act", lambda e, psg=psg, sg=sg: e.activation(out=sg[:], in_=psg[:], func=AF.Silu), reads=[psg.r()], writes=[sg.r()])
            P.op("dve", lambda e, psu=psu, sg=sg, at_=at_, f=f: e.tensor_tensor(out=at_[:, f, :], in0=psu[:], in1=sg[:], op=ALU.mult), reads=[psu.r(), sg.r()], writes=[at_.r(f)])
        for j in range(4):
            c = n * 4 + j
            for half in range(2):
                psy = bank()
                for f in range(2):
                    P.op("pe", lambda e, psy=psy, at_=at_, f=f, j=j, half=half: e.matmul(psy[:], lhsT=at_[:, f, j * 128:(j + 1) * 128], rhs=wdsh[:, f * 1024 + half * 512:f * 1024 + (half + 1) * 512],
                                                                                   start=(f == 0), stop=(f == 1)), reads=[at_.r(f), wdsh.r()], writes=[psy.r()])
                dst = acc[:, c, half * 512:(half + 1) * 512]
                P.op("dve", lambda e, psy=psy, dst=dst: e.tensor_tensor(out=dst, in0=psy[:], in1=dst, op=ALU.add), reads=[psy.r(), acc.r((c, half))], writes=[acc.r((c, half))])
    A.release(m_G0)

    onesb = A.alloc("onesb", [128, 128], BF16)
    sutb = A.alloc("sutb", [128, 128], BF16)
    onesf = A.alloc("onesf", [128, 256], F32)
    P.op("pool", lambda e: e.memset(onesb[:], 1.0), writes=[onesb.r()])
    P.op("pool", lambda e: e.memset(onesf[:], 1.0), writes=[onesf.r()])
    P.op("dve", lambda e: e.tensor_copy(out=sutb[:], in_=cst[:, 704:832]), reads=[cst.r()], writes=[sutb.r()])
    zt = A.alloc("zt", [128, 768], I32)
    P.op("pool", lambda e: e.memset(zt[:], 0), writes=[zt.r()])
    P.dma("sp", lambda e: e.dma_start(out=slottab_d.rearrange("(p n) two -> p (n two)", p=128), in_=zt[:]), reads=[zt.r()], writes=[r_slottab])
    cnt = A.alloc("cntt", [128, 1024], F32)
    cr_ = [cnt.r()]
    ps = bank()
    for c in range(NT):
        P.op("pe", lambda e, ps=ps, c=c: e.matmul(ps[:, 0:256], lhsT=onesb[:], rhs=Msk[:, c, :], start=(c == 0), stop=(c == NT - 1)), reads=[onesb.r(), Msk.r(c)], writes=[ps.r()])
    P.op("dve", lambda e, ps=ps: e.tensor_copy(out=cnt[:, 0:256], in_=ps[:, 0:256]), reads=[ps.r()], writes=cr_)
    widx = A.alloc("widx", [128, NB], I32)
    m_cmp = A.mark()
    cmp = A.alloc("cmpt", [128, 4096], F32)
    P.op("dve", lambda e: e.tensor_tensor(out=fv(cmp, 0, [(16, 256), (1, 16)]), in0=fv(cnt, 0, [(1, 256), (0, 16)]), in1=fv(cst, 680, [(0, 256), (1, 16)]), op=ALU.is_gt),
         reads=cr_ + [cst.r()], writes=[cmp.r()])
    P.op("dve", lambda e: e.tensor_reduce(out=cnt[:, 256:512], in_=fv(cmp, 0, [(16, 256), (1, 16)]), axis=AX.X, op=ALU.add), reads=[cmp.r()], writes=cr_)
    P.op("dve", lambda e: e.tensor_tensor_scan(out=cnt[:, 512:768], data0=onesf[:], data1=cnt[:, 256:512], initial=0.0, op0=ALU.mult, op1=ALU.add), reads=cr_ + [onesf.r()], writes=cr_)
    P.op("dve", lambda e: e.tensor_tensor(out=cnt[:, 768:1024], in0=cnt[:, 512:768], in1=cnt[:, 256:512], op=ALU.subtract), reads=cr_, writes=cr_)
    P.op("dve", lambda e: e.tensor_scalar(out=cnt[:, 768:1024], in0=cnt[:, 768:1024], scalar1=128.0, scalar2=None, op0=ALU.mult), reads=cr_, writes=cr_)
    bx = A.alloc("bx", [128, 8], F32)
    bxi = A.alloc("bxi", [128, 4], I32)
    for i in range(3):
        P.op("dve", lambda e, i=i: e.tensor_scalar(out=bx[:, 4 + i:5 + i], in0=cst[:, 840:841], scalar1=float(128 * i), scalar2=None, op0=ALU.add), reads=[cst.r()], writes=[bx.r()])
        P.op("dve", lambda e, i=i: e.tensor_scalar(out=cmp[:, 0:256], in0=cnt[:, 512:768], scalar1=bx[:, 4 + i:5 + i], scalar2=None, op0=ALU.is_le), reads=cr_ + [bx.r()], writes=[cmp.r()])
        P.op("dve", lambda e, i=i: e.tensor_reduce(out=bx[:, i:i + 1], in_=cmp[:, 0:256], axis=AX.X, op=ALU.add), reads=[cmp.r()], writes=[bx.r()])
    onesf128 = A.alloc("onesf128", [128, 128], F32)
    P.op("pool", lambda e: e.memset(onesf128[:], 1.0), writes=[onesf128.r()])
    dg = A.alloc("dg", [128, 3, 128], F32)
    widf = A.alloc("widf", [128, NB], F32)
    wtl = A.alloc("wtl", [128, NB], F32)
    psb_ = bank()
    for i in range(3):
        P.op("dve", lambda e, i=i: e.tensor_scalar(out=dg[:, i, :], in0=cst[:, 0:128], scalar1=bx[:, i:i + 1], scalar2=None, op0=ALU.mult), reads=[cst.r(), bx.r()], writes=[dg.r(i)])
        P.op("pe", lambda e, i=i, psb_=psb_: e.matmul(psb_[:, i * 128:(i + 1) * 128], lhsT=onesf128[:], rhs=dg[:, i, :], start=True, stop=True), reads=[onesf128.r(), dg.r(i)], writes=[psb_.r()], pmode="f32")
    P.op("dve", lambda e, psb_=psb_: e.tensor_scalar(out=widf[:], in0=psb_[:, 0:NB], scalar1=128.0, scalar2=cst[:, 840:841], op0=ALU.mult, op1=ALU.add), reads=[psb_.r(), cst.r()], writes=[widf.r()])
    P.op("dve", lambda e, psb_=psb_: e.tensor_scalar(out=wtl[:], in0=psb_[:, 0:NB], scalar1=float(NEXP), scalar2=1.0e6, op0=ALU.is_ge, op1=ALU.mult), reads=[psb_.r()], writes=[wtl.r()])
    P.op("dve", lambda e: e.tensor_tensor(out=widf[:], in0=widf[:], in1=wtl[:], op=ALU.add), reads=[widf.r(), wtl.r()], writes=[widf.r()])
    P.op("dve", lambda e: e.tensor_copy(out=widx[:], in_=widf[:]), reads=[widf.r()], writes=[widx.r()])
    A.release(m_cmp)
    tokid = A.alloc("tokid", [128, NT], I32)
    P.op("pool", lambda e: e.iota(tokid[:], [[128, NT]], base=0, channel_multiplier=1), writes=[tokid.r()])
    d8 = A.alloc("d8", [128, NT, 8], F32)
    d8i = A.alloc("d8i", [128, NT, 8], I32)
    m_bk = A.mark()
    keyt = [A.alloc(f"key{i}", [128, 256], F32) for i in range(2)]
    eqt = A.alloc("eqt", [128, 256], F32)
    junk = A.alloc("junk", [128, 256], F32)
    w8t = [A.alloc(f"w8_{i}", [128, 8], F32) for i in range(2)]
    ixt = [[A.alloc(f"ix{i}_{j}", [128, 1], I32) for j in range(8)] for i in range(2)]
    rct = [[A.alloc(f"rc{i}_{j}", [128, 2], I32) for j in range(8)] for i in range(2)]
    for c in range(NT):
        ps = bank()
        for c2 in range(c):
            P.op("pe", lambda e, ps=ps, c2=c2: e.matmul(ps[:, 0:256], lhsT=onesb[:], rhs=Msk[:, c2, :], start=(c2 == 0), stop=False), reads=[onesb.r(), Msk.r(c2)], writes=[ps.r()])
        P.op("pe", lambda e, ps=ps, c=c: e.matmul(ps[:, 0:256], lhsT=sutb[:], rhs=Msk[:, c, :], start=(c == 0), stop=True), reads=[sutb.r(), Msk.r(c)], writes=[ps.r()])
        key, w8 = keyt[c % 2], w8t[c % 2]
        P.op("dve", lambda e, ps=ps, key=key: e.scalar_tensor_tensor(out=key[:], in0=ps[:, 0:256], scalar=1.0, in1=cnt[:, 768:1024], op0=ALU.add, op1=ALU.add), reads=[ps.r()] + cr_, writes=[key.r()])
        P.op("dve", lambda e, key=key, c=c: e.tensor_tensor(out=key[:], in0=key[:], in1=Msk[:, c, :], op=ALU.mult), reads=[key.r(), Msk.r(c)], writes=[key.r()])
        P.op("dve", lambda e, key=key, c=c: e.max(out=d8[:, c, :], in_=key[:]), reads=[key.r()], writes=[d8.r(c)])
        for j in range(8):
            P.op("dve", lambda e, key=key, w8=w8, c=c, j=j: e.scalar_tensor_tensor(out=junk[:], in0=key[:], scalar=d8[:, c, j:j + 1], in1=Wr[:, c, 0:NEXP], op0=ALU.is_equal, op1=ALU.mult,
                                                                                   accum_out=w8[:, j:j + 1]), reads=[key.r(), d8.r(c), Wr.r(c)], writes=[junk.r(), w8.r()])
        P.op("dve", lambda e, c=c: e.tensor_scalar(out=d8[:, c, :], in0=d8[:, c, :], scalar1=-1.0, scalar2=None, op0=ALU.add), reads=[d8.r(c)], writes=[d8.r(c)])
        P.op("dve", lambda e, c=c: e.tensor_copy(out=d8i[:, c, :], in_=d8[:, c, :]), reads=[d8.r(c)], writes=[d8i.r(c)])
        for j in range(8):
            ix, rc = ixt[c % 2][j], rct[c % 2][j]
            P.op("dve", lambda e, ix=ix, c=c, j=j: e.tensor_copy(out=ix[:], in_=d8i[:, c, j:j + 1]), reads=[d8i.r(c)], writes=[ix.r()])
            P.op("pool", lambda e, rc=rc, c=c: e.tensor_copy(out=rc[:, 0:1], in_=tokid[:, c:c + 1]), reads=[tokid.r()], writes=[rc.r()])
            P.op("pool", lambda e, rc=rc, w8=w8, j=j: e.tensor_copy(out=rc[:, 1:2], in_=w8[:, j:j + 1].bitcast(I32)), reads=[w8.r()], writes=[rc.r()])
            P.dma("pool", lambda e, ix=ix, rc=rc: e.indirect_dma_start(out=slottab_d, out_offset=IOA(ap=ix[:, :], axis=0), in_=rc[:], in_offset=None,
                                                                     bounds_check=breg(e, NSLOT - 1), oob_is_err=False), reads=[rc.r(), ix.r()], wacc=[r_slottab])
    A.release(m_bk)

    if stage == 7:
        o1 = dbg_out("d_slottab", [NSLOT, 2], I32)
        P.dma("sp", lambda e: e.dma_start(out=o1, in_=slottab_d), reads=[r_slottab])
        o2 = dbg_out("d_d8i", [128, NT * 8], I32)
        P.dma("sp", lambda e: e.dma_start(out=o2, in_=d8i[:].rearrange("p a b -> p (a b)")), reads=[d8i.r(c) for c in range(NT)])
        o3 = dbg_out("d_widx", [128, NB], I32)
        P.dma("sp", lambda e: e.dma_start(out=o3, in_=widx[:]), reads=[widx.r()])
        o5 = dbg_out("d_W", [L, NEXP + 1], F32)
        P.dma("sp", lambda e: e.dma_start(out=o5.rearrange("(n p) d -> p n d", p=128), in_=Wr[:]), reads=[Wr.r(c) for c in range(NT)] + [Wr.r("sh")])
        o4 = dbg_out("d_cnt", [128, 1024], F32)
        P.dma("sp", lambda e: e.dma_start(out=o4, in_=cnt[:]), reads=cr_)
        P.wait_all_dma("sp")
        P.build()
        return nc

    m_blk = A.mark()
    NBUF = 4
    NFE = 6
    wgu = [A.alloc(f"wgu{i}", [128, 2, 2048], BF16, at=hT_off + i * 8192) for i in range(NBUF)]
    ysb = [A.alloc(f"ysb{i}", [128, D], F32) for i in range(2)]
    wixt = [A.alloc(f"wix{i}", [128, 1], I32) for i in range(NBUF)]
    stt = [A.alloc(f"st{i}", [128, 2], I32) for i in range(NFE)]
    stk = [A.alloc(f"stk{i}", [128, 1], I32) for i in range(NFE)]
    Xg = [A.alloc(f"Xg{i}", [128, D], BF16) for i in range(NFE)]
    XgT = [A.alloc(f"XgT{i}", [128, 8, 128], BF16) for i in range(2)]
    sgs = [A.alloc(f"sgs{i}", [128, 256], BF16) for i in range(2)]
    aTk = [A.alloc(f"aTk{i}", [128, 256], BF16) for i in range(2)]
    aTs = [A.alloc(f"aTs{i}", [128, 256], BF16) for i in range(2)]
    a2 = AB[:, 1, 0:1]
    A2b = bass.AP(a2.tensor, a2.offset, [list(a2.ap[0]), [1, 8], [0, 128]])
    b2 = modcol[:, 2, 0:1]
    B2b = bass.AP(b2.tensor, b2.offset, [list(b2.ap[0]), [1, 8], [0, 128]])
    nblk_run = NB if stage >= 99 else 4

    def front_end(b):
        s_, sk, xg = stt[b % NFE], stk[b % NFE], Xg[b % NFE]
        P.dma("sp", lambda e, s_=s_, b=b: e.dma_start(out=s_[:], in_=slottab_d[b * 128:(b + 1) * 128, :]), reads=[r_slottab], writes=[s_.r()])
        P.op("dve", lambda e, sk=sk, s_=s_: e.tensor_copy(out=sk[:], in_=s_[:, 0:1]), reads=[s_.r()], writes=[sk.r()])
        P.dma("pool", lambda e, xg=xg, sk=sk: e.indirect_dma_start(out=xg[:], out_offset=None, in_=xn2_d, in_offset=IOA(ap=sk[:, :], axis=0), bounds_check=breg(e, L - 1), oob_is_err=False),
              reads=[sk.r(), r_xn2], writes=[xg.r()])

    NWD = 5
    wdn = [A.alloc(f"wdn4_{i}", [128, 2048], BF16) for i in range(NWD)]

    def load_blk(b):
        wb, wd_, wx = wgu[b % NBUF], wdn[b % NWD], wixt[b % NBUF]
        P.op("pool", lambda e, wx=wx, b=b: e.tensor_copy(out=wx[:], in_=widx[:, b:b + 1]), reads=[widx.r()], writes=[wx.r()])
        for dst, src_d, rg in ((wb[:, 0, :], ewg_d, wb.r("g")), (wb[:, 1, :], ewu_d, wb.r("u")), (wd_[:], ewd_d, wd_.r())):
            P.dma("pool", lambda e, dst=dst, src_d=src_d, wx=wx: e.indirect_dma_start(out=dst, out_offset=None, in_=src_d, in_offset=IOA(ap=wx[:, :], axis=0),
                                                                                 bounds_check=breg(e, n_exp * 128 - 1), oob_is_err=False), reads=[wx.r()], writes=[rg])

    def st_T(b):
        xg, xt = Xg[b % NFE], XgT[b % 2]
        pst = bank()
        pstb = pst.t[:].bitcast(BF16)
        for k in range(8):
            P.op("pe", lambda e, pstb=pstb, xg=xg, k=k: e.transpose(pstb[:, k * 128:(k + 1) * 128], xg[:, k * 128:(k + 1) * 128], identb_t[:]), reads=[xg.r(), identb_t.r()], writes=[pst.r()])
        P.op("dve", lambda e, pstb=pstb, xt=xt: e.tensor_tensor(out=xt[:], in0=pstb[:, 0:1024].rearrange("p (a b) -> p a b", a=8), in1=A2b, op=ALU.mult), reads=[pst.r(), AB.r()], writes=[xt.r()])
        P.op("dve", lambda e, xt=xt: e.tensor_tensor(out=xt[:], in0=xt[:], in1=B2b, op=ALU.add), reads=[xt.r(), modcol.r()], writes=[xt.r()])

    def st_MM(b):
        wb, s_, xt = wgu[b % NBUF], stt[b % NFE], XgT[b % 2]
        psgu = bank()
        for k in range(8):
            wv = wb[:, 0, k * 256:k * 256 + 1]
            rhs = bass.AP(wv.tensor, wv.offset, [list(wv.ap[0]), [2048, 2], [1, 256]])
            P.op("pe", lambda e, psgu=psgu, xt=xt, k=k, rhs=rhs: e.matmul(psgu[:].rearrange("p (a b) -> p a b", a=2), lhsT=xt[:, k, :], rhs=rhs, start=(k == 0), stop=(k == 7)),
                 reads=[wb.r("g"), wb.r("u"), xt.r()], writes=[psgu.r()])
        sg, ak = sgs[b % 2], aTk[b % 2]
        swc = s_[:, 1:2].bitcast(F32)
        P.op("act", lambda e, psgu=psgu, sg=sg: e.activation(out=sg[:], in_=psgu[:, 0:256], func=AF.Silu), reads=[psgu.r()], writes=[sg.r()])
        P.op("dve", lambda e, psgu=psgu, sg=sg, ak=ak, swc=swc: e.scalar_tensor_tensor(out=ak[:], in0=sg[:], scalar=swc, in1=psgu[:, 256:512], op0=ALU.mult, op1=ALU.mult),
             reads=[psgu.r(), sg.r(), s_.r()], writes=[ak.r()])

    def st_T2(b):
        ak, at_ = aTk[b % 2], aTs[b % 2]
        psa = bank()
        psab = psa.t[:].bitcast(BF16)
        for f in range(2):
            P.op("pe", lambda e, psab=psab, ak=ak, f=f: e.transpose(psab[:, f * 128:(f + 1) * 128], ak[:, f * 128:(f + 1) * 128], identb_t[:]), reads=[ak.r(), identb_t.r()], writes=[psa.r()])
        P.op("act", lambda e, psab=psab, at_=at_: e.copy(out=at_[:], in_=psab[:, 0:256]), reads=[psa.r()], writes=[at_.r()])

    def st_MM2(b):
        at_, wd_, ys = aTs[b % 2], wdn[b % NWD], ysb[b % 2]
        for half in range(2):
            psy = bank()
            for f in range(2):
                P.op("pe", lambda e, psy=psy, at_=at_, wd_=wd_, f=f, half=half: e.matmul(psy[:], lhsT=at_[:, f * 128:(f + 1) * 128], rhs=wd_[:, f * 1024 + half * 512:f * 1024 + (half + 1) * 512],
                                                                                       start=(f == 0), stop=(f == 1)), reads=[at_.r(), wd_.r()], writes=[psy.r()])
            if half == 0:
                P.op("act", lambda e, psy=psy, ys=ys: e.copy(out=ys[:, 0:512], in_=psy[:]), reads=[psy.r()], writes=[ys.r(0)])
            else:
                P.op("dve", lambda e, psy=psy, ys=ys: e.tensor_copy(out=ys[:, 512:1024], in_=psy[:]), reads=[psy.r()], writes=[ys.r(1)])
        P.dma("act", lambda e, ys=ys, b=b: e.dma_start(out=yslots_d[b * 128:(b + 1) * 128, :], in_=ys[:]), reads=[ys.r(0), ys.r(1)], wacc=[r_yslots])

    for b in range(min(4, nblk_run)):
        front_end(b)
    for b in range(min(3, nblk_run)):
        load_blk(b)
    st_T(0)
    for b in range(nblk_run):
        if b + 4 < nblk_run:
            front_end(b + 4)
        if b + 3 < nblk_run:
            load_blk(b + 3)
        st_MM(b)
        if b + 1 < nblk_run:
            st_T(b + 1)
        st_T2(b)
        if b >= 1:
            st_MM2(b - 1)
    st_MM2(nblk_run - 1)

    A.release(m_blk)
    gbs = [[A.alloc(f"gb{s_}_{i}", [128, D], F32, at=(hT_off if s_ == 0 else mixedT_off) + i * 4096) for i in range(8)] for s_ in range(2)]
    ixgs = [[A.alloc(f"ixg{s_}_{j}", [128, 1], I32) for j in range(8)] for s_ in range(2)]
    for c in range(NT if stage >= 99 else 0):
        gb, ixg = gbs[c % 2], ixgs[c % 2]
        for j in range(8):
            P.op("dve", lambda e, c=c, j=j, ixg=ixg: e.tensor_copy(out=ixg[j][:], in_=d8i[:, c, j:j + 1]), reads=[d8i.r(c)], writes=[ixg[j].r()])
            P.dma("pool", lambda e, j=j, gb=gb, ixg=ixg: e.indirect_dma_start(out=gb[j][:], out_offset=None, in_=yslots_d, in_offset=IOA(ap=ixg[j][:, :], axis=0), bounds_check=breg(e, NSLOT - 1), oob_is_err=False),
                  reads=[ixg[j].r(), r_yslots], writes=[gb[j].r()])
        for (a_, b_, eng) in ((0, 1, "dve"), (2, 3, "dve"), (4, 5, "dve"), (6, 7, "pool"), (0, 2, "dve"), (4, 6, "dve"), (0, 4, "dve")):
            P.op(eng, lambda e, a_=a_, b_=b_, gb=gb: e.tensor_tensor(out=gb[a_][:], in0=gb[a_][:], in1=gb[b_][:], op=ALU.add), reads=[gb[a_].r(), gb[b_].r()], writes=[gb[a_].r()])
        P.op("dve", lambda e, gb=gb: e.tensor_tensor(out=gb[0][:], in0=gb[0][:], in1=g12[:, 1, :], op=ALU.mult), reads=[gb[0].r(), g12.r(1)], writes=[gb[0].r()])
        P.op("pool", lambda e, c=c, gb=gb: e.tensor_tensor(out=acc[:, c, :], in0=acc[:, c, :], in1=gb[0][:], op=ALU.add), reads=[gb[0].r(), acc.r((c, 0)), acc.r((c, 1))], writes=[acc.r((c, 0)), acc.r((c, 1))])

    fgb = A.alloc("fgb", [128, D], F32)
    P.dma("sp", lambda e: e.dma_start(out=fgb[:], in_=bass.AP(fg_d.tensor, fg_d.offset, [[0, 128], [1, D]])), writes=[fgb.r()])
    otl = [A.alloc(f"otl{i}", [128, D], F32) for i in range(2)]
    out_v = out_d.rearrange("(n p) d -> p n d", p=128)
    for c in range(NT):
        accr = [acc.r((c, 0)), acc.r((c, 1))]
        P.op("act", lambda e, c=c: e.activation(out=sq[:], in_=acc[:, c, :], func=AF.Square, accum_out=ssq[:, c:c + 1]), reads=accr, writes=[sq.r(), ssq.r(c // 4)])
        rs = rstd[:, c:c + 1]
        P.op("dve", lambda e, rs=rs, c=c: e.tensor_scalar(out=rs, in0=ssq[:, c:c + 1], scalar1=1.0 / D, scalar2=EPS, op0=ALU.mult, op1=ALU.add), reads=[ssq.r(c // 4)], writes=[rstd.r(c // 4)])
        P.op("act", lambda e, rs=rs: e.activation(out=rs, in_=rs, func=AF.Sqrt), reads=[rstd.r(c // 4)], writes=[rstd.r(c // 4)])
        P.op("dve", lambda e, rs=rs: e.reciprocal(out=rs, in_=rs), reads=[rstd.r(c // 4)], writes=[rstd.r(c // 4)])
        ot = otl[c % 2]
        P.op("dve", lambda e, ot=ot, rs=rs, c=c: e.scalar_tensor_tensor(out=ot[:], in0=acc[:, c, :], scalar=rs, in1=fgb[:], op0=ALU.mult, op1=ALU.mult),
             reads=accr + [rstd.r(c // 4), fgb.r()], writes=[ot.r()])
        P.dma("sp", lambda e, ot=ot, c=c: e.dma_start(out=out_v[:, c, :], in_=ot[:]), reads=[ot.r()])

    P.wait_all_dma("sp")
    P.build()
    return nc


def make_consts():
    c = np.zeros((128, 1024), np.float32)
    c[:, 0:128] = np.eye(128, dtype=np.float32)
    k = np.arange(128)
    c[k, 128 + (k + 64) % 128] = 1.0
    c[:64, 256] = 1.0
    c[64:, 256] = -1.0
    c[:64, 257] = -1.0
    c[64:, 257] = 1.0
    for gl in range(8):
        c[gl * 16:(gl + 1) * 16, 264 + gl] = 1.0
    c[:, 272:283] = (2.0 ** np.arange(11))[None, :]
    oh = np.zeros((32, 8), np.float32)
    oh[np.arange(32), np.arange(32) % 8] = 1.0
    c[:, 288:544] = oh.reshape(1, 256)
    j = np.arange(128)[:, None]
    i = np.arange(128)[None, :]
    c[:, 544:672] = (j <= i).astype(np.float32)
    c[:, 680:696] = (128.0 * np.arange(16))[None, :]
    c[:, 704:832] = (j < i).astype(np.float32)
    c[:, 840] = np.arange(128)
    return c


PER_BATCH = ("x", "c")
SHARED = ("ada_w", "ada_b", "norm1_g", "w_in", "s5_lam_re", "s5_lam_im", "s5_log_dt", "s5_b_re", "s5_b_im", "s5_c_re", "s5_c_im", "s5_d", "s5_w_glu", "s5_b_glu", "w_proj_a", "gla_w_gk2", "gla_b_gk2", "gla_norm_g", "w_proj_b", "w_out", "norm2_g", "router_w", "router_bias")
RESHAPE = {"s5_c_re": (512, 64), "s5_c_im": (512, 64), "s5_d": (512,)}


def make_inmaps(inputs, cores=range(8), n_exp=NEXP + 1):
    consts = make_consts()
    shared = {k: np.ascontiguousarray(np.asarray(inputs[k])[0]) for k in SHARED}
    for k, shp in RESHAPE.items():
        shared[k] = shared[k].reshape(shp)
    shared["final_g"] = np.ascontiguousarray(np.asarray(inputs["final_g"]))
    if "exp_w_gate" in inputs:
        for k, ks in (("exp_w_gate", "sh_w_gate"), ("exp_w_up", "sh_w_up"), ("exp_w_down", "sh_w_down")):
            full = np.concatenate([np.asarray(inputs[k])[0], np.asarray(inputs[ks])], axis=0) if n_exp == NEXP + 1 else np.asarray(inputs[k])[0]
            full = full[:n_exp]
            ne, kd, nn = full.shape
            shared[k] = np.ascontiguousarray(full.reshape(ne, kd // 128, 128, nn).transpose(0, 2, 1, 3)).reshape(ne * 128, (kd // 128) * nn)
    maps = []
    for b in cores:
        m = {k: np.ascontiguousarray(np.asarray(inputs[k])[b]) for k in PER_BATCH}
        m.update(shared)
        m["consts"] = consts
        maps.append(m)
    return maps


def kernel(**inputs):
    nc = build_program()
    res = run_bass_kernel_spmd(nc, make_inmaps(inputs), core_ids=list(range(8)))
    return np.stack([np.asarray(r["out"]) for r in res.results], axis=0)
```
